# Optimizing a Trainium2 kernel written in Bass

```python
import math
import jax
import jax.numpy as jnp
from jax import lax
import numpy as np

D_MODEL = 1024
BATCH = 16
SEQ = 4096
DEPTH = 2

HEAD_DIM = 64
N_GROUPS = 4
GROUP_WIDTH = D_MODEL // N_GROUPS
N_HEADS_GROUP = GROUP_WIDTH // HEAD_DIM
D_MIX = N_GROUPS * GROUP_WIDTH
ATTN_SCALE = HEAD_DIM ** -0.5
KV_RANK = 128
IDX_HEADS = 8
IDX_DIM = 32
TOPK_MAX = 256
Q_BLOCK = 128
MOBA_BLOCK = 256
MOBA_TOPK = 3
MOBA_QCHUNK = 32
HGRN_CHUNK = 64
DILATED_BRANCHES = ((128, 1), (512, 4), (2048, 16))
N_BUCKETS = 32
MAX_DISTANCE = 2048
N_ATTN_HEADS = 3 * N_HEADS_GROUP
D_FF = 2816
EPS = 1e-6
SPLITS = (GROUP_WIDTH, KV_RANK, IDX_HEADS * IDX_DIM, IDX_DIM, IDX_HEADS,
          GROUP_WIDTH, GROUP_WIDTH, GROUP_WIDTH,
          GROUP_WIDTH, GROUP_WIDTH, GROUP_WIDTH, GROUP_WIDTH,
          GROUP_WIDTH, GROUP_WIDTH, GROUP_WIDTH)
D_IN = sum(SPLITS)
SPLIT_POINTS = tuple(int(p) for p in np.cumsum(SPLITS)[:-1])

kernel_name = 'hybrid_parallel_dsa_moba_hgrn2_dilated_macaron'

F32 = jnp.float32


def rmsnorm(x, gain):
    xf = x.astype(F32)
    y = xf * lax.rsqrt(jnp.mean(xf * xf, axis=-1, keepdims=True) + EPS)
    return (y * gain.astype(F32)).astype(x.dtype)


def swiglu(h, w_gate, w_up, w_down):
    return (jax.nn.silu(h @ w_gate) * (h @ w_up)) @ w_down


def t5_bucket(dist):
    max_exact = N_BUCKETS // 2
    n = jnp.maximum(dist, 0)
    nf = jnp.maximum(n, 1).astype(F32)
    large = max_exact + (jnp.log(nf / max_exact) / math.log(MAX_DISTANCE / max_exact)
                         * (N_BUCKETS - max_exact)).astype(jnp.int32)
    large = jnp.minimum(large, N_BUCKETS - 1)
    return jnp.where(n < max_exact, n, large)


def dsa_mixer(q, ckv, iq, ik, iw, ckv_gain, w_kv_up, table):
    B, T, _ = q.shape
    H, hd = N_HEADS_GROUP, HEAD_DIM
    c = rmsnorm(ckv, ckv_gain)
    w_up = w_kv_up.reshape(KV_RANK, 2, H, hd)
    w_uk, w_uv = w_up[:, 0], w_up[:, 1]
    q = q.reshape(B, T, H, hd)
    iq = iq.reshape(B, T, IDX_HEADS, IDX_DIM)
    iw = iw * (IDX_HEADS * IDX_DIM) ** -0.5
    topk = min(TOPK_MAX, T // 4)
    key_pos = jnp.arange(T)
    bidx = jnp.arange(B)[:, None, None]

    def block(n):
        t0 = n * Q_BLOCK
        qb = lax.dynamic_slice_in_dim(q, t0, Q_BLOCK, 1)
        iqb = lax.dynamic_slice_in_dim(iq, t0, Q_BLOCK, 1)
        iwb = lax.dynamic_slice_in_dim(iw, t0, Q_BLOCK, 1)
        qpos = t0 + jnp.arange(Q_BLOCK)
        rel = jax.nn.relu(jnp.einsum('bqhd,bsd->bqhs', iqb, ik))
        index = jnp.einsum('bqh,bqhs->bqs', iwb, rel).astype(F32)
        index = jnp.where(key_pos[None, None, :] <= qpos[None, :, None], index, -jnp.inf)
        _, sel = lax.top_k(index, topk)
        valid = sel <= qpos[None, :, None]
        cg = c[bidx, sel]
        q_abs = jnp.einsum('bqhd,rhd->bqhr', qb, w_uk)
        logits = jnp.einsum('bqhr,bqkr->bhqk', q_abs, cg).astype(F32) * ATTN_SCALE
        bias = table[t5_bucket(qpos[None, :, None] - sel)]
        logits = jnp.where(valid[:, None], logits + jnp.moveaxis(bias, -1, 1).astype(F32), -jnp.inf)
        p = jax.nn.softmax(logits, axis=-1).astype(c.dtype)
        ctx = jnp.einsum('bhqk,bqkr->bqhr', p, cg)
        return jnp.einsum('bqhr,rhd->bqhd', ctx, w_uv)

    out = lax.map(block, jnp.arange(T // Q_BLOCK))
    return jnp.moveaxis(out, 0, 1).reshape(B, T, H * hd)


def moba_mixer(q, k, v, table):
    B, T, _ = q.shape
    H, hd, BLK, QC = N_HEADS_GROUP, HEAD_DIM, MOBA_BLOCK, MOBA_QCHUNK
    Tp = -(-T // BLK) * BLK
    nb = Tp // BLK
    topb = min(MOBA_TOPK, nb)

    def heads(a):
        a = a.reshape(B, T, H, hd).transpose(0, 2, 1, 3)
        return jnp.pad(a, ((0, 0), (0, 0), (0, Tp - T), (0, 0)))

    qh, kh, vh = heads(q), heads(k), heads(v)
    kb = kh.reshape(B, H, nb, BLK, hd)
    vb = vh.reshape(B, H, nb, BLK, hd)
    kmean = jnp.mean(kb.astype(F32), axis=3).astype(kh.dtype)
    bi = jnp.arange(B)[:, None, None, None]
    hi = jnp.arange(H)[None, :, None, None]
    hi5 = hi[..., None]
    blk_ids = jnp.arange(nb)
    inblk = jnp.arange(BLK)

    def step(i):
        t0 = i * QC
        own = t0 // BLK
        qb = lax.dynamic_slice_in_dim(qh, t0, QC, 2)
        qpos = t0 + jnp.arange(QC)
        gate = jnp.einsum('bhqd,bhnd->bhqn', qb, kmean).astype(F32)
        gate = jnp.where(blk_ids < own, gate, -jnp.inf)
        _, sel = lax.top_k(gate, topb)
        sel_ok = sel < own
        kg = kb[bi, hi, sel]
        vg = vb[bi, hi, sel]
        l_sel = jnp.einsum('bhqd,bhqnkd->bhqnk', qb, kg).astype(F32) * ATTN_SCALE
        d_sel = qpos[:, None, None] - (sel[..., None] * BLK + inblk)
        l_sel = jnp.where(sel_ok[..., None], l_sel + table[t5_bucket(d_sel), hi5].astype(F32), -jnp.inf)
        ko = lax.dynamic_index_in_dim(kb, own, axis=2, keepdims=False)
        vo = lax.dynamic_index_in_dim(vb, own, axis=2, keepdims=False)
        l_own = jnp.einsum('bhqd,bhkd->bhqk', qb, ko).astype(F32) * ATTN_SCALE
        d_own = qpos[:, None] - (own * BLK + inblk)[None, :]
        l_own = jnp.where(d_own >= 0, l_own + table[t5_bucket(d_own)].transpose(2, 0, 1).astype(F32), -jnp.inf)
        logits = jnp.concatenate([l_sel.reshape(B, H, QC, topb * BLK), l_own], axis=-1)
        p = jax.nn.softmax(logits, axis=-1).astype(vh.dtype)
        p_sel = p[..., :topb * BLK].reshape(B, H, QC, topb, BLK)
        p_own = p[..., topb * BLK:]
        return (jnp.einsum('bhqnk,bhqnkd->bhqd', p_sel, vg)
                + jnp.einsum('bhqk,bhkd->bhqd', p_own, vo))

    out = lax.map(step, jnp.arange(Tp // QC))
    return out.transpose(1, 0, 3, 2, 4).reshape(B, Tp, H * hd)[:, :T]


def hgrn2_mixer(q, fpre, inp, g, lb, norm_gain):
    B, T, _ = q.shape
    H, dk, dv, C = N_HEADS_GROUP, HEAD_DIM, HEAD_DIM, HGRN_CHUNK
    N = T // C
    f = lb + (1.0 - lb) * jax.nn.sigmoid(fpre.astype(F32))
    logf = jnp.log(f)
    kin = 1.0 - f

    def chunks(a, d):
        return a.astype(F32).reshape(B, N, C, H, d).transpose(1, 0, 3, 2, 4)

    qc, kc, vc = chunks(q, dk), chunks(kin, dk), chunks(inp, dv)
    bc = jnp.cumsum(chunks(logf, dk), axis=3)
    causal = jnp.arange(C)[:, None] >= jnp.arange(C)[None, :]

    def step(S, xs):
        qx, kx, vx, bx = xs
        diff = bx[:, :, :, None, :] - bx[:, :, None, :, :]
        decay = jnp.exp(jnp.where(causal[:, :, None], diff, -jnp.inf))
        A = jnp.einsum('bhtk,bhsk,bhtsk->bhts', qx, kx, decay)
        o = (jnp.einsum('bhts,bhsv->bhtv', A, vx)
             + jnp.einsum('bhtk,bhkv->bhtv', qx * jnp.exp(bx), S))
        b_last = bx[:, :, -1]
        S_new = (S * jnp.exp(b_last)[..., None]
                 + jnp.einsum('bhsk,bhsv->bhkv', kx * jnp.exp(b_last[:, :, None] - bx), vx))
        return S_new, o

    S0 = jnp.zeros((B, H, dk, dv), F32)
    _, o = lax.scan(step, S0, (qc, kc, vc, bc))
    o = o.transpose(1, 0, 3, 2, 4).reshape(B, T, H, dv)
    o = o * lax.rsqrt(jnp.mean(o * o, axis=-1, keepdims=True) + EPS)
    o = o.reshape(B, T, H * dv) * norm_gain.astype(F32) * jax.nn.silu(g.astype(F32))
    return o.astype(q.dtype)


def dilated_mixer(q, k, v, table):
    B, T, _ = q.shape
    H, hd = N_HEADS_GROUP, HEAD_DIM

    def heads(a):
        return a.reshape(B, T, H, hd).transpose(0, 2, 1, 3)

    qh, kh, vh = heads(q), heads(k), heads(v)
    ms, ss, nums = [], [], []
    for window, r in DILATED_BRANCHES:
        span = window // r
        unit = r * span
        Tp = -(-T // unit) * unit
        L = Tp // r
        nblk = L // span

        def strided(a):
            a = jnp.pad(a, ((0, 0), (0, 0), (0, Tp - T), (0, 0)))
            return a.reshape(B, H, L, r, hd).transpose(0, 1, 3, 2, 4).reshape(B, H, r, nblk, span, hd)

        def with_prev(a):
            prev = jnp.pad(a, ((0, 0), (0, 0), (0, 0), (1, 0), (0, 0), (0, 0)))[:, :, :, :-1]
            return jnp.concatenate([prev, a], axis=4)

        qs = strided(qh)
        kcat, vcat = with_prev(strided(kh)), with_prev(strided(vh))
        logits = jnp.einsum('bhrnqd,bhrnkd->bhrnqk', qs, kcat).astype(F32) * ATTN_SCALE
        a_i = jnp.arange(span)[:, None]
        c_i = jnp.arange(2 * span)[None, :]
        delta = a_i + span - c_i
        key_idx = jnp.arange(nblk)[:, None, None] * span - span + c_i[None]
        mask = (delta >= 0) & (delta <= span) & (key_idx >= 0)
        bias = table[t5_bucket(delta * r)].transpose(2, 0, 1)[:, None, None].astype(F32)
        logits = jnp.where(mask, logits + bias, -jnp.inf)
        m = jnp.max(logits, axis=-1)
        p = jnp.exp(logits - m[..., None])
        s = jnp.sum(p, axis=-1)
        num = jnp.einsum('bhrnqk,bhrnkd->bhrnqd', p.astype(vh.dtype), vcat).astype(F32)
        ms.append(m.reshape(B, H, r, L).transpose(0, 1, 3, 2).reshape(B, H, Tp)[:, :, :T])
        ss.append(s.reshape(B, H, r, L).transpose(0, 1, 3, 2).reshape(B, H, Tp)[:, :, :T])
        nums.append(num.reshape(B, H, r, L, hd).transpose(0, 1, 3, 2, 4).reshape(B, H, Tp, hd)[:, :, :T])
    m_stack = jnp.stack(ms)
    w = jnp.exp(m_stack - jnp.max(m_stack, axis=0))
    den = jnp.sum(w * jnp.stack(ss), axis=0)
    out = jnp.sum(w[..., None] * jnp.stack(nums), axis=0) / den[..., None]
    return out.transpose(0, 2, 1, 3).reshape(B, T, H * hd).astype(q.dtype)


def setup_inputs(seed: int = 0) -> dict:
    key = jax.random.key(seed)
    ks = jax.random.split(key, 20)

    def nrm(k, shape, scale):
        return jax.random.normal(k, shape, F32) * scale

    def gain(k, shape):
        return 1.0 + 0.05 * jax.random.normal(k, shape, F32)

    return {
        'x': nrm(ks[0], (BATCH, SEQ, D_MODEL), 1.0),
        'norm_ffn1': gain(ks[1], (DEPTH, D_MODEL)),
        'ffn1_gate': nrm(ks[2], (DEPTH, D_MODEL, D_FF), D_MODEL ** -0.5),
        'ffn1_up': nrm(ks[3], (DEPTH, D_MODEL, D_FF), D_MODEL ** -0.5),
        'ffn1_down': nrm(ks[4], (DEPTH, D_FF, D_MODEL), D_FF ** -0.5),
        'norm_mix': gain(ks[5], (DEPTH, D_MODEL)),
        'w_in': nrm(ks[6], (DEPTH, D_MODEL, D_IN), D_MODEL ** -0.5),
        'ckv_norm': gain(ks[7], (DEPTH, KV_RANK)),
        'w_kv_up': nrm(ks[8], (DEPTH, KV_RANK, 2 * GROUP_WIDTH), KV_RANK ** -0.5),
        'hgrn_lb_logits': nrm(ks[9], (DEPTH, GROUP_WIDTH), 0.5),
        'hgrn_norm': gain(ks[10], (DEPTH, GROUP_WIDTH)),
        'w_out': nrm(ks[11], (DEPTH, D_MIX, D_MODEL), D_MIX ** -0.5),
        'norm_ffn2': gain(ks[12], (DEPTH, D_MODEL)),
        'ffn2_gate': nrm(ks[13], (DEPTH, D_MODEL, D_FF), D_MODEL ** -0.5),
        'ffn2_up': nrm(ks[14], (DEPTH, D_MODEL, D_FF), D_MODEL ** -0.5),
        'ffn2_down': nrm(ks[15], (DEPTH, D_FF, D_MODEL), D_FF ** -0.5),
        'rel_bias': nrm(ks[16], (N_BUCKETS, N_ATTN_HEADS), 0.5),
        'norm_final': gain(ks[17], (D_MODEL,)),
    }


def reference(x, norm_ffn1, ffn1_gate, ffn1_up, ffn1_down, norm_mix, w_in, ckv_norm, w_kv_up,
              hgrn_lb_logits, hgrn_norm, w_out, norm_ffn2, ffn2_gate, ffn2_up, ffn2_down,
              rel_bias, norm_final):
    H = N_HEADS_GROUP
    lb_w = jax.nn.softmax(hgrn_lb_logits.astype(F32), axis=0)
    lower_bounds = jnp.cumsum(lb_w, axis=0) - lb_w[0]
    table_a, table_b, table_d = rel_bias[:, :H], rel_bias[:, H:2 * H], rel_bias[:, 2 * H:3 * H]
    for l in range(DEPTH):
        h = rmsnorm(x, norm_ffn1[l])
        x = x + 0.5 * swiglu(h, ffn1_gate[l], ffn1_up[l], ffn1_down[l])
        h = rmsnorm(x, norm_mix[l])
        (a_q, a_ckv, a_iq, a_ik, a_iw, b_q, b_k, b_v, c_q, c_f, c_i, c_g,
         d_q, d_k, d_v) = jnp.split(h @ w_in[l], SPLIT_POINTS, axis=-1)
        o_a = dsa_mixer(a_q, a_ckv, a_iq, a_ik, a_iw, ckv_norm[l], w_kv_up[l], table_a)
        o_b = moba_mixer(b_q, b_k, b_v, table_b)
        o_c = hgrn2_mixer(c_q, c_f, c_i, c_g, lower_bounds[l], hgrn_norm[l])
        o_d = dilated_mixer(d_q, d_k, d_v, table_d)
        x = x + jnp.concatenate([o_a, o_b, o_c, o_d], axis=-1) @ w_out[l]
        h = rmsnorm(x, norm_ffn2[l])
        x = x + 0.5 * swiglu(h, ffn2_gate[l], ffn2_up[l], ffn2_down[l])
    return rmsnorm(x, norm_final)
```

```python
import math
import contextlib
import numpy as np
import ml_dtypes
import concourse.bass as bass
import concourse.mybir as mybir
from concourse.bass_utils import run_bass_kernel_spmd

F32 = mybir.dt.float32
BF16 = mybir.dt.bfloat16
AF = mybir.ActivationFunctionType
ALU = mybir.AluOpType
AX = mybir.AxisListType

ENG = ('pe', 'act', 'dve', 'pool', 'sp')
SEM_PERIOD = 20000
NSLOT = 8
NEG = -30000.0
D = 1024
DFF = 2816
NFC = DFF // 128
EPS = 1e-6
SBUF_BYTES = 206 * 1024

OFF = dict(a_q=0, a_ckv=256, a_iq=384, a_ik=640, a_iw=672, b_q=680, b_k=936, b_v=1192,
           c_q=1448, c_f=1704, c_i=1960, c_g=2216, d_q=2472, d_k=2728, d_v=2984)


def win_cols():
    cols = []
    for nm in ('a_q', 'b_q', 'd_q', 'b_k', 'd_k'):
        cols += list(range(OFF[nm], OFF[nm] + 256))
    cols += list(range(OFF['a_ckv'], OFF['a_ckv'] + 128))
    for g in range(3):
        for hp in range(4):
            h = min(3 * g + hp, 7) if hp < 3 else min(3 * g, 7)
            cols += list(range(OFF['a_iq'] + 32 * h, OFF['a_iq'] + 32 * h + 32))
    for g in range(3):
        for hp in range(4):
            h = min(3 * g + hp, 7) if hp < 3 else min(3 * g, 7)
            cols += [OFF['a_iw'] + h] * 32
    for _ in range(4):
        cols += list(range(OFF['a_ik'], OFF['a_ik'] + 32))
    for nm in ('c_q', 'c_f', 'c_g'):
        cols += list(range(OFF[nm], OFF[nm] + 256))
    cols += list(range(OFF['b_v'], OFF['b_v'] + 256))
    cols += list(range(OFF['d_v'], OFF['d_v'] + 256))
    cols += list(range(OFF['c_i'], OFF['c_i'] + 256))
    return np.array(cols, dtype=np.int64)


NWX = 18 * 128 + 768 + 512 + 256
HG0 = 2304
TMV = 2304 + 768
TMI = TMV + 512

R_Q = {0: 0, 1: 256, 2: 512}
R_K = {1: 768, 2: 1024, 0: 1280}
R_IQP, R_IQN, R_IK = 1536, 1920, 2304
R_H = 2432
FMROWS = R_H + 5 * 256

UAB = 2048
UD = 2560
LAB = UAB + 128
LD = UD + 128
NEAR = 1664


def t5_bucket_np(d):
    n = np.maximum(d, 0)
    nf = np.maximum(n, 1).astype(np.float32)
    large = 16 + (np.log(nf / np.float32(16)) / np.float32(math.log(2048 / 16)) * np.float32(16)).astype(np.int32)
    large = np.minimum(large, 31)
    return np.where(n < 16, n, large)


def onehots():
    def mk(L, dil):
        x = np.arange(L)
        d = x - 127
        oh = np.zeros((33, L), np.float32)
        b = t5_bucket_np(d)
        if not dil:
            valid = d >= 0
            const = np.where(valid, 0.0, NEG)
        else:
            mult = ((d >= 0) & (d <= 128)).astype(np.int32) + ((d >= 0) & (d <= 512) & (d % 4 == 0)) \
                + ((d >= 0) & (d <= 2048) & (d % 16 == 0))
            valid = mult > 0
            const = np.where(valid, np.log(np.maximum(mult, 1)), NEG)
        oh[b[valid], x[valid]] = 1.0
        oh[32] = const
        return oh
    return mk(LAB, False), mk(LD, True)


class T:
    __slots__ = ('h', 'name', 'w', 'r', 'dram')

    def __init__(self, h, name, dram=False):
        self.h = h
        self.name = name
        self.w = None
        self.r = {}
        self.dram = dram

    def __getitem__(self, idx):
        return self.h[idx]


class Sched:
    def __init__(self, nc):
        self.nc = nc
        self.stack = contextlib.ExitStack()
        self.streams = {e: [] for e in ENG}
        self.known = {e: {} for e in ENG}
        self.marked = {e: set() for e in ENG}
        self.ndma = {e: 0 for e in ENG}
        self.selfsync = True
        self.arena = self.stack.enter_context(nc.sbuf_tensor("arena", [128, SBUF_BYTES // 4], F32))
        self.psum = self.stack.enter_context(nc.psum_tensor("psum", [128, 4096], F32))
        self.banks = [T(None, f"bank{i}") for i in range(8)]
        self.top = 0
        self.marks = []
        self.live = []
        self.dmaq = 0

    def tile(self, name, cols, dt, parts=128):
        nbytes = cols * (2 if dt == BF16 else 4)
        nbytes = (nbytes + 63) // 64 * 64
        assert self.top + nbytes <= SBUF_BYTES, (name, self.top, nbytes)
        a = self.arena[0:parts, self.top // 4:(self.top + nbytes) // 4]
        if dt == BF16:
            a = a.bitcast(BF16)
        a = a[:, 0:cols]
        self.top += nbytes
        t = T(a, name)
        self.live.append(t)
        return t

    def phase_begin(self):
        self.marks.append((self.top, len(self.live)))

    def phase_end(self):
        self.barrier()
        self.top, n = self.marks.pop()
        del self.live[n:]

    def pb(self, b, c0=0, c1=512, dt=F32, p0=0, p1=128):
        a = self.psum[p0:p1, b * 512:(b + 1) * 512]
        if dt == BF16:
            a = a.bitcast(BF16)
        return a[:, c0:c1]

    def pspan(self, b0, ncols, p0=0, p1=128):
        return self.psum[p0:p1, b0 * 512:b0 * 512 + ncols]

    def dram(self, name, shape, dt, kind="Internal"):
        h = self.nc.dram_tensor(name, list(shape), dt, kind=kind)
        return T(h.ap(), name, dram=True)

    def _need(self, eng, ev, waits, kind):
        if ev is None:
            return
        if ev[0] == 'c':
            if ev[1] == eng:
                if eng == 'pe' or kind != 'raw' or not self.selfsync:
                    return
            key = ('c', ev[1])
            val = ev[2]
        else:
            key = ('d', ev[1], ev[2])
            val = ev[3]
        if self.known[eng].get(key, -1) >= val:
            return
        self.known[eng][key] = val
        waits.append(ev)
        if ev[0] == 'c':
            self.marked[ev[1]].add(ev[2])

    def _deps(self, eng, r, w):
        waits = []
        for t in r:
            self._need(eng, t.w, waits, 'raw')
        for t in w:
            self._need(eng, t.w, waits, 'waw')
            for ev in t.r.values():
                self._need(eng, ev, waits, 'war')
        return waits

    def op(self, eng, fn, r=(), w=()):
        waits = self._deps(eng, r, w)
        idx = len(self.streams[eng])
        ev = ('c', eng, idx)
        self.streams[eng].append((waits, fn, None))
        for t in r:
            t.r[eng] = ev
        for t in w:
            t.w = ev
            t.r = {}
        return ev

    def dma(self, out_ap, in_ap, r=(), w=(), q=None, **kw):
        if q is None:
            q = 'sp'
        waits = self._deps(q, r, w)
        k = self.ndma[q]
        self.ndma[q] += 1
        slot = k % NSLOT
        cnt = k // NSLOT + 1
        if cnt > 1:
            self._need(q, ('d', q, slot, cnt - 1), waits, 'raw')
        ev = ('d', q, slot, cnt)
        self.streams[q].append(
            (waits, lambda e: e.dma_start(out=out_ap, in_=in_ap, **kw), (q, slot)))
        for t in r:
            t.r[('d', q, slot)] = ev
        for t in w:
            t.w = ev
            t.r = {}
        return ev

    def _all_events(self):
        evs = []
        for q in ENG:
            n = self.ndma[q]
            for slot in range(min(n, NSLOT)):
                evs.append(('d', q, slot, (n - 1 - slot) // NSLOT + 1))
        for e in ENG:
            n = len(self.streams[e])
            for i in range(n - 1, -1, -1):
                if self.streams[e][i][2] is None and self.streams[e][i][1] is not None:
                    evs.append(('c', e, i))
                    break
        return evs

    def barrier(self):
        evs = self._all_events()
        for e in ENG:
            waits = []
            for ev in evs:
                if ev[0] == 'c' and ev[1] == e:
                    continue
                self._need(e, ev, waits, 'raw')
            if waits:
                self.streams[e].append((waits, None, None))

    def finish(self):
        nc = self.nc
        self.barrier()
        self.streams['sp'].append(([], lambda e: e.nop(), None))
        rank = {}
        for e in ENG:
            ms = sorted(self.marked[e])
            rank[e] = {idx: i + 1 for i, idx in enumerate(ms)}
        csem = {e: [self.stack.enter_context(nc.semaphore(f"c_{e}_{i}"))
                    for i in range((len(rank[e]) + SEM_PERIOD - 1) // SEM_PERIOD)] for e in ENG}
        dsem = {}
        for q in ENG:
            for slot in range(min(self.ndma[q], NSLOT)):
                dsem[(q, slot)] = self.stack.enter_context(nc.semaphore(f"d_{q}_{slot}"))

        def ev2sem(ev):
            if ev[0] == 'c':
                c = rank[ev[1]][ev[2]]
                return csem[ev[1]][(c - 1) // SEM_PERIOD], (c - 1) % SEM_PERIOD + 1
            return dsem[(ev[1], ev[2])], 16 * ev[3]

        def replay(ename, eng):
            for idx, (waits, fn, dinfo) in enumerate(self.streams[ename]):
                for ev in waits:
                    s, v = ev2sem(ev)
                    eng.wait_ge(s, v)
                if fn is None:
                    continue
                inst = fn(eng)
                if dinfo is not None:
                    inst.then_inc(dsem[dinfo], 16)
                elif idx in rank[ename]:
                    c = rank[ename][idx]
                    inst.then_inc(csem[ename][(c - 1) // SEM_PERIOD], 1)

        allsems = [h for e in ENG for h in csem[e]] + list(dsem.values())
        for h in allsems:
            nc.gpsimd.sem_clear(h)
        nc.all_engine_barrier()
        with nc.Block() as block:
            @block.tensor
            def _(e):
                replay('pe', e)

            @block.scalar
            def _(e):
                replay('act', e)

            @block.vector
            def _(e):
                replay('dve', e)

            @block.gpsimd
            def _(e):
                replay('pool', e)

            @block.sync
            def _(e):
                replay('sp', e)
        nc.all_engine_barrier()
        for h in allsems:
            nc.gpsimd.sem_clear(h)
        nc.all_engine_barrier()
        self.stack.close()
        return {e: len(self.streams[e]) for e in ENG}

    def mm(self, out, lhsT, rhs, start, stop, r, w):
        return self.op('pe', lambda e: e.matmul(out, lhsT=lhsT, rhs=rhs, start=start, stop=stop,
                                                skip_group_check=True), r, w)

    def tr(self, out, in_, ident, r, w):
        return self.op('pe', lambda e: e.transpose(out, in_, ident), r, w)

    def act(self, out, in_, func, r, w, **kw):
        return self.op('act', lambda e: e.activation(out, in_, func, **kw), r, w)

    def ts(self, out, in0, s1, s2, op0, op1, r, w, eng='dve', accum_out=None):
        if op1 is None:
            return self.op(eng, lambda e: e.tensor_scalar(out, in0, s1, None, op0=op0), r, w)
        if accum_out is not None:
            return self.op(eng, lambda e: e.tensor_scalar(out, in0, s1, s2, op0=op0, op1=op1,
                                                          accum_out=accum_out), r, w)
        return self.op(eng, lambda e: e.tensor_scalar(out, in0, s1, s2, op0=op0, op1=op1), r, w)

    def stt(self, out, in0, sc, in1, op0, op1, r, w):
        return self.op('dve', lambda e: e.scalar_tensor_tensor(out, in0, sc, in1, op0=op0, op1=op1), r, w)

    def tt(self, out, in0, in1, op, r, w, eng='dve'):
        return self.op(eng, lambda e: e.tensor_tensor(out, in0, in1, op=op), r, w)

    def cp(self, out, in_, r, w, eng='dve'):
        if eng == 'act':
            return self.op('act', lambda e: e.copy(out, in_), r, w)
        return self.op(eng, lambda e: e.tensor_copy(out, in_), r, w)

    def recip(self, out, in_, r, w):
        return self.op('dve', lambda e: e.reciprocal(out, in_), r, w)

    def memset(self, ap, val, w, eng='dve'):
        return self.op(eng, lambda e: e.memset(ap, val), (), w)


class Ctx:
    pass


DBG = {'ffn_steps': 9, 'const': True, 'mixers': (0, 1, 2, 3), 'gate': True, 'bmask': True}


def load_cast(S, C, dst, dst_c0, src_ap, nrows, ncols, srcT, gcol=None):
    c = 0
    while c < ncols:
        n = min(512, ncols - c)
        st = C.stage[C.stage_i % 2]
        C.stage_i += 1
        S.dma(st[0:nrows, 0:n], src_ap[:, c:c + n], r=[srcT], w=[st])
        if gcol is None:
            S.ts(dst[0:nrows, dst_c0 + c:dst_c0 + c + n], st[0:nrows, 0:n], 1.0, 1.0, ALU.mult, ALU.mult,
                 [st], [dst], eng='pool')
        else:
            S.ts(dst[0:nrows, dst_c0 + c:dst_c0 + c + n], st[0:nrows, 0:n], gcol, 1.0, ALU.mult, ALU.mult,
                 [st, C.gcolT], [dst], eng='pool')
        c += n


def norm_front(S, C, xt, hbs, hT, junk, ssq, sd, rstd, ntr_bank=(0, 1)):
    for a in range(4):
        S.act(junk[:, :], xt[:, a * D:(a + 1) * D], AF.Square, [xt], [junk, ssq], accum_out=ssq[:, a:a + 1])
    S.act(sd[:, 0:4], ssq[:, 0:4], AF.Sqrt, [ssq, C.eps], [sd], scale=1.0 / D, bias=C.eps[:, 0:1])
    S.recip(rstd[:, 0:4], sd[:, 0:4], [sd], [rstd])
    hT3 = hT[:, :].rearrange("p (c t) -> p c t", c=8)
    for a in range(4):
        hb = hbs[a % 2]
        b = ntr_bank[a % 2]
        S.act(hb[:, :], xt[:, a * D:(a + 1) * D], AF.Copy, [xt, rstd], [hb], scale=rstd[:, a:a + 1])
        for c in range(8):
            S.tr(S.pb(b, c * 128, (c + 1) * 128, BF16), hb[:, c * 128:(c + 1) * 128], C.ident[:, :],
                 [hb, C.ident], [S.banks[b]])
        S.cp(hT3[:, :, a * 128:(a + 1) * 128], S.pb(b, 0, 1024, BF16).rearrange("p (c t) -> p c t", c=8),
             [S.banks[b]], [hT], eng=('act' if a % 2 else 'dve'))


def ffn_phase(S, C, l, which, src, dst, final):
    nm = 'ffn1' if which == 0 else 'ffn2'
    S.phase_begin()
    wg = S.tile('wg', 8 * DFF, BF16)
    wu = S.tile('wu', 8 * DFF, BF16)
    wd = S.tile('wd', NFC * D, BF16)
    C.stage = [S.tile('st0', 512, F32), S.tile('st1', 512, F32)]
    C.stage_i = 0
    gcol = S.tile('gcol', 8, F32)
    C.gcolT = gcol
    S.dma(gcol[:, :], C.din['g_' + nm][l], r=[C.dinT], w=[gcol])
    for c in range(8):
        load_cast(S, C, wg, c * DFF, C.din[nm + '_gate'][l, c * 128:(c + 1) * 128, :], 128, DFF, C.dinT, gcol[:, c:c + 1])
        load_cast(S, C, wu, c * DFF, C.din[nm + '_up'][l, c * 128:(c + 1) * 128, :], 128, DFF, C.dinT, gcol[:, c:c + 1])
    for f in range(NFC):
        load_cast(S, C, wd, f * D, C.din[nm + '_down'][l, f * 128:(f + 1) * 128, :], 128, D, C.dinT)
    xt = S.tile('xt', 4 * D, F32)
    junk = S.tile('junk', D, BF16)
    hb = [S.tile('hb0', D, BF16), S.tile('hb1', D, BF16)]
    hT = S.tile('hT', 8 * 512, BF16)
    aT = S.tile('aT', NFC * 512, BF16)
    sg = [S.tile('sg0', 512, BF16), S.tile('sg1', 512, BF16)]
    ssq = S.tile('ssq', 4, F32)
    sd = S.tile('sd', 4, F32)
    rstd = S.tile('rstd', 4, F32)
    if final:
        gfin = S.tile('gfin', D, F32)
        S.dma(gfin[:, :], C.din['norm_final_b'], r=[C.dinT], w=[gfin])
    for ti in range(C.NTOK // 512):
        t0 = ti * 512
        S.dma(xt[:, :].rearrange("p (a d) -> p a d", a=4), src[t0:t0 + 512, :].rearrange("(a p) d -> p a d", p=128),
              r=[src], w=[xt])
        if DBG['ffn_steps'] >= 1:
            norm_front(S, C, xt, hb, hT, junk, ssq, sd, rstd)
        for f in range(NFC if DBG['ffn_steps'] >= 2 else 0):
            bg = 2 + f % 2
            bu = 4 + f % 2
            for c in range(8):
                S.mm(S.pb(bg), wg[:, c * DFF + f * 128:c * DFF + (f + 1) * 128], hT[:, c * 512:(c + 1) * 512],
                     c == 0, c == 7, [wg, hT], [S.banks[bg]])
            for c in range(8):
                S.mm(S.pb(bu), wu[:, c * DFF + f * 128:c * DFF + (f + 1) * 128], hT[:, c * 512:(c + 1) * 512],
                     c == 0, c == 7, [wu, hT], [S.banks[bu]])
            s_ = sg[f % 2]
            S.act(s_[:, :], S.pb(bg), AF.Silu, [S.banks[bg]], [s_])
            S.tt(aT[:, f * 512:(f + 1) * 512], s_[:, :], S.pb(bu), ALU.mult, [s_, S.banks[bu]], [aT])
        for a in range(4 if DBG['ffn_steps'] >= 3 else 0):
            for hf in range(2):
                by = 6 + (a * 2 + hf) % 2
                for f in range(NFC):
                    S.mm(S.pb(by), aT[:, f * 512 + a * 128:f * 512 + (a + 1) * 128],
                         wd[:, f * D + hf * 512:f * D + (hf + 1) * 512], f == 0, f == NFC - 1, [aT, wd], [S.banks[by]])
                xs = xt[:, a * D + hf * 512:a * D + (hf + 1) * 512]
                S.stt(xs, S.pb(by), 0.5, xs, ALU.mult, ALU.add, [S.banks[by], xt], [xt])
        if final:
            for a in range(4):
                S.act(junk[:, :], xt[:, a * D:(a + 1) * D], AF.Square, [xt], [junk, ssq], accum_out=ssq[:, a:a + 1])
            S.act(sd[:, 0:4], ssq[:, 0:4], AF.Sqrt, [ssq, C.eps], [sd], scale=1.0 / D, bias=C.eps[:, 0:1])
            S.recip(rstd[:, 0:4], sd[:, 0:4], [sd], [rstd])
            for a in range(4):
                xs = xt[:, a * D:(a + 1) * D]
                S.stt(xs, xs, rstd[:, a:a + 1], gfin[:, :], ALU.mult, ALU.mult, [xt, rstd, gfin], [xt])
        S.dma(dst[t0:t0 + 512, :].rearrange("(a p) d -> p a d", p=128), xt[:, :].rearrange("p (a d) -> p a d", a=4),
              r=[xt], w=[dst])
    S.phase_end()


def proj_phase(S, C, l):
    S.phase_begin()
    NTOK = C.NTOK
    wi = S.tile('wi', 8 * NWX, BF16)
    wkv = S.tile('wkv', 512, BF16)
    C.stage = [S.tile('st0', 512, F32), S.tile('st1', 512, F32)]
    C.stage_i = 0
    gcol = S.tile('gcol', 8, F32)
    C.gcolT = gcol
    S.dma(gcol[:, :], C.din['g_mix'][l], r=[C.dinT], w=[gcol])
    for c in range(8):
        load_cast(S, C, wi, c * NWX, C.din['w_in_x'][l, c * 128:(c + 1) * 128, :], 128, NWX, C.dinT, gcol[:, c:c + 1])
    load_cast(S, C, wkv, 0, C.din['w_kv_up'][l], 128, 512, C.dinT)
    gck = S.tile('gck', 1, F32)
    S.dma(gck[:, :], C.din['g_ckv'][l], r=[C.dinT], w=[gck])
    lbl = S.tile('lbl', C.DEPTH * 4, F32, parts=64)
    S.dma(lbl[:, :], C.din['lb_logits'], r=[C.dinT], w=[lbl])
    lbe = S.tile('lbe', C.DEPTH * 4, F32, parts=64)
    for i in range(C.DEPTH):
        S.tt(lbe[:, i * 4:(i + 1) * 4], lbl[:, i * 4:(i + 1) * 4], lbl[:, 0:4], ALU.subtract, [lbl], [lbe])
    S.act(lbe[:, :], lbe[:, :], AF.Exp, [lbe], [lbe])
    tot = S.tile('lbtot', 4, F32, parts=64)
    num = S.tile('lbnum', 4, F32, parts=64)
    S.cp(tot[:, :], lbe[:, 0:4], [lbe], [tot])
    S.memset(num[:, :], 0.0, [num])
    for i in range(1, C.DEPTH):
        S.tt(tot[:, :], tot[:, :], lbe[:, i * 4:(i + 1) * 4], ALU.add, [tot, lbe], [tot])
        if i <= l:
            S.tt(num[:, :], num[:, :], lbe[:, i * 4:(i + 1) * 4], ALU.add, [num, lbe], [num])
    lb = S.tile('lb', 4, F32, parts=64)
    oml = S.tile('oml', 4, F32, parts=64)
    S.recip(tot[:, :], tot[:, :], [tot], [tot])
    S.tt(lb[:, :], num[:, :], tot[:, :], ALU.mult, [num, tot], [lb])
    S.ts(oml[:, :], lb[:, :], -1.0, 1.0, ALU.mult, ALU.add, [lb], [oml])

    xt = S.tile('xt', 4 * D, F32)
    junk = S.tile('junk', D, BF16)
    hb = [S.tile('hb0', D, BF16), S.tile('hb1', D, BF16)]
    hT = S.tile('hT', 8 * 512, BF16)
    ssq = S.tile('ssq', 4, F32)
    sd = S.tile('sd', 4, F32)
    rstd = S.tile('rstd', 4, F32)
    qk = S.tile('qk', 10 * 512, BF16)
    cf = S.tile('cf', 512, F32)
    csq = S.tile('csq', 512, BF16)
    crs = S.tile('crs', 512, F32)
    cTb = S.tile('cTb', 512, BF16)
    kast = S.tile('kast', 2 * 512, BF16)
    iqs = S.tile('iqs', 7 * 512, BF16)
    wP = S.tile('wP', 512, F32)
    wN = S.tile('wN', 512, F32)
    vst = S.tile('vst', 4 * 3 * 260, BF16)
    hvst = S.tile('hvst', 8 * 256, BF16, parts=64)
    hst = S.tile('hst', 5 * 4 * 512, BF16, parts=64)
    dst_ = S.tile('dst', 32, F32, parts=64)
    hf_ = [S.tile(f'hf{i}', 512, F32, parts=64) for i in range(8)]
    S.memset(vst[:, :], 1.0, [vst])
    vst4 = vst[:, :].rearrange("p (a m h c) -> p a m h c", a=4, m=3, h=4)
    fm = C.fm
    for ti in range(NTOK // 512):
        t0 = ti * 512
        S.dma(xt[:, :].rearrange("p (a d) -> p a d", a=4), C.xres[t0:t0 + 512, :].rearrange("(a p) d -> p a d", p=128),
              r=[C.xres], w=[xt])
        norm_front(S, C, xt, hb, hT, junk, ssq, sd, rstd)

        def fmproj(bank, col0, M):
            for c in range(8):
                S.mm(S.pb(bank, 0, 512, F32, 0, M), wi[:, c * NWX + col0:c * NWX + col0 + M], hT[:, c * 512:(c + 1) * 512],
                     c == 0, c == 7, [wi, hT], [S.banks[bank]])
        for g in range(10):
            b = 2 + g % 2
            fmproj(b, g * 128, 128)
            if g < 6:
                S.act(qk[:, g * 512:(g + 1) * 512], S.pb(b), AF.Copy, [S.banks[b]], [qk], scale=0.125)
            else:
                S.cp(qk[:, g * 512:(g + 1) * 512], S.pb(b), [S.banks[b]], [qk])
        S.dma(fm[0:1280, t0:t0 + 512].rearrange("(g p) t -> p g t", p=128), qk[:, :].rearrange("p (g t) -> p g t", g=10),
              r=[qk], w=[fm])
        fmproj(4, 10 * 128, 128)
        S.cp(cf[:, :], S.pb(4), [S.banks[4]], [cf], eng='act')
        S.act(csq[:, :], S.pb(4), AF.Square, [S.banks[4]], [csq])
        S.mm(S.pb(5), C.ones[:, :], csq[:, :], True, True, [C.ones, csq], [S.banks[5]])
        S.act(crs[:, :], S.pb(5), AF.Sqrt, [S.banks[5], C.eps], [crs], scale=1.0 / 128, bias=C.eps[:, 0:1])
        S.recip(crs[:, :], crs[:, :], [crs], [crs])
        S.stt(cTb[:, :], cf[:, :], gck[:, 0:1], crs[:, :], ALU.mult, ALU.mult, [cf, gck, crs], [cTb])
        for pr in range(2):
            S.mm(S.pb(6 + pr), wkv[:, pr * 128:(pr + 1) * 128], cTb[:, :], True, True, [wkv, cTb], [S.banks[6 + pr]])
            S.act(kast[:, pr * 512:(pr + 1) * 512], S.pb(6 + pr), AF.Copy, [S.banks[6 + pr]], [kast])
        S.dma(fm[R_K[0]:R_K[0] + 256, t0:t0 + 512].rearrange("(g p) t -> p g t", p=128),
              kast[:, :].rearrange("p (g t) -> p g t", g=2), r=[kast], w=[fm])
        for a in range(4):
            b = 2 + a % 2
            S.mm(S.pb(b, 0, 256), cTb[:, a * 128:(a + 1) * 128], wkv[:, 256:512], True, True, [cTb, wkv], [S.banks[b]])
            S.cp(vst4[:, a, 0, :, 0:64], S.pb(b, 0, 256).rearrange("p (h c) -> p h c", h=4), [S.banks[b]], [vst])
        for g in range(3):
            fmproj(4, (11 + g) * 128, 128)
            fmproj(5, (14 + g) * 128, 128)
            S.act(wP[:, :], S.pb(5), AF.Relu, [S.banks[5]], [wP])
            S.act(wN[:, :], S.pb(5), AF.Relu, [S.banks[5]], [wN], scale=-1.0)
            S.tt(iqs[:, g * 512:(g + 1) * 512], S.pb(4), wP[:, :], ALU.mult, [S.banks[4], wP], [iqs])
            S.stt(iqs[:, (3 + g) * 512:(4 + g) * 512], S.pb(4), -1.0, wN[:, :], ALU.mult, ALU.mult, [S.banks[4], wN], [iqs])
        fmproj(6, 17 * 128, 128)
        S.cp(iqs[:, 6 * 512:7 * 512], S.pb(6), [S.banks[6]], [iqs], eng='act')
        S.dma(fm[R_IQP:R_IQP + 896, t0:t0 + 512].rearrange("(g p) t -> p g t", p=128),
              iqs[:, :].rearrange("p (g t) -> p g t", g=7), r=[iqs], w=[fm])
        for a in range(4):
            b = 2 + a % 2
            for c in range(8):
                S.mm(S.pb(b), hT[:, c * 512 + a * 128:c * 512 + (a + 1) * 128], wi[:, c * NWX + TMV:c * NWX + TMV + 512],
                     c == 0, c == 7, [hT, wi], [S.banks[b]])
            S.cp(vst4[:, a, 1:3, :, 0:64], S.pb(b).rearrange("p (m h c) -> p m h c", m=2, h=4), [S.banks[b]], [vst],
                 eng=('act' if a % 2 else 'dve'))
        for m_ in range(3):
            S.dma(C.v1s[m_, t0:t0 + 512, :].rearrange("(a p) c -> p a c", p=128),
                  vst[:, :].rearrange("p (a m c) -> p a m c", a=4, m=3)[:, :, m_, :], r=[vst], w=[C.v1s])
        for ch in range(8):
            b = 4 + (ch // 2) % 2
            o = (ch % 2) * 256
            for c in range(8):
                S.mm(S.pb(b, o, o + 256, F32, 0, 64), hT[:, c * 512 + ch * 64:c * 512 + (ch + 1) * 64],
                     wi[:, c * NWX + TMI:c * NWX + TMI + 256], c == 0, c == 7, [hT, wi], [S.banks[b]])
            S.cp(hvst[:, ch * 256:(ch + 1) * 256], S.pb(b, o, o + 256, F32, 0, 64), [S.banks[b]], [hvst],
                 eng=('act' if ch % 2 else 'dve'))
        S.dma(C.hv[t0:t0 + 512, :].rearrange("(c p) v -> p c v", p=64), hvst[:, :].rearrange("p (c v) -> p c v", c=8),
              r=[hvst], w=[C.hv])
        hst4 = hst[:, :].rearrange("p (k h t) -> p k h t", k=5, h=4)
        for hd in range(4):
            fmproj(6, HG0 + hd * 64, 64)
            fmproj(7, HG0 + 256 + hd * 64, 64)
            fmproj(2, HG0 + 512 + hd * 64, 64)
            q_ps = S.pb(6, 0, 512, F32, 0, 64)
            sig, f_, lf, kin, b_, d1, d4, e1 = hf_
            S.act(sig[:, :], S.pb(7, 0, 512, F32, 0, 64), AF.Sigmoid, [S.banks[7]], [sig])
            S.ts(f_[:, :], sig[:, :], oml[:, hd:hd + 1], lb[:, hd:hd + 1], ALU.mult, ALU.add, [sig, oml, lb], [f_])
            S.act(lf[:, :], f_[:, :], AF.Ln, [f_], [lf])
            S.ts(kin[:, :], f_[:, :], -1.0, 1.0, ALU.mult, ALU.add, [f_], [kin])
            S.op('dve', lambda e, b_=b_, lf=lf: e.tensor_tensor_scan(b_[:, :], C.cmask[:, :], lf[:, :], 0.0,
                                                                        op0=ALU.mult, op1=ALU.add), [C.cmask, lf], [b_])
            b3 = b_[:, :].rearrange("p (c j) -> p c j", j=64)
            S.tt(d1[:, :].rearrange("p (c j) -> p c j", j=64), b3, b3[:, :, 31:32].broadcast_to([64, 8, 64]),
                 ALU.subtract, [b_], [d1])
            S.tt(d4[:, :].rearrange("p (c j) -> p c j", j=64), b3[:, :, 63:64].broadcast_to([64, 8, 64]), b3,
                 ALU.subtract, [b_], [d4])
            S.act(e1[:, :], d1[:, :], AF.Exp, [d1], [e1])
            S.tt(hst4[:, 0, hd, :], q_ps, e1[:, :], ALU.mult, [S.banks[6], e1], [hst])
            S.act(e1[:, :], d1[:, :], AF.Exp, [d1], [e1], scale=-1.0)
            S.tt(hst4[:, 1, hd, :], kin[:, :], e1[:, :], ALU.mult, [kin, e1], [hst])
            S.act(e1[:, :], b_[:, :], AF.Exp, [b_], [e1])
            S.tt(hst4[:, 2, hd, :], q_ps, e1[:, :], ALU.mult, [S.banks[6], e1], [hst])
            S.cp(dst_[:, hd * 8:(hd + 1) * 8], e1[:, :].rearrange("p (c j) -> p c j", j=64)[:, :, 63], [e1], [dst_])
            S.act(e1[:, :], d4[:, :], AF.Exp, [d4], [e1])
            S.tt(hst4[:, 3, hd, :], kin[:, :], e1[:, :], ALU.mult, [kin, e1], [hst])
            S.act(hst4[:, 4, hd, :], S.pb(2, 0, 512, F32, 0, 64), AF.Silu, [S.banks[2]], [hst])
        for k_ in range(5):
            S.dma(fm[R_H + k_ * 256:R_H + (k_ + 1) * 256, t0:t0 + 512].rearrange("(h p) t -> p h t", p=64), hst4[:, k_],
                  r=[hst], w=[fm])
        S.dma(C.hdec[:, t0 // 64:t0 // 64 + 8].rearrange("(h p) c -> p h c", p=64),
              dst_[:, :].rearrange("p (h c) -> p h c", h=4), r=[dst_], w=[C.hdec])
    S.phase_end()


def attn_group(S, C, m, G, s0, qT, kT, v1, HK, extra, pts, ost, st):
    T_ = C.T
    nkt = 4 * G + 4
    jmin = max(0, 4 * G - 16) if m == 2 else 0
    for hl in range(4):
        pr, hf = divmod(hl, 2)
        p0 = 64 * hf
        head = m * 4 + hl
        bo = 3 + st['oi'] % 2
        st['oi'] += 1
        for j in range(jmin, nkt):
            c0 = max(0, j - 4 * G) * 128
            N = 512 - c0
            dist0 = (4 * G * 128 + c0) - j * 128
            near = (m == 2) or dist0 < NEAR
            bs = st['si'] % 3
            st['si'] += 1
            nmm = 1 + (1 if near else 0)
            if m == 0:
                nmm += sum(1 for qt in range(4) if 4 * G + qt >= j)
            if m == 1 and DBG['bmask']:
                nmm += 1
            k = 0
            S.mm(S.pb(bs, c0, 512), kT[p0:p0 + 64, pr * T_ + j * 128:pr * T_ + (j + 1) * 128],
                 qT[p0:p0 + 64, pr * T_ + G * 512 + c0:pr * T_ + (G + 1) * 512], True, nmm == 1, [kT, qT], [S.banks[bs]])
            k += 1
            if near:
                S.mm(S.pb(bs, c0, 512), C.antiI[:, :], HK[hl][:, dist0:dist0 + N], False, k == nmm - 1,
                     [C.antiI, HK[hl]], [S.banks[bs]])
                k += 1
            if m == 0:
                for qt in range(4):
                    if 4 * G + qt >= j:
                        mk = extra[qt]
                        S.mm(S.pb(bs, qt * 128, (qt + 1) * 128), mk[:, j * 128:(j + 1) * 128], C.ident[:, :], False,
                             k == nmm - 1, [mk, C.ident], [S.banks[bs]])
                        k += 1
            if m == 1 and DBG['bmask']:
                ex = extra[hl // 2]
                hq = 32 * (hl % 2)
                S.mm(S.pb(bs, c0, 512), C.esel[hq:hq + 32, (j // 2) * 128:(j // 2 + 1) * 128],
                     ex[hq:hq + 32, c0:512], False, True, [C.esel, ex], [S.banks[bs]])
            pt = pts[st['pi'] % 3]
            st['pi'] += 1
            bcol = C.negM[:, 0:1] if near else C.far[:, head:head + 1]
            S.act(pt[:, c0:512], S.pb(bs, c0, 512), AF.Exp, [S.banks[bs], C.far, C.negM], [pt], bias=bcol)
            S.mm(S.pb(bo, c0, 512, F32, 0, 65), v1[:, j * 260 + hl * 65:j * 260 + hl * 65 + 65], pt[:, c0:512],
                 j == jmin, j == nkt - 1, [v1, pt], [S.banks[bo]])
        den, dhi, dlo, rd = st['den'], st['dhi'], st['dlo'], st['rd']
        S.cp(den[64:65, :], S.pb(bo, 0, 512, F32, 64, 65), [S.banks[bo]], [den], eng='act')
        S.cp(dhi[64:65, :], den[64:65, :], [den], [dhi])
        S.tt(dlo[64:65, :], den[64:65, :], dhi[64:65, :], ALU.subtract, [den, dhi], [dlo])
        S.mm(S.pb(6, 0, 512, F32, 0, 64), C.ones[64:65, 0:64], dhi[64:65, :], True, False, [C.ones, dhi], [S.banks[6]])
        S.mm(S.pb(6, 0, 512, F32, 0, 64), C.ones[64:65, 0:64], dlo[64:65, :], False, True, [C.ones, dlo], [S.banks[6]])
        S.recip(rd[0:64, :], S.pb(6, 0, 512, F32, 0, 64), [S.banks[6]], [rd])
        S.tt(ost[0:64, hl * 512:(hl + 1) * 512], S.pb(bo, 0, 512, F32, 0, 64), rd[0:64, :], ALU.mult,
             [S.banks[bo], rd], [ost])
    S.dma(C.oT[m_rows(m):m_rows(m) + 256, s0 + G * 512:s0 + (G + 1) * 512].rearrange("(h p) t -> p h t", p=64),
          ost[0:64, :].rearrange("p (h t) -> p h t", h=4), r=[ost], w=[C.oT])


def m_rows(m):
    return {0: 0, 1: 256, 2: 768}[m]


def load_hankel(S, C, m, HK):
    for hl in range(4):
        head = m * 4 + hl
        U = UD if m == 2 else UAB
        L = LD if m == 2 else LAB
        src = bass.AP(tensor=C.vecs.h.tensor, offset=head * LD, ap=[[1, 128], [1, U]])
        S.dma(HK[hl][:, 0:U], src, r=[C.vecs], w=[HK[hl]])


def mixer_attn_phase(S, C, l, si, m):
    T_ = C.T
    s0 = si * T_
    S.phase_begin()
    qT = S.tile('qT', 2 * T_, BF16)
    kT = S.tile('kT', 2 * T_, BF16)
    v1 = S.tile('v1', (T_ // 128) * 260, BF16)
    HK = [S.tile(f'HK{i}', UD if m == 2 else UAB, BF16) for i in range(4)]
    pts = [S.tile(f'pt{i}', 512, BF16) for i in range(3)]
    ost = S.tile('ost', 4 * 512, BF16, parts=64)
    st = dict(oi=0, si=0, pi=0, den=S.tile('den', 512, F32), dhi=S.tile('dhi', 512, BF16),
              dlo=S.tile('dlo', 512, BF16), rd=S.tile('rd', 512, F32, parts=64))
    fm = C.fm
    S.dma(qT[:, :].rearrange("p (g t) -> p g t", g=2), fm[R_Q[m]:R_Q[m] + 256, s0:s0 + T_].rearrange("(g p) t -> p g t", p=128),
          r=[fm], w=[qT])
    S.dma(kT[:, :].rearrange("p (g t) -> p g t", g=2), fm[R_K[m]:R_K[m] + 256, s0:s0 + T_].rearrange("(g p) t -> p g t", p=128),
          r=[fm], w=[kT])
    for j0 in range(0, T_ // 128, 4):
        S.dma(v1[:, j0 * 260:(j0 + 4) * 260].rearrange("p (j c) -> p j c", c=260),
              C.v1s[m, s0 + j0 * 128:s0 + (j0 + 4) * 128, :].rearrange("(j p) c -> p j c", p=128), r=[C.v1s], w=[v1])
    load_hankel(S, C, m, HK)
    NG = T_ // 512
    if m == 2:
        for G in range(NG):
            attn_group(S, C, m, G, s0, qT, kT, v1, HK, None, pts, ost, st)
    elif m == 1:
        nb = T_ // 256
        C.esel = S.tile('esel', 16 * 128, BF16, parts=64)
        S.dma(C.esel[:, :], C.din['c_esel'], r=[C.dinT], w=[C.esel])
        kmf = S.tile('kmf', 2 * nb, F32)
        kmT = S.tile('kmT', 2 * 16, BF16)
        S.memset(kmT[:, :], 0.0, [kmT])
        S.op('dve', lambda e: e.tensor_reduce(kmf[:, :].rearrange("p (g n) -> p g n", g=2), kT[:, :].rearrange("p (g n k) -> p g n k", g=2, n=nb),
                                              axis=AX.X, op=ALU.add), [kT], [kmf])
        S.ts(kmT[:, :].rearrange("p (g n) -> p g n", g=2)[:, :, 0:nb], kmf[:, :].rearrange("p (g n) -> p g n", g=2),
             1.0 / 256, None, ALU.mult, None, [kmf], [kmT])
        gm = S.tile('gm', 64, F32)
        m8 = S.tile('m8', 32, F32)
        mb = S.tile('mb', 128, BF16)
        mbT = [S.tile('mbT0', 512, BF16, parts=64), S.tile('mbT1', 512, BF16, parts=64)]
        S.memset(gm[:, :], -1e30, [gm])
        S.memset(mb[:, :], 0.0, [mb])
        for G in range(NG):
            for qt in range(4):
                n = 4 * G + qt
                own = n // 2
                if own > 0 and DBG['gate']:
                    for hl in range(4):
                        pr, hf = divmod(hl, 2)
                        p0 = 64 * hf
                        gb = 7 if hf == 0 else 5
                        S.mm(S.pb(gb, pr * 16, pr * 16 + own), qT[p0:p0 + 64, pr * T_ + n * 128:pr * T_ + (n + 1) * 128],
                             kmT[p0:p0 + 64, pr * 16:pr * 16 + own], True, True, [qT, kmT], [S.banks[gb]])
                    gm4 = gm[:, :].rearrange("p (r f n) -> p r f n", r=2, f=2)
                    for hf_ in range(2):
                        gb = 7 if hf_ == 0 else 5
                        S.cp(gm4[:, :, hf_, 0:own], S.pb(gb, 0, 32).rearrange("p (r n) -> p r n", r=2)[:, :, 0:own],
                             [S.banks[gb]], [gm])
                for hl in range(4):
                    S.op('dve', lambda e, hl=hl: e.max(m8[:, hl * 8:(hl + 1) * 8], gm[:, hl * 16:(hl + 1) * 16]), [gm], [m8])
                    S.ts(mb[:, hl * 32:hl * 32 + 16], gm[:, hl * 16:(hl + 1) * 16], m8[:, hl * 8 + 2:hl * 8 + 3], NEG,
                         ALU.is_lt, ALU.mult, [gm, m8], [mb])
                S.memset(mb[:, :].rearrange("p (h n) -> p h n", h=4)[:, :, own:own + 1], 0.0, [mb])
                for pr in range(2):
                    S.tr(S.pb(7, 256 + pr * 128, 384 + pr * 128, BF16, 0, 64), mb[:, pr * 64:(pr + 1) * 64], C.ident[:, :],
                         [mb, C.ident], [S.banks[7]])
                    S.cp(mbT[pr][:, qt * 128:(qt + 1) * 128], S.pb(7, 256 + pr * 128, 384 + pr * 128, BF16, 0, 64),
                         [S.banks[7]], [mbT[pr]], eng='act')
            attn_group(S, C, m, G, s0, qT, kT, v1, HK, mbT, pts, ost, st)
    else:
        iqP = S.tile('iqP', 3 * T_, BF16)
        iqN = S.tile('iqN', 3 * T_, BF16)
        ikT = S.tile('ikT', T_, BF16)
        S.dma(iqP[:, :].rearrange("p (g t) -> p g t", g=3), fm[R_IQP:R_IQP + 384, s0:s0 + T_].rearrange("(g p) t -> p g t", p=128),
              r=[fm], w=[iqP])
        S.dma(iqN[:, :].rearrange("p (g t) -> p g t", g=3), fm[R_IQN:R_IQN + 384, s0:s0 + T_].rearrange("(g p) t -> p g t", p=128),
              r=[fm], w=[iqN])
        S.dma(ikT[:, :], fm[R_IK:R_IK + 128, s0:s0 + T_], r=[fm], w=[ikT])
        idx = S.tile('idx', T_, F32)
        jk = S.tile('jk', T_, BF16)
        mks = [S.tile(f'mk{i}', T_, BF16) for i in range(4)]
        cand = S.tile('cand', 1, F32)
        cnt = S.tile('cnt', 1, F32)
        dd = S.tile('dd', 1, F32)
        topk = min(256, T_ // 4)
        R = 512.0
        it = 0
        for G in range(NG):
            for qt in range(4):
                n = 4 * G + qt
                Sn = (n + 1) * 128
                first = True
                for sgn in range(2):
                    src = iqP if sgn == 0 else iqN
                    aop = ALU.max if sgn == 0 else ALU.min
                    for h in range(8):
                        g, hp = divmod(h, 3)
                        for c_lo in range(0, Sn, 2048):
                            c_hi = min(Sn, c_lo + 2048)
                            b0 = (it % 2) * 4
                            it += 1
                            nb_ = (c_hi - c_lo + 511) // 512
                            for k in range(nb_):
                                a0 = c_lo + k * 512
                                a1 = min(c_hi, a0 + 512)
                                S.mm(S.pb(b0 + k, 0, a1 - a0), src[32 * hp:32 * hp + 32, g * T_ + n * 128:g * T_ + (n + 1) * 128],
                                     ikT[32 * hp:32 * hp + 32, a0:a1], True, True, [src, ikT], [S.banks[b0 + k]])
                            bl = [S.banks[b0 + k] for k in range(nb_)]
                            if first:
                                S.ts(idx[:, c_lo:c_hi], S.pspan(b0, c_hi - c_lo), 0.0, None, aop, None, bl, [idx])
                            else:
                                S.stt(idx[:, c_lo:c_hi], S.pspan(b0, c_hi - c_lo), 0.0, idx[:, c_lo:c_hi], aop, ALU.add,
                                      bl + [idx], [idx])
                        first = False
                S.tt(idx[:, n * 128:(n + 1) * 128], idx[:, n * 128:(n + 1) * 128], C.tri[:, :], ALU.add, [idx, C.tri], [idx])
                mk = mks[qt]
                if Sn <= topk:
                    S.ts(mk[:, 0:Sn], idx[:, 0:Sn], -1e29, NEG, ALU.is_lt, ALU.mult, [idx], [mk])
                    continue
                S.memset(cand[:, :], 0.0, [cand])
                step = R
                for i in range(C.NBIS):
                    S.ts(jk[:, 0:Sn], idx[:, 0:Sn], cand[:, 0:1], 0.0, ALU.is_ge, ALU.add, [idx, cand], [jk, cnt],
                         accum_out=cnt[:, 0:1])
                    S.ts(dd[:, :], cnt[:, :], topk - 0.5, step, ALU.is_ge, ALU.mult, [cnt], [dd])
                    S.stt(cand[:, :], dd[:, :], -step / 2, cand[:, :], ALU.add, ALU.add, [dd, cand], [cand])
                    step /= 2
                S.ts(cand[:, :], cand[:, :], -step, None, ALU.add, None, [cand], [cand])
                S.ts(mk[:, 0:Sn], idx[:, 0:Sn], cand[:, 0:1], NEG, ALU.is_lt, ALU.mult, [idx, cand], [mk])
            attn_group(S, C, m, G, s0, qT, kT, v1, HK, mks, pts, ost, st)
    S.phase_end()


def hgrn_phase(S, C, l, si):
    T_ = C.T
    s0 = si * T_
    S.phase_begin()
    fm = C.fm
    ins = [S.tile(f'hin{i}', 5 * 4 * 512, BF16, parts=64) for i in range(2)]
    vin = [S.tile(f'hvin{i}', 8 * 256, BF16, parts=64) for i in range(2)]
    dec = [S.tile(f'hdec{i}', 32, F32, parts=64) for i in range(2)]
    ATs = S.tile('ATs', 256, BF16, parts=64)
    ksT = S.tile('ksT', 256, BF16, parts=64)
    S32 = S.tile('S32', 256, F32, parts=64)
    Sbf = S.tile('Sbf', 256, BF16, parts=64)
    sq = S.tile('hsq', 512, BF16, parts=64)
    sdt = S.tile('hsd', 512, F32, parts=64)
    t1 = S.tile('ht1', 512, F32, parts=64)
    ost = S.tile('host', 4 * 512, BF16, parts=64)
    hg = S.tile('hgain', 4, F32, parts=64)
    S.dma(hg[:, :], C.din['g_hgrn'][l], r=[C.dinT], w=[hg])
    for ti in range(T_ // 512):
        t0 = s0 + ti * 512
        i4 = ins[ti % 2][:, :].rearrange("p (k h t) -> p k h t", k=5, h=4)
        for k_ in range(5):
            S.dma(i4[:, k_], fm[R_H + k_ * 256:R_H + (k_ + 1) * 256, t0:t0 + 512].rearrange("(h p) t -> p h t", p=64),
                  r=[fm], w=[ins[ti % 2]])
        vv = vin[ti % 2]
        S.dma(vv[:, :].rearrange("p (c v) -> p c v", c=8), C.hv[t0:t0 + 512, :].rearrange("(c p) v -> p c v", p=64),
              r=[C.hv], w=[vv])
        dc = dec[ti % 2]
        S.dma(dc[:, :].rearrange("p (h c) -> p h c", h=4), C.hdec[:, t0 // 64:t0 // 64 + 8].rearrange("(h p) c -> p h c", p=64),
              r=[C.hdec], w=[dc])
        inT = ins[ti % 2]
        for ch in range(8):
            cs = slice(ch * 64, (ch + 1) * 64)
            first = (ti == 0 and ch == 0)
            for hd in range(4):
                S.mm(S.pb(0, hd * 64, (hd + 1) * 64, F32, 0, 64), i4[:, 1, hd, cs], i4[:, 0, hd, cs], True, True,
                     [inT], [S.banks[0]])
            S.tt(ATs[:, :], S.pb(0, 0, 256, F32, 0, 64), C.hmask[:, :], ALU.mult, [S.banks[0], C.hmask], [ATs])
            for hd in range(4):
                S.tr(S.pb(1, hd * 64, (hd + 1) * 64, BF16, 0, 64), i4[:, 3, hd, cs], C.ident[0:64, 0:64],
                     [inT, C.ident], [S.banks[1]])
            S.cp(ksT[:, :], S.pb(1, 0, 256, BF16, 0, 64), [S.banks[1]], [ksT], eng='act')
            for hd in range(4):
                vs = vv[:, ch * 256 + hd * 64:ch * 256 + (hd + 1) * 64]
                S.mm(S.pb(3 + hd, ch * 64, (ch + 1) * 64, F32, 0, 64), vs, ATs[:, hd * 64:(hd + 1) * 64], True, first,
                     [vv, ATs], [S.banks[3 + hd]])
                if not first:
                    S.mm(S.pb(3 + hd, ch * 64, (ch + 1) * 64, F32, 0, 64), Sbf[:, hd * 64:(hd + 1) * 64], i4[:, 2, hd, cs],
                         False, True, [Sbf, inT], [S.banks[3 + hd]])
            for hd in range(4):
                vs = vv[:, ch * 256 + hd * 64:ch * 256 + (hd + 1) * 64]
                S.mm(S.pb(2, hd * 64, (hd + 1) * 64, F32, 0, 64), ksT[:, hd * 64:(hd + 1) * 64], vs, True, True,
                     [ksT, vv], [S.banks[2]])
            if first:
                S.cp(S32[:, :], S.pb(2, 0, 256, F32, 0, 64), [S.banks[2]], [S32])
            else:
                for hd in range(4):
                    s_ = S32[:, hd * 64:(hd + 1) * 64]
                    S.stt(s_, s_, dc[:, hd * 8 + ch:hd * 8 + ch + 1], S.pb(2, hd * 64, (hd + 1) * 64, F32, 0, 64),
                          ALU.mult, ALU.add, [S32, dc, S.banks[2]], [S32])
            S.cp(Sbf[:, :], S32[:, :], [S32], [Sbf], eng='act')
        for hd in range(4):
            ops = S.pb(3 + hd, 0, 512, F32, 0, 64)
            S.act(sq[:, :], ops, AF.Square, [S.banks[3 + hd]], [sq])
            S.mm(S.pb(7, 0, 512, F32, 0, 64), C.ones[0:64, 0:64], sq[:, :], True, True, [C.ones, sq], [S.banks[7]])
            S.act(sdt[:, :], S.pb(7, 0, 512, F32, 0, 64), AF.Sqrt, [S.banks[7], C.eps], [sdt], scale=1.0 / 64,
                  bias=C.eps[0:64, 0:1])
            S.recip(sdt[:, :], sdt[:, :], [sdt], [sdt])
            S.stt(t1[:, :], ops, hg[:, hd:hd + 1], sdt[:, :], ALU.mult, ALU.mult, [S.banks[3 + hd], hg, sdt], [t1])
            S.tt(ost[:, hd * 512:(hd + 1) * 512], t1[:, :], i4[:, 4, hd, :], ALU.mult, [t1, inT], [ost])
        S.dma(C.oT[512:768, t0:t0 + 512].rearrange("(h p) t -> p h t", p=64), ost[:, :].rearrange("p (h t) -> p h t", h=4),
              r=[ost], w=[C.oT])
    S.phase_end()


def wout_phase(S, C, l):
    S.phase_begin()
    wo = S.tile('wo', 8 * D, BF16)
    C.stage = [S.tile('st0', 512, F32), S.tile('st1', 512, F32)]
    C.stage_i = 0
    for c in range(8):
        load_cast(S, C, wo, c * D, C.din['w_out'][l, c * 128:(c + 1) * 128, :], 128, D, C.dinT)
    xts = [S.tile(f'xt{i}', 4 * D, F32) for i in range(2)]
    ots = [S.tile(f'ot{i}', 8 * 512, BF16) for i in range(2)]
    for ti in range(C.NTOK // 512):
        t0 = ti * 512
        xt = xts[ti % 2]
        ot = ots[ti % 2]
        S.dma(xt[:, :].rearrange("p (a d) -> p a d", a=4), C.xres[t0:t0 + 512, :].rearrange("(a p) d -> p a d", p=128),
              r=[C.xres], w=[xt])
        S.dma(ot[:, :].rearrange("p (c t) -> p c t", c=8), C.oT[:, t0:t0 + 512].rearrange("(c p) t -> p c t", p=128),
              r=[C.oT], w=[ot])
        for a in range(4):
            for hf in range(2):
                b = (a * 2 + hf) % 4
                for c in range(8):
                    S.mm(S.pb(b), ot[:, c * 512 + a * 128:c * 512 + (a + 1) * 128], wo[:, c * D + hf * 512:c * D + (hf + 1) * 512],
                         c == 0, c == 7, [ot, wo], [S.banks[b]])
                xs = xt[:, a * D + hf * 512:a * D + (hf + 1) * 512]
                S.tt(xs, S.pb(b), xs, ALU.add, [S.banks[b], xt], [xt])
        S.dma(C.xres[t0:t0 + 512, :].rearrange("(a p) d -> p a d", p=128), xt[:, :].rearrange("p (a d) -> p a d", a=4),
              r=[xt], w=[C.xres])
    S.phase_end()


def build(T_=4096, NSEQ=2, DEPTH=2, NBIS=26, stop_after=None):
    nc = bass.Bass("TRN2", target_bir_lowering=False)
    S = Sched(nc)
    C = Ctx()
    C.T, C.NSEQ, C.DEPTH, C.NBIS = T_, NSEQ, DEPTH, NBIS
    NTOK = C.NTOK = T_ * NSEQ
    C.dinT = T(None, 'din')
    din = {}

    def inp(name, shape, dt=F32):
        din[name] = nc.dram_tensor(name, list(shape), dt, kind="ExternalInput").ap()
    inp('x', [NTOK, D])
    for nm in ('ffn1', 'ffn2'):
        inp(nm + '_gate', [DEPTH, D, DFF])
        inp(nm + '_up', [DEPTH, D, DFF])
        inp(nm + '_down', [DEPTH, DFF, D])
        inp('g_' + nm, [DEPTH, 128, 8])
    inp('g_mix', [DEPTH, 128, 8])
    inp('w_in_x', [DEPTH, D, NWX])
    inp('g_ckv', [DEPTH, 128, 1])
    inp('w_kv_up', [DEPTH, 128, 512])
    inp('lb_logits', [64, DEPTH * 4])
    inp('g_hgrn', [DEPTH, 64, 4])
    inp('w_out', [DEPTH, D, D])
    inp('rel_bias', [32, 12])
    inp('norm_final_b', [128, D])
    inp('c_ident', [128, 128], BF16)
    inp('c_anti', [128, 128], BF16)
    inp('c_ones', [128, 128], BF16)
    inp('c_tri', [128, 128])
    inp('c_hmask', [64, 256])
    inp('c_cmask', [64, 512])
    inp('c_esel', [64, 16 * 128], BF16)
    inp('c_ohab', [33, LAB], BF16)
    inp('c_ohd', [33, LD], BF16)
    C.din = din
    y = S.dram('y', [NTOK, D], F32, kind="ExternalOutput")
    C.xres = S.dram('xres', [NTOK, D], F32)
    C.fm = S.dram('fm', [FMROWS, NTOK], BF16)
    C.v1s = S.dram('v1s', [3, NTOK, 260], BF16)
    C.hv = S.dram('hv', [NTOK, 256], BF16)
    C.hdec = S.dram('hdec', [256, NTOK // 64], F32)
    C.oT = S.dram('oT', [1024, NTOK], BF16, kind=('ExternalOutput' if stop_after is not None else 'Internal'))
    C.vecs = S.dram('vecs', [12, LD], BF16)
    xin = T(din['x'], 'xin', dram=True)

    def cload(name, key, cols, dt, parts=128):
        t = S.tile(name, cols, dt, parts=parts)
        S.dma(t[:, :], din[key], r=[C.dinT], w=[t])
        return t
    C.ident = cload('ident', 'c_ident', 128, BF16)
    C.antiI = cload('antiI', 'c_anti', 128, BF16)
    C.ones = cload('ones', 'c_ones', 128, BF16)
    C.tri = cload('tri', 'c_tri', 128, F32)
    C.hmask = cload('hmask', 'c_hmask', 256, F32, parts=64)
    C.cmask = cload('cmask', 'c_cmask', 512, F32, parts=64)
    C.eps = S.tile('eps', 1, F32)
    S.memset(C.eps[:, :], EPS, [C.eps])
    C.negM = S.tile('negM', 1, F32)
    S.memset(C.negM[:, :], 0.0, [C.negM])
    C.far = S.tile('far', 12, F32)
    S.dma(C.far[:, :], bass.AP(tensor=din['rel_bias'].tensor, offset=31 * 12, ap=[[0, 128], [1, 12]]), r=[C.dinT], w=[C.far])
    S.phase_begin()
    tabf = S.tile('tabf', 12, F32, parts=33)
    tabb = S.tile('tabb', 12, BF16, parts=33)
    S.memset(tabf[:, :], 1.0, [tabf])
    S.dma(tabf[0:32, :], din['rel_bias'], r=[C.dinT], w=[tabf])
    S.cp(tabb[:, :], tabf[:, :], [tabf], [tabb])
    ohab = cload('ohab', 'c_ohab', LAB, BF16, parts=33)
    ohd = cload('ohd', 'c_ohd', LD, BF16, parts=33)
    vst_ = S.tile('vecst', LD, BF16, parts=12)
    S.memset(vst_[:, :], 0.0, [vst_])
    for (oh, L, h0, h1) in ((ohab, LAB, 0, 8), (ohd, LD, 8, 12)):
        for c0 in range(0, L, 512):
            c1 = min(L, c0 + 512)
            S.mm(S.pb(0, 0, c1 - c0, F32, 0, 12), tabb[:, :], oh[:, c0:c1], True, True, [tabb, oh], [S.banks[0]])
            tmp = S.tile(f'vtmp{h0}_{c0}', 512, BF16, parts=12)
            S.cp(tmp[:, 0:c1 - c0], S.pb(0, 0, c1 - c0, F32, 0, 12), [S.banks[0]], [tmp])
            S.dma(C.vecs[h0:h1, c0:c1], tmp[h0:h1, 0:c1 - c0], r=[tmp], w=[C.vecs])
    S.phase_end()

    for l in range(DEPTH):
        if stop_after == ('const', l):
            break
        ffn_phase(S, C, l, 0, xin if l == 0 else C.xres, C.xres, False)
        if stop_after == ('ffn1', l):
            break
        proj_phase(S, C, l)
        if stop_after == ('proj', l):
            break
        for si in range(NSEQ):
            for m in (0, 1, 2):
                if m in DBG['mixers']:
                    mixer_attn_phase(S, C, l, si, m)
            if 3 in DBG['mixers']:
                hgrn_phase(S, C, l, si)
        wout_phase(S, C, l)
        if stop_after == ('wout', l):
            break
        last = (l == DEPTH - 1)
        ffn_phase(S, C, l, 1, C.xres, y if last else C.xres, last)
    if stop_after is not None:
        S.phase_begin()
        xt = S.tile('dbgx', 4 * D, F32)
        for ti in range(NTOK // 512):
            t0 = ti * 512
            S.dma(xt[:, :].rearrange("p (a d) -> p a d", a=4), C.xres[t0:t0 + 512, :].rearrange("(a p) d -> p a d", p=128),
                  r=[C.xres], w=[xt])
            S.dma(y[t0:t0 + 512, :].rearrange("(a p) d -> p a d", p=128), xt[:, :].rearrange("p (a d) -> p a d", a=4),
                  r=[xt], w=[y])
        S.phase_end()
    counts = S.finish()
    return nc, counts


def host_consts(DEPTH, inputs):
    bf = ml_dtypes.bfloat16
    f32 = np.float32
    c = {}
    c['c_ident'] = np.eye(128, dtype=f32).astype(bf)
    c['c_anti'] = np.eye(128, dtype=f32)[::-1].copy().astype(bf)
    c['c_ones'] = np.ones((128, 128), f32).astype(bf)
    t = np.arange(128)
    c['c_tri'] = np.where(t[None, :] <= t[:, None], 0.0, -1e30).astype(f32)
    s = np.arange(64)
    hm = (s[:, None] <= s[None, :]).astype(f32)
    c['c_hmask'] = np.ascontiguousarray(np.tile(hm, (1, 4)))
    cm = np.ones((64, 512), f32)
    cm[:, ::64] = 0.0
    c['c_cmask'] = cm
    es = np.zeros((64, 16, 128), f32)
    for hl in range(2):
        for n in range(16):
            es[hl * 32 + n, n, :] = 1.0
    c['c_esel'] = es.reshape(64, 16 * 128).astype(bf)
    a, d = onehots()
    c['c_ohab'] = a.astype(bf)
    c['c_ohd'] = d.astype(bf)
    cols = win_cols()
    c['w_in_x'] = np.ascontiguousarray(inputs['w_in'][:, :, cols])

    def gc(v):
        return np.ascontiguousarray(v.reshape(DEPTH, 8, 128).transpose(0, 2, 1))
    c['g_ffn1'] = gc(inputs['norm_ffn1'])
    c['g_ffn2'] = gc(inputs['norm_ffn2'])
    c['g_mix'] = gc(inputs['norm_mix'])
    c['g_ckv'] = np.ascontiguousarray(inputs['ckv_norm'].reshape(DEPTH, 128, 1))
    c['g_hgrn'] = np.ascontiguousarray(inputs['hgrn_norm'].reshape(DEPTH, 4, 64).transpose(0, 2, 1))
    c['lb_logits'] = np.ascontiguousarray(inputs['hgrn_lb_logits'].reshape(DEPTH, 4, 64).transpose(2, 0, 1).reshape(64, DEPTH * 4))
    c['norm_final_b'] = np.ascontiguousarray(np.broadcast_to(inputs['norm_final'][None, :], (128, D)))
    for k in ('ffn1_gate', 'ffn1_up', 'ffn1_down', 'ffn2_gate', 'ffn2_up', 'ffn2_down', 'w_kv_up', 'w_out', 'rel_bias'):
        c[k] = np.ascontiguousarray(inputs[k])
    return c


_CACHE = {}


def run(inputs, T_, NSEQ, DEPTH, ncores, NBIS=26, stop_after=None):
    inputs = {k: np.asarray(v) for k, v in inputs.items()}
    key = (T_, NSEQ, DEPTH, NBIS, stop_after)
    if key not in _CACHE:
        _CACHE[key] = build(T_, NSEQ, DEPTH, NBIS, stop_after)
    nc, counts = _CACHE[key]
    consts = host_consts(DEPTH, inputs)
    x = inputs['x'].astype(np.float32, copy=False)
    in_maps = []
    for ci in range(ncores):
        mp = dict(consts)
        mp['x'] = np.ascontiguousarray(x[ci * NSEQ:(ci + 1) * NSEQ].reshape(NSEQ * T_, D))
        in_maps.append(mp)
    import os, time as _t
    _t0 = _t.time()
    res = run_bass_kernel_spmd(nc, in_maps, core_ids=list(range(ncores)), trace=bool(os.environ.get('KTRACE')))
    if os.environ.get('KTRACE'):
        print('EXEC_NS', getattr(res, 'exec_time_ns', None), 'wall', _t.time() - _t0)
    out = np.concatenate([np.asarray(r['y']).reshape(NSEQ, T_, D) for r in res.results], axis=0)
    if stop_after is not None:
        DBG['oT'] = [np.asarray(r['oT']) for r in res.results]
    return out.astype(np.float32)


def kernel(**inputs):
    return run(inputs, 4096, 2, 2, 8)
```

```python
import math
import contextlib
import numpy as np
import ml_dtypes
import concourse.bass as bass
import concourse.mybir as mybir
from concourse.bass_utils import run_bass_kernel_spmd

F32 = mybir.dt.float32
BF16 = mybir.dt.bfloat16
AF = mybir.ActivationFunctionType
ALU = mybir.AluOpType
AX = mybir.AxisListType

ENG = ('pe', 'act', 'dve', 'pool', 'sp')
SEM_PERIOD = 20000
NSLOT = 8
NEG = -30000.0
D = 1024
DFF = 2816
NFC = DFF // 128
EPS = 1e-6
SBUF_BYTES = 206 * 1024

OFF = dict(a_q=0, a_ckv=256, a_iq=384, a_ik=640, a_iw=672, b_q=680, b_k=936, b_v=1192,
           c_q=1448, c_f=1704, c_i=1960, c_g=2216, d_q=2472, d_k=2728, d_v=2984)


def win_cols():
    cols = []
    for nm in ('a_q', 'b_q', 'd_q', 'b_k', 'd_k'):
        cols += list(range(OFF[nm], OFF[nm] + 256))
    cols += list(range(OFF['a_ckv'], OFF['a_ckv'] + 128))
    for g in range(3):
        for hp in range(4):
            h = min(3 * g + hp, 7) if hp < 3 else min(3 * g, 7)
            cols += list(range(OFF['a_iq'] + 32 * h, OFF['a_iq'] + 32 * h + 32))
    for g in range(3):
        for hp in range(4):
            h = min(3 * g + hp, 7) if hp < 3 else min(3 * g, 7)
            cols += [OFF['a_iw'] + h] * 32
    for _ in range(4):
        cols += list(range(OFF['a_ik'], OFF['a_ik'] + 32))
    for nm in ('c_q', 'c_f', 'c_g'):
        cols += list(range(OFF[nm], OFF[nm] + 256))
    cols += list(range(OFF['b_v'], OFF['b_v'] + 256))
    cols += list(range(OFF['d_v'], OFF['d_v'] + 256))
    cols += list(range(OFF['c_i'], OFF['c_i'] + 256))
    return np.array(cols, dtype=np.int64)


NWX = 18 * 128 + 768 + 512 + 256
HG0 = 2304
TMV = 2304 + 768
TMI = TMV + 512

R_Q = {0: 0, 1: 256, 2: 512}
R_K = {1: 768, 2: 1024, 0: 1280}
R_IQP, R_IQN, R_IK = 1536, 1920, 2304
R_H = 2432
FMROWS = R_H + 5 * 256

UAB = 2048
UD = 2560
LAB = UAB + 128
LD = UD + 128
NEAR = 1664


def t5_bucket_np(d):
    n = np.maximum(d, 0)
    nf = np.maximum(n, 1).astype(np.float32)
    large = 16 + (np.log(nf / np.float32(16)) / np.float32(math.log(2048 / 16)) * np.float32(16)).astype(np.int32)
    large = np.minimum(large, 31)
    return np.where(n < 16, n, large)


def onehots():
    def mk(L, dil):
        x = np.arange(L)
        d = x - 127
        oh = np.zeros((33, L), np.float32)
        b = t5_bucket_np(d)
        if not dil:
            valid = d >= 0
            const = np.where(valid, 0.0, NEG)
        else:
            mult = ((d >= 0) & (d <= 128)).astype(np.int32) + ((d >= 0) & (d <= 512) & (d % 4 == 0)) \
                + ((d >= 0) & (d <= 2048) & (d % 16 == 0))
            valid = mult > 0
            const = np.where(valid, np.log(np.maximum(mult, 1)), NEG)
        oh[b[valid], x[valid]] = 1.0
        oh[32] = const
        return oh
    return mk(LAB, False), mk(LD, True)


class T:
    __slots__ = ('h', 'name', 'w', 'r', 'dram')

    def __init__(self, h, name, dram=False):
        self.h = h
        self.name = name
        self.w = None
        self.r = {}
        self.dram = dram

    def __getitem__(self, idx):
        return self.h[idx]


class Sched:
    def __init__(self, nc):
        self.nc = nc
        self.stack = contextlib.ExitStack()
        self.streams = {e: [] for e in ENG}
        self.known = {e: {} for e in ENG}
        self.marked = {e: set() for e in ENG}
        self.ndma = {e: 0 for e in ENG}
        self.selfsync = True
        self.arena = self.stack.enter_context(nc.sbuf_tensor("arena", [128, SBUF_BYTES // 4], F32))
        self.psum = self.stack.enter_context(nc.psum_tensor("psum", [128, 4096], F32))
        self.banks = [T(None, f"bank{i}") for i in range(8)]
        self.top = 0
        self.marks = []
        self.live = []
        self.dmaq = 0

    def tile(self, name, cols, dt, parts=128):
        nbytes = cols * (2 if dt == BF16 else 4)
        nbytes = (nbytes + 63) // 64 * 64
        assert self.top + nbytes <= SBUF_BYTES, (name, self.top, nbytes)
        a = self.arena[0:parts, self.top // 4:(self.top + nbytes) // 4]
        if dt == BF16:
            a = a.bitcast(BF16)
        a = a[:, 0:cols]
        self.top += nbytes
        t = T(a, name)
        self.live.append(t)
        return t

    def phase_begin(self):
        self.marks.append((self.top, len(self.live)))

    def phase_end(self):
        self.barrier()
        self.top, n = self.marks.pop()
        del self.live[n:]

    def pb(self, b, c0=0, c1=512, dt=F32, p0=0, p1=128):
        a = self.psum[p0:p1, b * 512:(b + 1) * 512]
        if dt == BF16:
            a = a.bitcast(BF16)
        return a[:, c0:c1]

    def pspan(self, b0, ncols, p0=0, p1=128):
        return self.psum[p0:p1, b0 * 512:b0 * 512 + ncols]

    def dram(self, name, shape, dt, kind="Internal"):
        h = self.nc.dram_tensor(name, list(shape), dt, kind=kind)
        return T(h.ap(), name, dram=True)

    def _need(self, eng, ev, waits, kind):
        if ev is None:
            return
        if ev[0] == 'c':
            if ev[1] == eng:
                if eng == 'pe' or kind != 'raw' or not self.selfsync:
                    return
            key = ('c', ev[1])
            val = ev[2]
        else:
            key = ('d', ev[1], ev[2])
            val = ev[3]
        if self.known[eng].get(key, -1) >= val:
            return
        self.known[eng][key] = val
        waits.append(ev)
        if ev[0] == 'c':
            self.marked[ev[1]].add(ev[2])

    def _deps(self, eng, r, w):
        waits = []
        for t in r:
            self._need(eng, t.w, waits, 'raw')
        for t in w:
            self._need(eng, t.w, waits, 'waw')
            for ev in t.r.values():
                self._need(eng, ev, waits, 'war')
        return waits

    def op(self, eng, fn, r=(), w=()):
        waits = self._deps(eng, r, w)
        idx = len(self.streams[eng])
        ev = ('c', eng, idx)
        self.streams[eng].append((waits, fn, None))
        for t in r:
            t.r[eng] = ev
        for t in w:
            t.w = ev
            t.r = {}
        return ev

    def dma(self, out_ap, in_ap, r=(), w=(), q=None, **kw):
        if q is None:
            q = 'sp'
        waits = self._deps(q, r, w)
        k = self.ndma[q]
        self.ndma[q] += 1
        slot = k % NSLOT
        cnt = k // NSLOT + 1
        if cnt > 1:
            self._need(q, ('d', q, slot, cnt - 1), waits, 'raw')
        ev = ('d', q, slot, cnt)
        self.streams[q].append(
            (waits, lambda e: e.dma_start(out=out_ap, in_=in_ap, **kw), (q, slot)))
        for t in r:
            t.r[('d', q, slot)] = ev
        for t in w:
            t.w = ev
            t.r = {}
        return ev

    def _all_events(self):
        evs = []
        for q in ENG:
            n = self.ndma[q]
            for slot in range(min(n, NSLOT)):
                evs.append(('d', q, slot, (n - 1 - slot) // NSLOT + 1))
        for e in ENG:
            n = len(self.streams[e])
            for i in range(n - 1, -1, -1):
                if self.streams[e][i][2] is None and self.streams[e][i][1] is not None:
                    evs.append(('c', e, i))
                    break
        return evs

    def barrier(self):
        evs = self._all_events()
        for e in ENG:
            waits = []
            for ev in evs:
                if ev[0] == 'c' and ev[1] == e:
                    continue
                self._need(e, ev, waits, 'raw')
            if waits:
                self.streams[e].append((waits, None, None))

    def finish(self):
        nc = self.nc
        self.barrier()
        self.streams['sp'].append(([], lambda e: e.nop(), None))
        rank = {}
        for e in ENG:
            ms = sorted(self.marked[e])
            rank[e] = {idx: i + 1 for i, idx in enumerate(ms)}
        csem = {e: [self.stack.enter_context(nc.semaphore(f"c_{e}_{i}"))
                    for i in range((len(rank[e]) + SEM_PERIOD - 1) // SEM_PERIOD)] for e in ENG}
        dsem = {}
        for q in ENG:
            for slot in range(min(self.ndma[q], NSLOT)):
                dsem[(q, slot)] = self.stack.enter_context(nc.semaphore(f"d_{q}_{slot}"))

        def ev2sem(ev):
            if ev[0] == 'c':
                c = rank[ev[1]][ev[2]]
                return csem[ev[1]][(c - 1) // SEM_PERIOD], (c - 1) % SEM_PERIOD + 1
            return dsem[(ev[1], ev[2])], 16 * ev[3]

        def replay(ename, eng):
            for idx, (waits, fn, dinfo) in enumerate(self.streams[ename]):
                for ev in waits:
                    s, v = ev2sem(ev)
                    eng.wait_ge(s, v)
                if fn is None:
                    continue
                inst = fn(eng)
                if dinfo is not None:
                    inst.then_inc(dsem[dinfo], 16)
                elif idx in rank[ename]:
                    c = rank[ename][idx]
                    inst.then_inc(csem[ename][(c - 1) // SEM_PERIOD], 1)

        allsems = [h for e in ENG for h in csem[e]] + list(dsem.values())
        for h in allsems:
            nc.gpsimd.sem_clear(h)
        nc.all_engine_barrier()
        with nc.Block() as block:
            @block.tensor
            def _(e):
                replay('pe', e)

            @block.scalar
            def _(e):
                replay('act', e)

            @block.vector
            def _(e):
                replay('dve', e)

            @block.gpsimd
            def _(e):
                replay('pool', e)

            @block.sync
            def _(e):
                replay('sp', e)
        nc.all_engine_barrier()
        for h in allsems:
            nc.gpsimd.sem_clear(h)
        nc.all_engine_barrier()
        self.stack.close()
        return {e: len(self.streams[e]) for e in ENG}

    def mm(self, out, lhsT, rhs, start, stop, r, w):
        return self.op('pe', lambda e: e.matmul(out, lhsT=lhsT, rhs=rhs, start=start, stop=stop,
                                                skip_group_check=True), r, w)

    def tr(self, out, in_, ident, r, w):
        return self.op('pe', lambda e: e.transpose(out, in_, ident), r, w)

    def act(self, out, in_, func, r, w, **kw):
        return self.op('act', lambda e: e.activation(out, in_, func, **kw), r, w)

    def ts(self, out, in0, s1, s2, op0, op1, r, w, eng='dve', accum_out=None):
        if op1 is None:
            return self.op(eng, lambda e: e.tensor_scalar(out, in0, s1, None, op0=op0), r, w)
        if accum_out is not None:
            return self.op(eng, lambda e: e.tensor_scalar(out, in0, s1, s2, op0=op0, op1=op1,
                                                          accum_out=accum_out), r, w)
        return self.op(eng, lambda e: e.tensor_scalar(out, in0, s1, s2, op0=op0, op1=op1), r, w)

    def stt(self, out, in0, sc, in1, op0, op1, r, w):
        return self.op('dve', lambda e: e.scalar_tensor_tensor(out, in0, sc, in1, op0=op0, op1=op1), r, w)

    def tt(self, out, in0, in1, op, r, w, eng='dve'):
        return self.op(eng, lambda e: e.tensor_tensor(out, in0, in1, op=op), r, w)

    def cp(self, out, in_, r, w, eng='dve'):
        if eng == 'act':
            return self.op('act', lambda e: e.copy(out, in_), r, w)
        return self.op(eng, lambda e: e.tensor_copy(out, in_), r, w)

    def recip(self, out, in_, r, w):
        return self.op('dve', lambda e: e.reciprocal(out, in_), r, w)

    def memset(self, ap, val, w, eng='dve'):
        return self.op(eng, lambda e: e.memset(ap, val), (), w)


class Ctx:
    pass


DBG = {'ffn_steps': 9, 'const': True, 'mixers': (0, 1, 2, 3), 'gate': True, 'bmask': True}


def load_cast(S, C, dst, dst_c0, src_ap, nrows, ncols, srcT, gcol=None):
    c = 0
    while c < ncols:
        n = min(512, ncols - c)
        st = C.stage[C.stage_i % 2]
        C.stage_i += 1
        S.dma(st[0:nrows, 0:n], src_ap[:, c:c + n], r=[srcT], w=[st])
        if gcol is None:
            S.ts(dst[0:nrows, dst_c0 + c:dst_c0 + c + n], st[0:nrows, 0:n], 1.0, 1.0, ALU.mult, ALU.mult,
                 [st], [dst], eng='pool')
        else:
            S.ts(dst[0:nrows, dst_c0 + c:dst_c0 + c + n], st[0:nrows, 0:n], gcol, 1.0, ALU.mult, ALU.mult,
                 [st, C.gcolT], [dst], eng='pool')
        c += n


def norm_front(S, C, xt, hbs, hT, junk, ssq, sd, rstd, ntr_bank=(0, 1)):
    for a in range(4):
        S.act(junk[:, :], xt[:, a * D:(a + 1) * D], AF.Square, [xt], [junk, ssq], accum_out=ssq[:, a:a + 1])
    S.act(sd[:, 0:4], ssq[:, 0:4], AF.Sqrt, [ssq, C.eps], [sd], scale=1.0 / D, bias=C.eps[:, 0:1])
    S.recip(rstd[:, 0:4], sd[:, 0:4], [sd], [rstd])
    hT3 = hT[:, :].rearrange("p (c t) -> p c t", c=8)
    for a in range(4):
        hb = hbs[a % 2]
        b = ntr_bank[a % 2]
        S.act(hb[:, :], xt[:, a * D:(a + 1) * D], AF.Copy, [xt, rstd], [hb], scale=rstd[:, a:a + 1])
        for c in range(8):
            S.tr(S.pb(b, c * 128, (c + 1) * 128, BF16), hb[:, c * 128:(c + 1) * 128], C.ident[:, :],
                 [hb, C.ident], [S.banks[b]])
        S.cp(hT3[:, :, a * 128:(a + 1) * 128], S.pb(b, 0, 1024, BF16).rearrange("p (c t) -> p c t", c=8),
             [S.banks[b]], [hT], eng=('act' if a % 2 else 'dve'))


def ffn_phase(S, C, l, which, src, dst, final):
    nm = 'ffn1' if which == 0 else 'ffn2'
    S.phase_begin()
    wg = S.tile('wg', 8 * DFF, BF16)
    wu = S.tile('wu', 8 * DFF, BF16)
    wd = S.tile('wd', NFC * D, BF16)
    C.stage = [S.tile('st0', 512, F32), S.tile('st1', 512, F32)]
    C.stage_i = 0
    gcol = S.tile('gcol', 8, F32)
    C.gcolT = gcol
    S.dma(gcol[:, :], C.din['g_' + nm][l], r=[C.dinT], w=[gcol])
    for c in range(8):
        load_cast(S, C, wg, c * DFF, C.din[nm + '_gate'][l, c * 128:(c + 1) * 128, :], 128, DFF, C.dinT, gcol[:, c:c + 1])
        load_cast(S, C, wu, c * DFF, C.din[nm + '_up'][l, c * 128:(c + 1) * 128, :], 128, DFF, C.dinT, gcol[:, c:c + 1])
    for f in range(NFC):
        load_cast(S, C, wd, f * D, C.din[nm + '_down'][l, f * 128:(f + 1) * 128, :], 128, D, C.dinT)
    xt = S.tile('xt', 4 * D, F32)
    junk = S.tile('junk', D, BF16)
    hb = [S.tile('hb0', D, BF16), S.tile('hb1', D, BF16)]
    hT = S.tile('hT', 8 * 512, BF16)
    aT = S.tile('aT', NFC * 512, BF16)
    sg = [S.tile('sg0', 512, BF16), S.tile('sg1', 512, BF16)]
    ssq = S.tile('ssq', 4, F32)
    sd = S.tile('sd', 4, F32)
    rstd = S.tile('rstd', 4, F32)
    if final:
        gfin = S.tile('gfin', D, F32)
        S.dma(gfin[:, :], C.din['norm_final_b'], r=[C.dinT], w=[gfin])
    for ti in range(C.NTOK // 512):
        t0 = ti * 512
        S.dma(xt[:, :].rearrange("p (a d) -> p a d", a=4), src[t0:t0 + 512, :].rearrange("(a p) d -> p a d", p=128),
              r=[src], w=[xt])
        if DBG['ffn_steps'] >= 1:
            norm_front(S, C, xt, hb, hT, junk, ssq, sd, rstd)
        for f in range(NFC if DBG['ffn_steps'] >= 2 else 0):
            bg = 2 + f % 2
            bu = 4 + f % 2
            for c in range(8):
                S.mm(S.pb(bg), wg[:, c * DFF + f * 128:c * DFF + (f + 1) * 128], hT[:, c * 512:(c + 1) * 512],
                     c == 0, c == 7, [wg, hT], [S.banks[bg]])
            for c in range(8):
                S.mm(S.pb(bu), wu[:, c * DFF + f * 128:c * DFF + (f + 1) * 128], hT[:, c * 512:(c + 1) * 512],
                     c == 0, c == 7, [wu, hT], [S.banks[bu]])
            s_ = sg[f % 2]
            S.act(s_[:, :], S.pb(bg), AF.Silu, [S.banks[bg]], [s_])
            S.tt(aT[:, f * 512:(f + 1) * 512], s_[:, :], S.pb(bu), ALU.mult, [s_, S.banks[bu]], [aT])
        for a in range(4 if DBG['ffn_steps'] >= 3 else 0):
            for hf in range(2):
                by = 6 + (a * 2 + hf) % 2
                for f in range(NFC):
                    S.mm(S.pb(by), aT[:, f * 512 + a * 128:f * 512 + (a + 1) * 128],
                         wd[:, f * D + hf * 512:f * D + (hf + 1) * 512], f == 0, f == NFC - 1, [aT, wd], [S.banks[by]])
                xs = xt[:, a * D + hf * 512:a * D + (hf + 1) * 512]
                S.stt(xs, S.pb(by), 0.5, xs, ALU.mult, ALU.add, [S.banks[by], xt], [xt])
        if final:
            for a in range(4):
                S.act(junk[:, :], xt[:, a * D:(a + 1) * D], AF.Square, [xt], [junk, ssq], accum_out=ssq[:, a:a + 1])
            S.act(sd[:, 0:4], ssq[:, 0:4], AF.Sqrt, [ssq, C.eps], [sd], scale=1.0 / D, bias=C.eps[:, 0:1])
            S.recip(rstd[:, 0:4], sd[:, 0:4], [sd], [rstd])
            for a in range(4):
                xs = xt[:, a * D:(a + 1) * D]
                S.stt(xs, xs, rstd[:, a:a + 1], gfin[:, :], ALU.mult, ALU.mult, [xt, rstd, gfin], [xt])
        S.dma(dst[t0:t0 + 512, :].rearrange("(a p) d -> p a d", p=128), xt[:, :].rearrange("p (a d) -> p a d", a=4),
              r=[xt], w=[dst])
    S.phase_end()


def proj_phase(S, C, l):
    S.phase_begin()
    NTOK = C.NTOK
    wi = S.tile('wi', 8 * NWX, BF16)
    wkv = S.tile('wkv', 512, BF16)
    C.stage = [S.tile('st0', 512, F32), S.tile('st1', 512, F32)]
    C.stage_i = 0
    gcol = S.tile('gcol', 8, F32)
    C.gcolT = gcol
    S.dma(gcol[:, :], C.din['g_mix'][l], r=[C.dinT], w=[gcol])
    for c in range(8):
        load_cast(S, C, wi, c * NWX, C.din['w_in_x'][l, c * 128:(c + 1) * 128, :], 128, NWX, C.dinT, gcol[:, c:c + 1])
    load_cast(S, C, wkv, 0, C.din['w_kv_up'][l], 128, 512, C.dinT)
    gck = S.tile('gck', 1, F32)
    S.dma(gck[:, :], C.din['g_ckv'][l], r=[C.dinT], w=[gck])
    lbl = S.tile('lbl', C.DEPTH * 4, F32, parts=64)
    S.dma(lbl[:, :], C.din['lb_logits'], r=[C.dinT], w=[lbl])
    lbe = S.tile('lbe', C.DEPTH * 4, F32, parts=64)
    for i in range(C.DEPTH):
        S.tt(lbe[:, i * 4:(i + 1) * 4], lbl[:, i * 4:(i + 1) * 4], lbl[:, 0:4], ALU.subtract, [lbl], [lbe])
    S.act(lbe[:, :], lbe[:, :], AF.Exp, [lbe], [lbe])
    tot = S.tile('lbtot', 4, F32, parts=64)
    num = S.tile('lbnum', 4, F32, parts=64)
    S.cp(tot[:, :], lbe[:, 0:4], [lbe], [tot])
    S.memset(num[:, :], 0.0, [num])
    for i in range(1, C.DEPTH):
        S.tt(tot[:, :], tot[:, :], lbe[:, i * 4:(i + 1) * 4], ALU.add, [tot, lbe], [tot])
        if i <= l:
            S.tt(num[:, :], num[:, :], lbe[:, i * 4:(i + 1) * 4], ALU.add, [num, lbe], [num])
    lb = S.tile('lb', 4, F32, parts=64)
    oml = S.tile('oml', 4, F32, parts=64)
    S.recip(tot[:, :], tot[:, :], [tot], [tot])
    S.tt(lb[:, :], num[:, :], tot[:, :], ALU.mult, [num, tot], [lb])
    S.ts(oml[:, :], lb[:, :], -1.0, 1.0, ALU.mult, ALU.add, [lb], [oml])

    xt = S.tile('xt', 4 * D, F32)
    junk = S.tile('junk', D, BF16)
    hb = [S.tile('hb0', D, BF16), S.tile('hb1', D, BF16)]
    hT = S.tile('hT', 8 * 512, BF16)
    ssq = S.tile('ssq', 4, F32)
    sd = S.tile('sd', 4, F32)
    rstd = S.tile('rstd', 4, F32)
    qk = S.tile('qk', 10 * 512, BF16)
    cf = S.tile('cf', 512, F32)
    csq = S.tile('csq', 512, BF16)
    crs = S.tile('crs', 512, F32)
    cTb = S.tile('cTb', 512, BF16)
    kast = S.tile('kast', 2 * 512, BF16)
    iqs = S.tile('iqs', 7 * 512, BF16)
    wP = S.tile('wP', 512, F32)
    wN = S.tile('wN', 512, F32)
    vst = S.tile('vst', 4 * 3 * 260, BF16)
    hvst = S.tile('hvst', 8 * 256, BF16, parts=64)
    hst = S.tile('hst', 5 * 4 * 512, BF16, parts=64)
    dst_ = S.tile('dst', 32, F32, parts=64)
    hf_ = [S.tile(f'hf{i}', 512, F32, parts=64) for i in range(8)]
    S.memset(vst[:, :], 1.0, [vst])
    vst4 = vst[:, :].rearrange("p (a m h c) -> p a m h c", a=4, m=3, h=4)
    fm = C.fm
    for ti in range(NTOK // 512):
        t0 = ti * 512
        S.dma(xt[:, :].rearrange("p (a d) -> p a d", a=4), C.xres[t0:t0 + 512, :].rearrange("(a p) d -> p a d", p=128),
              r=[C.xres], w=[xt])
        norm_front(S, C, xt, hb, hT, junk, ssq, sd, rstd)

        def fmproj(bank, col0, M):
            for c in range(8):
                S.mm(S.pb(bank, 0, 512, F32, 0, M), wi[:, c * NWX + col0:c * NWX + col0 + M], hT[:, c * 512:(c + 1) * 512],
                     c == 0, c == 7, [wi, hT], [S.banks[bank]])
        for g in range(10):
            b = 2 + g % 2
            fmproj(b, g * 128, 128)
            if g < 6:
                S.act(qk[:, g * 512:(g + 1) * 512], S.pb(b), AF.Copy, [S.banks[b]], [qk], scale=0.125)
            else:
                S.cp(qk[:, g * 512:(g + 1) * 512], S.pb(b), [S.banks[b]], [qk])
        S.dma(fm[0:1280, t0:t0 + 512].rearrange("(g p) t -> p g t", p=128), qk[:, :].rearrange("p (g t) -> p g t", g=10),
              r=[qk], w=[fm])
        fmproj(4, 10 * 128, 128)
        S.cp(cf[:, :], S.pb(4), [S.banks[4]], [cf], eng='act')
        S.act(csq[:, :], S.pb(4), AF.Square, [S.banks[4]], [csq])
        S.mm(S.pb(5), C.ones[:, :], csq[:, :], True, True, [C.ones, csq], [S.banks[5]])
        S.act(crs[:, :], S.pb(5), AF.Sqrt, [S.banks[5], C.eps], [crs], scale=1.0 / 128, bias=C.eps[:, 0:1])
        S.recip(crs[:, :], crs[:, :], [crs], [crs])
        S.stt(cTb[:, :], cf[:, :], gck[:, 0:1], crs[:, :], ALU.mult, ALU.mult, [cf, gck, crs], [cTb])
        for pr in range(2):
            S.mm(S.pb(6 + pr), wkv[:, pr * 128:(pr + 1) * 128], cTb[:, :], True, True, [wkv, cTb], [S.banks[6 + pr]])
            S.act(kast[:, pr * 512:(pr + 1) * 512], S.pb(6 + pr), AF.Copy, [S.banks[6 + pr]], [kast])
        S.dma(fm[R_K[0]:R_K[0] + 256, t0:t0 + 512].rearrange("(g p) t -> p g t", p=128),
              kast[:, :].rearrange("p (g t) -> p g t", g=2), r=[kast], w=[fm])
        for a in range(4):
            b = 2 + a % 2
            S.mm(S.pb(b, 0, 256), cTb[:, a * 128:(a + 1) * 128], wkv[:, 256:512], True, True, [cTb, wkv], [S.banks[b]])
            S.cp(vst4[:, a, 0, :, 0:64], S.pb(b, 0, 256).rearrange("p (h c) -> p h c", h=4), [S.banks[b]], [vst])
        for g in range(3):
            fmproj(4, (11 + g) * 128, 128)
            fmproj(5, (14 + g) * 128, 128)
            S.act(wP[:, :], S.pb(5), AF.Relu, [S.banks[5]], [wP])
            S.act(wN[:, :], S.pb(5), AF.Relu, [S.banks[5]], [wN], scale=-1.0)
            S.tt(iqs[:, g * 512:(g + 1) * 512], S.pb(4), wP[:, :], ALU.mult, [S.banks[4], wP], [iqs])
            S.stt(iqs[:, (3 + g) * 512:(4 + g) * 512], S.pb(4), -1.0, wN[:, :], ALU.mult, ALU.mult, [S.banks[4], wN], [iqs])
        fmproj(6, 17 * 128, 128)
        S.cp(iqs[:, 6 * 512:7 * 512], S.pb(6), [S.banks[6]], [iqs], eng='act')
        S.dma(fm[R_IQP:R_IQP + 896, t0:t0 + 512].rearrange("(g p) t -> p g t", p=128),
              iqs[:, :].rearrange("p (g t) -> p g t", g=7), r=[iqs], w=[fm])
        for a in range(4):
            b = 2 + a % 2
            for c in range(8):
                S.mm(S.pb(b), hT[:, c * 512 + a * 128:c * 512 + (a + 1) * 128], wi[:, c * NWX + TMV:c * NWX + TMV + 512],
                     c == 0, c == 7, [hT, wi], [S.banks[b]])
            S.cp(vst4[:, a, 1:3, :, 0:64], S.pb(b).rearrange("p (m h c) -> p m h c", m=2, h=4), [S.banks[b]], [vst],
                 eng=('act' if a % 2 else 'dve'))
        for m_ in range(3):
            S.dma(C.v1s[m_, t0:t0 + 512, :].rearrange("(a p) c -> p a c", p=128),
                  vst[:, :].rearrange("p (a m c) -> p a m c", a=4, m=3)[:, :, m_, :], r=[vst], w=[C.v1s])
        for ch in range(8):
            b = 4 + (ch // 2) % 2
            o = (ch % 2) * 256
            for c in range(8):
                S.mm(S.pb(b, o, o + 256, F32, 0, 64), hT[:, c * 512 + ch * 64:c * 512 + (ch + 1) * 64],
                     wi[:, c * NWX + TMI:c * NWX + TMI + 256], c == 0, c == 7, [hT, wi], [S.banks[b]])
            S.cp(hvst[:, ch * 256:(ch + 1) * 256], S.pb(b, o, o + 256, F32, 0, 64), [S.banks[b]], [hvst],
                 eng=('act' if ch % 2 else 'dve'))
        S.dma(C.hv[t0:t0 + 512, :].rearrange("(c p) v -> p c v", p=64), hvst[:, :].rearrange("p (c v) -> p c v", c=8),
              r=[hvst], w=[C.hv])
        hst4 = hst[:, :].rearrange("p (k h t) -> p k h t", k=5, h=4)
        for hd in range(4):
            fmproj(6, HG0 + hd * 64, 64)
            fmproj(7, HG0 + 256 + hd * 64, 64)
            fmproj(2, HG0 + 512 + hd * 64, 64)
            q_ps = S.pb(6, 0, 512, F32, 0, 64)
            sig, f_, lf, kin, b_, d1, d4, e1 = hf_
            S.act(sig[:, :], S.pb(7, 0, 512, F32, 0, 64), AF.Sigmoid, [S.banks[7]], [sig])
            S.ts(f_[:, :], sig[:, :], oml[:, hd:hd + 1], lb[:, hd:hd + 1], ALU.mult, ALU.add, [sig, oml, lb], [f_])
            S.act(lf[:, :], f_[:, :], AF.Ln, [f_], [lf])
            S.ts(kin[:, :], f_[:, :], -1.0, 1.0, ALU.mult, ALU.add, [f_], [kin])
            S.op('dve', lambda e, b_=b_, lf=lf: e.tensor_tensor_scan(b_[:, :], C.cmask[:, :], lf[:, :], 0.0,
                                                                        op0=ALU.mult, op1=ALU.add), [C.cmask, lf], [b_])
            b3 = b_[:, :].rearrange("p (c j) -> p c j", j=64)
            S.tt(d1[:, :].rearrange("p (c j) -> p c j", j=64), b3, b3[:, :, 31:32].broadcast_to([64, 8, 64]),
                 ALU.subtract, [b_], [d1])
            S.tt(d4[:, :].rearrange("p (c j) -> p c j", j=64), b3[:, :, 63:64].broadcast_to([64, 8, 64]), b3,
                 ALU.subtract, [b_], [d4])
            S.act(e1[:, :], d1[:, :], AF.Exp, [d1], [e1])
            S.tt(hst4[:, 0, hd, :], q_ps, e1[:, :], ALU.mult, [S.banks[6], e1], [hst])
            S.act(e1[:, :], d1[:, :], AF.Exp, [d1], [e1], scale=-1.0)
            S.tt(hst4[:, 1, hd, :], kin[:, :], e1[:, :], ALU.mult, [kin, e1], [hst])
            S.act(e1[:, :], b_[:, :], AF.Exp, [b_], [e1])
            S.tt(hst4[:, 2, hd, :], q_ps, e1[:, :], ALU.mult, [S.banks[6], e1], [hst])
            S.cp(dst_[:, hd * 8:(hd + 1) * 8], e1[:, :].rearrange("p (c j) -> p c j", j=64)[:, :, 63], [e1], [dst_])
            S.act(e1[:, :], d4[:, :], AF.Exp, [d4], [e1])
            S.tt(hst4[:, 3, hd, :], kin[:, :], e1[:, :], ALU.mult, [kin, e1], [hst])
            S.act(hst4[:, 4, hd, :], S.pb(2, 0, 512, F32, 0, 64), AF.Silu, [S.banks[2]], [hst])
        for k_ in range(5):
            S.dma(fm[R_H + k_ * 256:R_H + (k_ + 1) * 256, t0:t0 + 512].rearrange("(h p) t -> p h t", p=64), hst4[:, k_],
                  r=[hst], w=[fm])
        S.dma(C.hdec[:, t0 // 64:t0 // 64 + 8].rearrange("(h p) c -> p h c", p=64),
              dst_[:, :].rearrange("p (h c) -> p h c", h=4), r=[dst_], w=[C.hdec])
    S.phase_end()


def attn_group(S, C, m, G, s0, qT, kT, v1, HK, extra, pts, ost, st):
    T_ = C.T
    nkt = 4 * G + 4
    jmin = max(0, 4 * G - 16) if m == 2 else 0
    for hl in range(4):
        pr, hf = divmod(hl, 2)
        p0 = 64 * hf
        head = m * 4 + hl
        bo = 3 + st['oi'] % 2
        st['oi'] += 1
        for j in range(jmin, nkt):
            c0 = max(0, j - 4 * G) * 128
            N = 512 - c0
            dist0 = (4 * G * 128 + c0) - j * 128
            near = (m == 2) or dist0 < NEAR
            bs = st['si'] % 3
            st['si'] += 1
            nmm = 1 + (1 if near else 0)
            if m == 0:
                nmm += sum(1 for qt in range(4) if 4 * G + qt >= j)
            if m == 1 and DBG['bmask']:
                nmm += 1
            k = 0
            S.mm(S.pb(bs, c0, 512), kT[p0:p0 + 64, pr * T_ + j * 128:pr * T_ + (j + 1) * 128],
                 qT[p0:p0 + 64, pr * T_ + G * 512 + c0:pr * T_ + (G + 1) * 512], True, nmm == 1, [kT, qT], [S.banks[bs]])
            k += 1
            if near:
                S.mm(S.pb(bs, c0, 512), C.antiI[:, :], HK[hl][:, dist0:dist0 + N], False, k == nmm - 1,
                     [C.antiI, HK[hl]], [S.banks[bs]])
                k += 1
            if m == 0:
                for qt in range(4):
                    if 4 * G + qt >= j:
                        mk = extra[qt]
                        S.mm(S.pb(bs, qt * 128, (qt + 1) * 128), mk[:, j * 128:(j + 1) * 128], C.ident[:, :], False,
                             k == nmm - 1, [mk, C.ident], [S.banks[bs]])
                        k += 1
            if m == 1 and DBG['bmask']:
                ex = extra[hl // 2]
                hq = 32 * (hl % 2)
                S.mm(S.pb(bs, c0, 512), C.esel[hq:hq + 32, (j // 2) * 128:(j // 2 + 1) * 128],
                     ex[hq:hq + 32, c0:512], False, True, [C.esel, ex], [S.banks[bs]])
            pt = pts[st['pi'] % 3]
            st['pi'] += 1
            bcol = C.negM[:, 0:1] if near else C.far[:, head:head + 1]
            S.act(pt[:, c0:512], S.pb(bs, c0, 512), AF.Exp, [S.banks[bs], C.far, C.negM], [pt], bias=bcol)
            S.mm(S.pb(bo, c0, 512, F32, 0, 65), v1[:, j * 260 + hl * 65:j * 260 + hl * 65 + 65], pt[:, c0:512],
                 j == jmin, j == nkt - 1, [v1, pt], [S.banks[bo]])
        den, dhi, dlo, rd = st['den'], st['dhi'], st['dlo'], st['rd']
        S.cp(den[64:65, :], S.pb(bo, 0, 512, F32, 64, 65), [S.banks[bo]], [den], eng='act')
        S.cp(dhi[64:65, :], den[64:65, :], [den], [dhi])
        S.tt(dlo[64:65, :], den[64:65, :], dhi[64:65, :], ALU.subtract, [den, dhi], [dlo])
        S.mm(S.pb(6, 0, 512, F32, 0, 64), C.ones[64:65, 0:64], dhi[64:65, :], True, False, [C.ones, dhi], [S.banks[6]])
        S.mm(S.pb(6, 0, 512, F32, 0, 64), C.ones[64:65, 0:64], dlo[64:65, :], False, True, [C.ones, dlo], [S.banks[6]])
        S.recip(rd[0:64, :], S.pb(6, 0, 512, F32, 0, 64), [S.banks[6]], [rd])
        S.tt(ost[0:64, hl * 512:(hl + 1) * 512], S.pb(bo, 0, 512, F32, 0, 64), rd[0:64, :], ALU.mult,
             [S.banks[bo], rd], [ost])
    S.dma(C.oT[m_rows(m):m_rows(m) + 256, s0 + G * 512:s0 + (G + 1) * 512].rearrange("(h p) t -> p h t", p=64),
          ost[0:64, :].rearrange("p (h t) -> p h t", h=4), r=[ost], w=[C.oT])


def m_rows(m):
    return {0: 0, 1: 256, 2: 768}[m]


def load_hankel(S, C, m, HK):
    for hl in range(4):
        head = m * 4 + hl
        U = UD if m == 2 else UAB
        L = LD if m == 2 else LAB
        src = bass.AP(tensor=C.vecs.h.tensor, offset=head * LD, ap=[[1, 128], [1, U]])
        S.dma(HK[hl][:, 0:U], src, r=[C.vecs], w=[HK[hl]])


def mixer_attn_phase(S, C, l, si, m):
    T_ = C.T
    s0 = si * T_
    S.phase_begin()
    qT = S.tile('qT', 2 * T_, BF16)
    kT = S.tile('kT', 2 * T_, BF16)
    v1 = S.tile('v1', (T_ // 128) * 260, BF16)
    HK = [S.tile(f'HK{i}', UD if m == 2 else UAB, BF16) for i in range(4)]
    pts = [S.tile(f'pt{i}', 512, BF16) for i in range(3)]
    ost = S.tile('ost', 4 * 512, BF16, parts=64)
    st = dict(oi=0, si=0, pi=0, den=S.tile('den', 512, F32), dhi=S.tile('dhi', 512, BF16),
              dlo=S.tile('dlo', 512, BF16), rd=S.tile('rd', 512, F32, parts=64))
    fm = C.fm
    S.dma(qT[:, :].rearrange("p (g t) -> p g t", g=2), fm[R_Q[m]:R_Q[m] + 256, s0:s0 + T_].rearrange("(g p) t -> p g t", p=128),
          r=[fm], w=[qT])
    S.dma(kT[:, :].rearrange("p (g t) -> p g t", g=2), fm[R_K[m]:R_K[m] + 256, s0:s0 + T_].rearrange("(g p) t -> p g t", p=128),
          r=[fm], w=[kT])
    for j0 in range(0, T_ // 128, 4):
        S.dma(v1[:, j0 * 260:(j0 + 4) * 260].rearrange("p (j c) -> p j c", c=260),
              C.v1s[m, s0 + j0 * 128:s0 + (j0 + 4) * 128, :].rearrange("(j p) c -> p j c", p=128), r=[C.v1s], w=[v1])
    load_hankel(S, C, m, HK)
    NG = T_ // 512
    if m == 2:
        for G in range(NG):
            attn_group(S, C, m, G, s0, qT, kT, v1, HK, None, pts, ost, st)
    elif m == 1:
        nb = T_ // 256
        C.esel = S.tile('esel', 16 * 128, BF16, parts=64)
        S.dma(C.esel[:, :], C.din['c_esel'], r=[C.dinT], w=[C.esel])
        kmf = S.tile('kmf', 2 * nb, F32)
        kmT = S.tile('kmT', 2 * 16, BF16)
        S.memset(kmT[:, :], 0.0, [kmT])
        S.op('dve', lambda e: e.tensor_reduce(kmf[:, :].rearrange("p (g n) -> p g n", g=2), kT[:, :].rearrange("p (g n k) -> p g n k", g=2, n=nb),
                                              axis=AX.X, op=ALU.add), [kT], [kmf])
        S.ts(kmT[:, :].rearrange("p (g n) -> p g n", g=2)[:, :, 0:nb], kmf[:, :].rearrange("p (g n) -> p g n", g=2),
             1.0 / 256, None, ALU.mult, None, [kmf], [kmT])
        gm = S.tile('gm', 64, F32)
        m8 = S.tile('m8', 32, F32)
        mb = S.tile('mb', 128, BF16)
        mbT = [S.tile('mbT0', 512, BF16, parts=64), S.tile('mbT1', 512, BF16, parts=64)]
        S.memset(gm[:, :], -1e30, [gm])
        S.memset(mb[:, :], 0.0, [mb])
        for G in range(NG):
            for qt in range(4):
                n = 4 * G + qt
                own = n // 2
                if own > 0 and DBG['gate']:
                    for hl in range(4):
                        pr, hf = divmod(hl, 2)
                        p0 = 64 * hf
                        gb = 7 if hf == 0 else 5
                        S.mm(S.pb(gb, pr * 16, pr * 16 + own), qT[p0:p0 + 64, pr * T_ + n * 128:pr * T_ + (n + 1) * 128],
                             kmT[p0:p0 + 64, pr * 16:pr * 16 + own], True, True, [qT, kmT], [S.banks[gb]])
                    gm4 = gm[:, :].rearrange("p (r f n) -> p r f n", r=2, f=2)
                    for hf_ in range(2):
                        gb = 7 if hf_ == 0 else 5
                        S.cp(gm4[:, :, hf_, 0:own], S.pb(gb, 0, 32).rearrange("p (r n) -> p r n", r=2)[:, :, 0:own],
                             [S.banks[gb]], [gm])
                for hl in range(4):
                    S.op('dve', lambda e, hl=hl: e.max(m8[:, hl * 8:(hl + 1) * 8], gm[:, hl * 16:(hl + 1) * 16]), [gm], [m8])
                    S.ts(mb[:, hl * 32:hl * 32 + 16], gm[:, hl * 16:(hl + 1) * 16], m8[:, hl * 8 + 2:hl * 8 + 3], NEG,
                         ALU.is_lt, ALU.mult, [gm, m8], [mb])
                S.memset(mb[:, :].rearrange("p (h n) -> p h n", h=4)[:, :, own:own + 1], 0.0, [mb])
                for pr in range(2):
                    S.tr(S.pb(7, 256 + pr * 128, 384 + pr * 128, BF16, 0, 64), mb[:, pr * 64:(pr + 1) * 64], C.ident[:, :],
                         [mb, C.ident], [S.banks[7]])
                    S.cp(mbT[pr][:, qt * 128:(qt + 1) * 128], S.pb(7, 256 + pr * 128, 384 + pr * 128, BF16, 0, 64),
                         [S.banks[7]], [mbT[pr]], eng='act')
            attn_group(S, C, m, G, s0, qT, kT, v1, HK, mbT, pts, ost, st)
    else:
        iqP = S.tile('iqP', 3 * T_, BF16)
        iqN = S.tile('iqN', 3 * T_, BF16)
        ikT = S.tile('ikT', T_, BF16)
        S.dma(iqP[:, :].rearrange("p (g t) -> p g t", g=3), fm[R_IQP:R_IQP + 384, s0:s0 + T_].rearrange("(g p) t -> p g t", p=128),
              r=[fm], w=[iqP])
        S.dma(iqN[:, :].rearrange("p (g t) -> p g t", g=3), fm[R_IQN:R_IQN + 384, s0:s0 + T_].rearrange("(g p) t -> p g t", p=128),
              r=[fm], w=[iqN])
        S.dma(ikT[:, :], fm[R_IK:R_IK + 128, s0:s0 + T_], r=[fm], w=[ikT])
        idx = S.tile('idx', T_, F32)
        jk = S.tile('jk', T_, BF16)
        jk2 = S.tile('jk2', T_, BF16)
        sB = S.tile('sB', 1, F32)
        mks = [S.tile(f'mk{i}', T_, BF16) for i in range(4)]
        cand = S.tile('cand', 1, F32)
        cnt = S.tile('cnt', 1, F32)
        dd = S.tile('dd', 1, F32)
        topk = min(256, T_ // 4)
        R = 512.0
        it = 0
        for G in range(NG):
            for qt in range(4):
                n = 4 * G + qt
                Sn = (n + 1) * 128
                first = True
                for sgn in range(2):
                    src = iqP if sgn == 0 else iqN
                    aop = ALU.max if sgn == 0 else ALU.min
                    for h in range(8):
                        g, hp = divmod(h, 3)
                        for c_lo in range(0, Sn, 2048):
                            c_hi = min(Sn, c_lo + 2048)
                            b0 = (it % 2) * 4
                            it += 1
                            nb_ = (c_hi - c_lo + 511) // 512
                            for k in range(nb_):
                                a0 = c_lo + k * 512
                                a1 = min(c_hi, a0 + 512)
                                S.mm(S.pb(b0 + k, 0, a1 - a0), src[32 * hp:32 * hp + 32, g * T_ + n * 128:g * T_ + (n + 1) * 128],
                                     ikT[32 * hp:32 * hp + 32, a0:a1], True, True, [src, ikT], [S.banks[b0 + k]])
                            bl = [S.banks[b0 + k] for k in range(nb_)]
                            if first:
                                S.ts(idx[:, c_lo:c_hi], S.pspan(b0, c_hi - c_lo), 0.0, None, aop, None, bl, [idx])
                            else:
                                S.stt(idx[:, c_lo:c_hi], S.pspan(b0, c_hi - c_lo), 0.0, idx[:, c_lo:c_hi], aop, ALU.add,
                                      bl + [idx], [idx])
                        first = False
                S.tt(idx[:, n * 128:(n + 1) * 128], idx[:, n * 128:(n + 1) * 128], C.tri[:, :], ALU.add, [idx, C.tri], [idx])
                mk = mks[qt]
                if Sn <= topk:
                    S.ts(mk[:, 0:Sn], idx[:, 0:Sn], -1e29, NEG, ALU.is_lt, ALU.mult, [idx], [mk])
                    continue
                S.memset(cand[:, :], 0.0, [cand])
                step = R
                hA = max(128, (int(Sn * 0.42) // 128) * 128)
                nB = Sn - hA
                for i in range(C.NBIS):
                    S.ts(jk[:, 0:hA], idx[:, 0:hA], cand[:, 0:1], 0.0, ALU.is_ge, ALU.add, [idx, cand], [jk, cnt],
                         accum_out=cnt[:, 0:1])
                    S.act(jk2[:, hA:Sn], idx[:, hA:Sn], AF.Sign, [idx, cand], [jk2, sB], bias=cand[:, 0:1], scale=-1.0,
                          accum_out=sB[:, 0:1])
                    S.stt(cnt[:, :], sB[:, :], -0.5, cnt[:, :], ALU.mult, ALU.add, [sB, cnt], [cnt])
                    S.ts(dd[:, :], cnt[:, :], topk - 0.5 - nB / 2.0, step, ALU.is_ge, ALU.mult, [cnt], [dd])
                    S.stt(cand[:, :], dd[:, :], -step / 2, cand[:, :], ALU.add, ALU.add, [dd, cand], [cand])
                    step /= 2
                S.ts(cand[:, :], cand[:, :], -step, None, ALU.add, None, [cand], [cand])
                S.ts(mk[:, 0:Sn], idx[:, 0:Sn], cand[:, 0:1], NEG, ALU.is_lt, ALU.mult, [idx, cand], [mk])
            attn_group(S, C, m, G, s0, qT, kT, v1, HK, mks, pts, ost, st)
    S.phase_end()


def hgrn_phase(S, C, l, si):
    T_ = C.T
    s0 = si * T_
    S.phase_begin()
    fm = C.fm
    ins = [S.tile(f'hin{i}', 5 * 4 * 512, BF16, parts=64) for i in range(2)]
    vin = [S.tile(f'hvin{i}', 8 * 256, BF16, parts=64) for i in range(2)]
    dec = [S.tile(f'hdec{i}', 32, F32, parts=64) for i in range(2)]
    ATs = S.tile('ATs', 256, BF16, parts=64)
    ksT = S.tile('ksT', 256, BF16, parts=64)
    S32 = S.tile('S32', 256, F32, parts=64)
    Sbf = S.tile('Sbf', 256, BF16, parts=64)
    sq = S.tile('hsq', 512, BF16, parts=64)
    sdt = S.tile('hsd', 512, F32, parts=64)
    t1 = S.tile('ht1', 512, F32, parts=64)
    ost = S.tile('host', 4 * 512, BF16, parts=64)
    hg = S.tile('hgain', 4, F32, parts=64)
    S.dma(hg[:, :], C.din['g_hgrn'][l], r=[C.dinT], w=[hg])
    for ti in range(T_ // 512):
        t0 = s0 + ti * 512
        i4 = ins[ti % 2][:, :].rearrange("p (k h t) -> p k h t", k=5, h=4)
        for k_ in range(5):
            S.dma(i4[:, k_], fm[R_H + k_ * 256:R_H + (k_ + 1) * 256, t0:t0 + 512].rearrange("(h p) t -> p h t", p=64),
                  r=[fm], w=[ins[ti % 2]])
        vv = vin[ti % 2]
        S.dma(vv[:, :].rearrange("p (c v) -> p c v", c=8), C.hv[t0:t0 + 512, :].rearrange("(c p) v -> p c v", p=64),
              r=[C.hv], w=[vv])
        dc = dec[ti % 2]
        S.dma(dc[:, :].rearrange("p (h c) -> p h c", h=4), C.hdec[:, t0 // 64:t0 // 64 + 8].rearrange("(h p) c -> p h c", p=64),
              r=[C.hdec], w=[dc])
        inT = ins[ti % 2]
        for ch in range(8):
            cs = slice(ch * 64, (ch + 1) * 64)
            first = (ti == 0 and ch == 0)
            for hd in range(4):
                S.mm(S.pb(0, hd * 64, (hd + 1) * 64, F32, 0, 64), i4[:, 1, hd, cs], i4[:, 0, hd, cs], True, True,
                     [inT], [S.banks[0]])
            S.tt(ATs[:, :], S.pb(0, 0, 256, F32, 0, 64), C.hmask[:, :], ALU.mult, [S.banks[0], C.hmask], [ATs])
            for hd in range(4):
                S.tr(S.pb(1, hd * 64, (hd + 1) * 64, BF16, 0, 64), i4[:, 3, hd, cs], C.ident[0:64, 0:64],
                     [inT, C.ident], [S.banks[1]])
            S.cp(ksT[:, :], S.pb(1, 0, 256, BF16, 0, 64), [S.banks[1]], [ksT], eng='act')
            for hd in range(4):
                vs = vv[:, ch * 256 + hd * 64:ch * 256 + (hd + 1) * 64]
                S.mm(S.pb(3 + hd, ch * 64, (ch + 1) * 64, F32, 0, 64), vs, ATs[:, hd * 64:(hd + 1) * 64], True, first,
                     [vv, ATs], [S.banks[3 + hd]])
                if not first:
                    S.mm(S.pb(3 + hd, ch * 64, (ch + 1) * 64, F32, 0, 64), Sbf[:, hd * 64:(hd + 1) * 64], i4[:, 2, hd, cs],
                         False, True, [Sbf, inT], [S.banks[3 + hd]])
            for hd in range(4):
                vs = vv[:, ch * 256 + hd * 64:ch * 256 + (hd + 1) * 64]
                S.mm(S.pb(2, hd * 64, (hd + 1) * 64, F32, 0, 64), ksT[:, hd * 64:(hd + 1) * 64], vs, True, True,
                     [ksT, vv], [S.banks[2]])
            if first:
                S.cp(S32[:, :], S.pb(2, 0, 256, F32, 0, 64), [S.banks[2]], [S32])
            else:
                for hd in range(4):
                    s_ = S32[:, hd * 64:(hd + 1) * 64]
                    S.stt(s_, s_, dc[:, hd * 8 + ch:hd * 8 + ch + 1], S.pb(2, hd * 64, (hd + 1) * 64, F32, 0, 64),
                          ALU.mult, ALU.add, [S32, dc, S.banks[2]], [S32])
            S.cp(Sbf[:, :], S32[:, :], [S32], [Sbf], eng='act')
        for hd in range(4):
            ops = S.pb(3 + hd, 0, 512, F32, 0, 64)
            S.act(sq[:, :], ops, AF.Square, [S.banks[3 + hd]], [sq])
            S.mm(S.pb(7, 0, 512, F32, 0, 64), C.ones[0:64, 0:64], sq[:, :], True, True, [C.ones, sq], [S.banks[7]])
            S.act(sdt[:, :], S.pb(7, 0, 512, F32, 0, 64), AF.Sqrt, [S.banks[7], C.eps], [sdt], scale=1.0 / 64,
                  bias=C.eps[0:64, 0:1])
            S.recip(sdt[:, :], sdt[:, :], [sdt], [sdt])
            S.stt(t1[:, :], ops, hg[:, hd:hd + 1], sdt[:, :], ALU.mult, ALU.mult, [S.banks[3 + hd], hg, sdt], [t1])
            S.tt(ost[:, hd * 512:(hd + 1) * 512], t1[:, :], i4[:, 4, hd, :], ALU.mult, [t1, inT], [ost])
        S.dma(C.oT[512:768, t0:t0 + 512].rearrange("(h p) t -> p h t", p=64), ost[:, :].rearrange("p (h t) -> p h t", h=4),
              r=[ost], w=[C.oT])
    S.phase_end()


def wout_phase(S, C, l):
    S.phase_begin()
    wo = S.tile('wo', 8 * D, BF16)
    C.stage = [S.tile('st0', 512, F32), S.tile('st1', 512, F32)]
    C.stage_i = 0
    for c in range(8):
        load_cast(S, C, wo, c * D, C.din['w_out'][l, c * 128:(c + 1) * 128, :], 128, D, C.dinT)
    xts = [S.tile(f'xt{i}', 4 * D, F32) for i in range(2)]
    ots = [S.tile(f'ot{i}', 8 * 512, BF16) for i in range(2)]
    for ti in range(C.NTOK // 512):
        t0 = ti * 512
        xt = xts[ti % 2]
        ot = ots[ti % 2]
        S.dma(xt[:, :].rearrange("p (a d) -> p a d", a=4), C.xres[t0:t0 + 512, :].rearrange("(a p) d -> p a d", p=128),
              r=[C.xres], w=[xt])
        S.dma(ot[:, :].rearrange("p (c t) -> p c t", c=8), C.oT[:, t0:t0 + 512].rearrange("(c p) t -> p c t", p=128),
              r=[C.oT], w=[ot])
        for a in range(4):
            for hf in range(2):
                b = (a * 2 + hf) % 4
                for c in range(8):
                    S.mm(S.pb(b), ot[:, c * 512 + a * 128:c * 512 + (a + 1) * 128], wo[:, c * D + hf * 512:c * D + (hf + 1) * 512],
                         c == 0, c == 7, [ot, wo], [S.banks[b]])
                xs = xt[:, a * D + hf * 512:a * D + (hf + 1) * 512]
                S.tt(xs, S.pb(b), xs, ALU.add, [S.banks[b], xt], [xt])
        S.dma(C.xres[t0:t0 + 512, :].rearrange("(a p) d -> p a d", p=128), xt[:, :].rearrange("p (a d) -> p a d", a=4),
              r=[xt], w=[C.xres])
    S.phase_end()


def build(T_=4096, NSEQ=2, DEPTH=2, NBIS=26, stop_after=None):
    nc = bass.Bass("TRN2", target_bir_lowering=False)
    S = Sched(nc)
    C = Ctx()
    C.T, C.NSEQ, C.DEPTH, C.NBIS = T_, NSEQ, DEPTH, NBIS
    NTOK = C.NTOK = T_ * NSEQ
    C.dinT = T(None, 'din')
    din = {}

    def inp(name, shape, dt=F32):
        din[name] = nc.dram_tensor(name, list(shape), dt, kind="ExternalInput").ap()
    inp('x', [NTOK, D])
    for nm in ('ffn1', 'ffn2'):
        inp(nm + '_gate', [DEPTH, D, DFF])
        inp(nm + '_up', [DEPTH, D, DFF])
        inp(nm + '_down', [DEPTH, DFF, D])
        inp('g_' + nm, [DEPTH, 128, 8])
    inp('g_mix', [DEPTH, 128, 8])
    inp('w_in_x', [DEPTH, D, NWX])
    inp('g_ckv', [DEPTH, 128, 1])
    inp('w_kv_up', [DEPTH, 128, 512])
    inp('lb_logits', [64, DEPTH * 4])
    inp('g_hgrn', [DEPTH, 64, 4])
    inp('w_out', [DEPTH, D, D])
    inp('rel_bias', [32, 12])
    inp('norm_final_b', [128, D])
    inp('c_ident', [128, 128], BF16)
    inp('c_anti', [128, 128], BF16)
    inp('c_ones', [128, 128], BF16)
    inp('c_tri', [128, 128])
    inp('c_hmask', [64, 256])
    inp('c_cmask', [64, 512])
    inp('c_esel', [64, 16 * 128], BF16)
    inp('c_ohab', [33, LAB], BF16)
    inp('c_ohd', [33, LD], BF16)
    C.din = din
    y = S.dram('y', [NTOK, D], F32, kind="ExternalOutput")
    C.xres = S.dram('xres', [NTOK, D], F32)
    C.fm = S.dram('fm', [FMROWS, NTOK], BF16)
    C.v1s = S.dram('v1s', [3, NTOK, 260], BF16)
    C.hv = S.dram('hv', [NTOK, 256], BF16)
    C.hdec = S.dram('hdec', [256, NTOK // 64], F32)
    C.oT = S.dram('oT', [1024, NTOK], BF16, kind=('ExternalOutput' if stop_after is not None else 'Internal'))
    C.vecs = S.dram('vecs', [12, LD], BF16)
    xin = T(din['x'], 'xin', dram=True)

    def cload(name, key, cols, dt, parts=128):
        t = S.tile(name, cols, dt, parts=parts)
        S.dma(t[:, :], din[key], r=[C.dinT], w=[t])
        return t
    C.ident = cload('ident', 'c_ident', 128, BF16)
    C.antiI = cload('antiI', 'c_anti', 128, BF16)
    C.ones = cload('ones', 'c_ones', 128, BF16)
    C.tri = cload('tri', 'c_tri', 128, F32)
    C.hmask = cload('hmask', 'c_hmask', 256, F32, parts=64)
    C.cmask = cload('cmask', 'c_cmask', 512, F32, parts=64)
    C.eps = S.tile('eps', 1, F32)
    S.memset(C.eps[:, :], EPS, [C.eps])
    C.negM = S.tile('negM', 1, F32)
    S.memset(C.negM[:, :], 0.0, [C.negM])
    C.far = S.tile('far', 12, F32)
    S.dma(C.far[:, :], bass.AP(tensor=din['rel_bias'].tensor, offset=31 * 12, ap=[[0, 128], [1, 12]]), r=[C.dinT], w=[C.far])
    S.phase_begin()
    tabf = S.tile('tabf', 12, F32, parts=33)
    tabb = S.tile('tabb', 12, BF16, parts=33)
    S.memset(tabf[:, :], 1.0, [tabf])
    S.dma(tabf[0:32, :], din['rel_bias'], r=[C.dinT], w=[tabf])
    S.cp(tabb[:, :], tabf[:, :], [tabf], [tabb])
    ohab = cload('ohab', 'c_ohab', LAB, BF16, parts=33)
    ohd = cload('ohd', 'c_ohd', LD, BF16, parts=33)
    vst_ = S.tile('vecst', LD, BF16, parts=12)
    S.memset(vst_[:, :], 0.0, [vst_])
    for (oh, L, h0, h1) in ((ohab, LAB, 0, 8), (ohd, LD, 8, 12)):
        for c0 in range(0, L, 512):
            c1 = min(L, c0 + 512)
            S.mm(S.pb(0, 0, c1 - c0, F32, 0, 12), tabb[:, :], oh[:, c0:c1], True, True, [tabb, oh], [S.banks[0]])
            tmp = S.tile(f'vtmp{h0}_{c0}', 512, BF16, parts=12)
            S.cp(tmp[:, 0:c1 - c0], S.pb(0, 0, c1 - c0, F32, 0, 12), [S.banks[0]], [tmp])
            S.dma(C.vecs[h0:h1, c0:c1], tmp[h0:h1, 0:c1 - c0], r=[tmp], w=[C.vecs])
    S.phase_end()

    for l in range(DEPTH):
        if stop_after == ('const', l):
            break
        ffn_phase(S, C, l, 0, xin if l == 0 else C.xres, C.xres, False)
        if stop_after == ('ffn1', l):
            break
        proj_phase(S, C, l)
        if stop_after == ('proj', l):
            break
        for si in range(NSEQ):
            for m in (0, 1, 2):
                if m in DBG['mixers']:
                    mixer_attn_phase(S, C, l, si, m)
            if 3 in DBG['mixers']:
                hgrn_phase(S, C, l, si)
        wout_phase(S, C, l)
        if stop_after == ('wout', l):
            break
        last = (l == DEPTH - 1)
        ffn_phase(S, C, l, 1, C.xres, y if last else C.xres, last)
    if stop_after is not None:
        S.phase_begin()
        xt = S.tile('dbgx', 4 * D, F32)
        for ti in range(NTOK // 512):
            t0 = ti * 512
            S.dma(xt[:, :].rearrange("p (a d) -> p a d", a=4), C.xres[t0:t0 + 512, :].rearrange("(a p) d -> p a d", p=128),
                  r=[C.xres], w=[xt])
            S.dma(y[t0:t0 + 512, :].rearrange("(a p) d -> p a d", p=128), xt[:, :].rearrange("p (a d) -> p a d", a=4),
                  r=[xt], w=[y])
        S.phase_end()
    counts = S.finish()
    return nc, counts


def host_consts(DEPTH, inputs):
    bf = ml_dtypes.bfloat16
    f32 = np.float32
    c = {}
    c['c_ident'] = np.eye(128, dtype=f32).astype(bf)
    c['c_anti'] = np.eye(128, dtype=f32)[::-1].copy().astype(bf)
    c['c_ones'] = np.ones((128, 128), f32).astype(bf)
    t = np.arange(128)
    c['c_tri'] = np.where(t[None, :] <= t[:, None], 0.0, -1e30).astype(f32)
    s = np.arange(64)
    hm = (s[:, None] <= s[None, :]).astype(f32)
    c['c_hmask'] = np.ascontiguousarray(np.tile(hm, (1, 4)))
    cm = np.ones((64, 512), f32)
    cm[:, ::64] = 0.0
    c['c_cmask'] = cm
    es = np.zeros((64, 16, 128), f32)
    for hl in range(2):
        for n in range(16):
            es[hl * 32 + n, n, :] = 1.0
    c['c_esel'] = es.reshape(64, 16 * 128).astype(bf)
    a, d = onehots()
    c['c_ohab'] = a.astype(bf)
    c['c_ohd'] = d.astype(bf)
    cols = win_cols()
    c['w_in_x'] = np.ascontiguousarray(inputs['w_in'][:, :, cols])

    def gc(v):
        return np.ascontiguousarray(v.reshape(DEPTH, 8, 128).transpose(0, 2, 1))
    c['g_ffn1'] = gc(inputs['norm_ffn1'])
    c['g_ffn2'] = gc(inputs['norm_ffn2'])
    c['g_mix'] = gc(inputs['norm_mix'])
    c['g_ckv'] = np.ascontiguousarray(inputs['ckv_norm'].reshape(DEPTH, 128, 1))
    c['g_hgrn'] = np.ascontiguousarray(inputs['hgrn_norm'].reshape(DEPTH, 4, 64).transpose(0, 2, 1))
    c['lb_logits'] = np.ascontiguousarray(inputs['hgrn_lb_logits'].reshape(DEPTH, 4, 64).transpose(2, 0, 1).reshape(64, DEPTH * 4))
    c['norm_final_b'] = np.ascontiguousarray(np.broadcast_to(inputs['norm_final'][None, :], (128, D)))
    for k in ('ffn1_gate', 'ffn1_up', 'ffn1_down', 'ffn2_gate', 'ffn2_up', 'ffn2_down', 'w_kv_up', 'w_out', 'rel_bias'):
        c[k] = np.ascontiguousarray(inputs[k])
    return c


_CACHE = {}


def run(inputs, T_, NSEQ, DEPTH, ncores, NBIS=26, stop_after=None):
    inputs = {k: np.asarray(v) for k, v in inputs.items()}
    key = (T_, NSEQ, DEPTH, NBIS, stop_after)
    if key not in _CACHE:
        _CACHE[key] = build(T_, NSEQ, DEPTH, NBIS, stop_after)
    nc, counts = _CACHE[key]
    consts = host_consts(DEPTH, inputs)
    x = inputs['x'].astype(np.float32, copy=False)
    in_maps = []
    for ci in range(ncores):
        mp = dict(consts)
        mp['x'] = np.ascontiguousarray(x[ci * NSEQ:(ci + 1) * NSEQ].reshape(NSEQ * T_, D))
        in_maps.append(mp)
    import os, time as _t
    _t0 = _t.time()
    res = run_bass_kernel_spmd(nc, in_maps, core_ids=list(range(ncores)), trace=bool(os.environ.get('KTRACE')))
    if os.environ.get('KTRACE'):
        print('EXEC_NS', getattr(res, 'exec_time_ns', None), 'wall', _t.time() - _t0)
    out = np.concatenate([np.asarray(r['y']).reshape(NSEQ, T_, D) for r in res.results], axis=0)
    if stop_after is not None:
        DBG['oT'] = [np.asarray(r['oT']) for r in res.results]
    return out.astype(np.float32)


def kernel(**inputs):
    return run(inputs, 4096, 2, 2, 8)
```

```python
import math
import contextlib
import numpy as np
import ml_dtypes
import concourse.bass as bass
import concourse.mybir as mybir
from concourse.bass_utils import run_bass_kernel_spmd

F32 = mybir.dt.float32
BF16 = mybir.dt.bfloat16
AF = mybir.ActivationFunctionType
ALU = mybir.AluOpType
AX = mybir.AxisListType

ENG = ('pe', 'act', 'dve', 'pool', 'sp')
SEM_PERIOD = 20000
NSLOT = 8
NEG = -30000.0
D = 1024
DFF = 2816
NFC = DFF // 128
EPS = 1e-6
SBUF_BYTES = 206 * 1024

OFF = dict(a_q=0, a_ckv=256, a_iq=384, a_ik=640, a_iw=672, b_q=680, b_k=936, b_v=1192,
           c_q=1448, c_f=1704, c_i=1960, c_g=2216, d_q=2472, d_k=2728, d_v=2984)


def win_cols():
    cols = []
    for nm in ('a_q', 'b_q', 'd_q', 'b_k', 'd_k'):
        cols += list(range(OFF[nm], OFF[nm] + 256))
    cols += list(range(OFF['a_ckv'], OFF['a_ckv'] + 128))
    for g in range(3):
        for hp in range(4):
            h = min(3 * g + hp, 7) if hp < 3 else min(3 * g, 7)
            cols += list(range(OFF['a_iq'] + 32 * h, OFF['a_iq'] + 32 * h + 32))
    for g in range(3):
        for hp in range(4):
            h = min(3 * g + hp, 7) if hp < 3 else min(3 * g, 7)
            cols += [OFF['a_iw'] + h] * 32
    for _ in range(4):
        cols += list(range(OFF['a_ik'], OFF['a_ik'] + 32))
    for nm in ('c_q', 'c_f', 'c_g'):
        cols += list(range(OFF[nm], OFF[nm] + 256))
    cols += list(range(OFF['b_v'], OFF['b_v'] + 256))
    cols += list(range(OFF['d_v'], OFF['d_v'] + 256))
    cols += list(range(OFF['c_i'], OFF['c_i'] + 256))
    return np.array(cols, dtype=np.int64)


NWX = 18 * 128 + 768 + 512 + 256
HG0 = 2304
TMV = 2304 + 768
TMI = TMV + 512

R_Q = {0: 0, 1: 256, 2: 512}
R_K = {1: 768, 2: 1024, 0: 1280}
R_IQP, R_IQN, R_IK = 1536, 1920, 2304
R_H = 2432
FMROWS = R_H + 5 * 256

UAB = 2048
UD = 2560
LAB = UAB + 128
LD = UD + 128
NEAR = 1664


def t5_bucket_np(d):
    n = np.maximum(d, 0)
    nf = np.maximum(n, 1).astype(np.float32)
    large = 16 + (np.log(nf / np.float32(16)) / np.float32(math.log(2048 / 16)) * np.float32(16)).astype(np.int32)
    large = np.minimum(large, 31)
    return np.where(n < 16, n, large)


def onehots():
    def mk(L, dil):
        x = np.arange(L)
        d = x - 127
        oh = np.zeros((33, L), np.float32)
        b = t5_bucket_np(d)
        if not dil:
            valid = d >= 0
            const = np.where(valid, 0.0, NEG)
        else:
            mult = ((d >= 0) & (d <= 128)).astype(np.int32) + ((d >= 0) & (d <= 512) & (d % 4 == 0)) \
                + ((d >= 0) & (d <= 2048) & (d % 16 == 0))
            valid = mult > 0
            const = np.where(valid, np.log(np.maximum(mult, 1)), NEG)
        oh[b[valid], x[valid]] = 1.0
        oh[32] = const
        return oh
    return mk(LAB, False), mk(LD, True)


class T:
    __slots__ = ('h', 'name', 'w', 'r', 'dram')

    def __init__(self, h, name, dram=False):
        self.h = h
        self.name = name
        self.w = None
        self.r = {}
        self.dram = dram

    def __getitem__(self, idx):
        return self.h[idx]


class Sched:
    def __init__(self, nc):
        self.nc = nc
        self.stack = contextlib.ExitStack()
        self.streams = {e: [] for e in ENG}
        self.known = {e: {} for e in ENG}
        self.marked = {e: set() for e in ENG}
        self.ndma = {e: 0 for e in ENG}
        self.selfsync = True
        self.arena = self.stack.enter_context(nc.sbuf_tensor("arena", [128, SBUF_BYTES // 4], F32))
        self.psum = self.stack.enter_context(nc.psum_tensor("psum", [128, 4096], F32))
        self.banks = [T(None, f"bank{i}") for i in range(8)]
        self.top = 0
        self.marks = []
        self.live = []
        self.dmaq = 0

    def tile(self, name, cols, dt, parts=128):
        nbytes = cols * (2 if dt == BF16 else 4)
        nbytes = (nbytes + 63) // 64 * 64
        assert self.top + nbytes <= SBUF_BYTES, (name, self.top, nbytes)
        a = self.arena[0:parts, self.top // 4:(self.top + nbytes) // 4]
        if dt == BF16:
            a = a.bitcast(BF16)
        a = a[:, 0:cols]
        self.top += nbytes
        t = T(a, name)
        self.live.append(t)
        return t

    def phase_begin(self):
        self.marks.append((self.top, len(self.live)))

    def phase_end(self):
        self.barrier()
        self.top, n = self.marks.pop()
        del self.live[n:]

    def pb(self, b, c0=0, c1=512, dt=F32, p0=0, p1=128):
        a = self.psum[p0:p1, b * 512:(b + 1) * 512]
        if dt == BF16:
            a = a.bitcast(BF16)
        return a[:, c0:c1]

    def pspan(self, b0, ncols, p0=0, p1=128):
        return self.psum[p0:p1, b0 * 512:b0 * 512 + ncols]

    def dram(self, name, shape, dt, kind="Internal"):
        h = self.nc.dram_tensor(name, list(shape), dt, kind=kind)
        return T(h.ap(), name, dram=True)

    def _need(self, eng, ev, waits, kind):
        if ev is None:
            return
        if ev[0] == 'c':
            if ev[1] == eng:
                if eng == 'pe' or kind != 'raw' or not self.selfsync:
                    return
            key = ('c', ev[1])
            val = ev[2]
        else:
            key = ('d', ev[1], ev[2])
            val = ev[3]
        if self.known[eng].get(key, -1) >= val:
            return
        self.known[eng][key] = val
        waits.append(ev)
        if ev[0] == 'c':
            self.marked[ev[1]].add(ev[2])

    def _deps(self, eng, r, w):
        waits = []
        for t in r:
            self._need(eng, t.w, waits, 'raw')
        for t in w:
            self._need(eng, t.w, waits, 'waw')
            for ev in t.r.values():
                self._need(eng, ev, waits, 'war')
        return waits

    def op(self, eng, fn, r=(), w=()):
        waits = self._deps(eng, r, w)
        idx = len(self.streams[eng])
        ev = ('c', eng, idx)
        self.streams[eng].append((waits, fn, None))
        for t in r:
            t.r[eng] = ev
        for t in w:
            t.w = ev
            t.r = {}
        return ev

    def dma(self, out_ap, in_ap, r=(), w=(), q=None, **kw):
        if q is None:
            q = 'sp'
        waits = self._deps(q, r, w)
        k = self.ndma[q]
        self.ndma[q] += 1
        slot = k % NSLOT
        cnt = k // NSLOT + 1
        if cnt > 1:
            self._need(q, ('d', q, slot, cnt - 1), waits, 'raw')
        ev = ('d', q, slot, cnt)
        self.streams[q].append(
            (waits, lambda e: e.dma_start(out=out_ap, in_=in_ap, **kw), (q, slot)))
        for t in r:
            t.r[('d', q, slot)] = ev
        for t in w:
            t.w = ev
            t.r = {}
        return ev

    def _all_events(self):
        evs = []
        for q in ENG:
            n = self.ndma[q]
            for slot in range(min(n, NSLOT)):
                evs.append(('d', q, slot, (n - 1 - slot) // NSLOT + 1))
        for e in ENG:
            n = len(self.streams[e])
            for i in range(n - 1, -1, -1):
                if self.streams[e][i][2] is None and self.streams[e][i][1] is not None:
                    evs.append(('c', e, i))
                    break
        return evs

    def barrier(self):
        evs = self._all_events()
        for e in ENG:
            waits = []
            for ev in evs:
                if ev[0] == 'c' and ev[1] == e:
                    continue
                self._need(e, ev, waits, 'raw')
            if waits:
                self.streams[e].append((waits, None, None))

    def finish(self):
        nc = self.nc
        self.barrier()
        self.streams['sp'].append(([], lambda e: e.nop(), None))
        rank = {}
        for e in ENG:
            ms = sorted(self.marked[e])
            rank[e] = {idx: i + 1 for i, idx in enumerate(ms)}
        csem = {e: [self.stack.enter_context(nc.semaphore(f"c_{e}_{i}"))
                    for i in range((len(rank[e]) + SEM_PERIOD - 1) // SEM_PERIOD)] for e in ENG}
        dsem = {}
        for q in ENG:
            for slot in range(min(self.ndma[q], NSLOT)):
                dsem[(q, slot)] = self.stack.enter_context(nc.semaphore(f"d_{q}_{slot}"))

        def ev2sem(ev):
            if ev[0] == 'c':
                c = rank[ev[1]][ev[2]]
                return csem[ev[1]][(c - 1) // SEM_PERIOD], (c - 1) % SEM_PERIOD + 1
            return dsem[(ev[1], ev[2])], 16 * ev[3]

        def replay(ename, eng):
            for idx, (waits, fn, dinfo) in enumerate(self.streams[ename]):
                for ev in waits:
                    s, v = ev2sem(ev)
                    eng.wait_ge(s, v)
                if fn is None:
                    continue
                inst = fn(eng)
                if dinfo is not None:
                    inst.then_inc(dsem[dinfo], 16)
                elif idx in rank[ename]:
                    c = rank[ename][idx]
                    inst.then_inc(csem[ename][(c - 1) // SEM_PERIOD], 1)

        allsems = [h for e in ENG for h in csem[e]] + list(dsem.values())
        for h in allsems:
            nc.gpsimd.sem_clear(h)
        nc.all_engine_barrier()
        with nc.Block() as block:
            @block.tensor
            def _(e):
                replay('pe', e)

            @block.scalar
            def _(e):
                replay('act', e)

            @block.vector
            def _(e):
                replay('dve', e)

            @block.gpsimd
            def _(e):
                replay('pool', e)

            @block.sync
            def _(e):
                replay('sp', e)
        nc.all_engine_barrier()
        for h in allsems:
            nc.gpsimd.sem_clear(h)
        nc.all_engine_barrier()
        self.stack.close()
        return {e: len(self.streams[e]) for e in ENG}

    def mm(self, out, lhsT, rhs, start, stop, r, w):
        return self.op('pe', lambda e: e.matmul(out, lhsT=lhsT, rhs=rhs, start=start, stop=stop,
                                                skip_group_check=True), r, w)

    def tr(self, out, in_, ident, r, w):
        return self.op('pe', lambda e: e.transpose(out, in_, ident), r, w)

    def act(self, out, in_, func, r, w, **kw):
        return self.op('act', lambda e: e.activation(out, in_, func, **kw), r, w)

    def ts(self, out, in0, s1, s2, op0, op1, r, w, eng='dve', accum_out=None):
        if op1 is None:
            return self.op(eng, lambda e: e.tensor_scalar(out, in0, s1, None, op0=op0), r, w)
        if accum_out is not None:
            return self.op(eng, lambda e: e.tensor_scalar(out, in0, s1, s2, op0=op0, op1=op1,
                                                          accum_out=accum_out), r, w)
        return self.op(eng, lambda e: e.tensor_scalar(out, in0, s1, s2, op0=op0, op1=op1), r, w)

    def stt(self, out, in0, sc, in1, op0, op1, r, w):
        return self.op('dve', lambda e: e.scalar_tensor_tensor(out, in0, sc, in1, op0=op0, op1=op1), r, w)

    def tt(self, out, in0, in1, op, r, w, eng='dve'):
        return self.op(eng, lambda e: e.tensor_tensor(out, in0, in1, op=op), r, w)

    def cp(self, out, in_, r, w, eng='dve'):
        if eng == 'act':
            return self.op('act', lambda e: e.copy(out, in_), r, w)
        return self.op(eng, lambda e: e.tensor_copy(out, in_), r, w)

    def recip(self, out, in_, r, w):
        return self.op('dve', lambda e: e.reciprocal(out, in_), r, w)

    def memset(self, ap, val, w, eng='dve'):
        return self.op(eng, lambda e: e.memset(ap, val), (), w)


class Ctx:
    pass


DBG = {'ffn_steps': 9, 'const': True, 'mixers': (0, 1, 2, 3), 'gate': True, 'bmask': True}


def load_cast(S, C, dst, dst_c0, src_ap, nrows, ncols, srcT, gcol=None):
    c = 0
    while c < ncols:
        n = min(512, ncols - c)
        st = C.stage[C.stage_i % 2]
        C.stage_i += 1
        S.dma(st[0:nrows, 0:n], src_ap[:, c:c + n], r=[srcT], w=[st])
        if gcol is None:
            S.ts(dst[0:nrows, dst_c0 + c:dst_c0 + c + n], st[0:nrows, 0:n], 1.0, 1.0, ALU.mult, ALU.mult,
                 [st], [dst], eng='pool')
        else:
            S.ts(dst[0:nrows, dst_c0 + c:dst_c0 + c + n], st[0:nrows, 0:n], gcol, 1.0, ALU.mult, ALU.mult,
                 [st, C.gcolT], [dst], eng='pool')
        c += n


def norm_front(S, C, xt, hbs, hT, junk, ssq, sd, rstd, ntr_bank=(0, 1)):
    for a in range(4):
        S.act(junk[:, :], xt[:, a * D:(a + 1) * D], AF.Square, [xt], [junk, ssq], accum_out=ssq[:, a:a + 1])
    S.act(sd[:, 0:4], ssq[:, 0:4], AF.Sqrt, [ssq, C.eps], [sd], scale=1.0 / D, bias=C.eps[:, 0:1])
    S.recip(rstd[:, 0:4], sd[:, 0:4], [sd], [rstd])
    hT3 = hT[:, :].rearrange("p (c t) -> p c t", c=8)
    for a in range(4):
        hb = hbs[a % 2]
        b = ntr_bank[a % 2]
        S.act(hb[:, :], xt[:, a * D:(a + 1) * D], AF.Copy, [xt, rstd], [hb], scale=rstd[:, a:a + 1])
        for c in range(8):
            S.tr(S.pb(b, c * 128, (c + 1) * 128, BF16), hb[:, c * 128:(c + 1) * 128], C.ident[:, :],
                 [hb, C.ident], [S.banks[b]])
        S.cp(hT3[:, :, a * 128:(a + 1) * 128], S.pb(b, 0, 1024, BF16).rearrange("p (c t) -> p c t", c=8),
             [S.banks[b]], [hT], eng=('act' if a % 2 else 'dve'))


def ffn_phase(S, C, l, which, src, dst, final):
    nm = 'ffn1' if which == 0 else 'ffn2'
    S.phase_begin()
    wg = S.tile('wg', 8 * DFF, BF16)
    wu = S.tile('wu', 8 * DFF, BF16)
    wd = S.tile('wd', NFC * D, BF16)
    C.stage = [S.tile('st0', 512, F32), S.tile('st1', 512, F32)]
    C.stage_i = 0
    gcol = S.tile('gcol', 8, F32)
    C.gcolT = gcol
    S.dma(gcol[:, :], C.din['g_' + nm][l], r=[C.dinT], w=[gcol])
    for c in range(8):
        load_cast(S, C, wg, c * DFF, C.din[nm + '_gate'][l, c * 128:(c + 1) * 128, :], 128, DFF, C.dinT, gcol[:, c:c + 1])
        load_cast(S, C, wu, c * DFF, C.din[nm + '_up'][l, c * 128:(c + 1) * 128, :], 128, DFF, C.dinT, gcol[:, c:c + 1])
    for f in range(NFC):
        load_cast(S, C, wd, f * D, C.din[nm + '_down'][l, f * 128:(f + 1) * 128, :], 128, D, C.dinT)
    xt = S.tile('xt', 4 * D, F32)
    junk = S.tile('junk', D, BF16)
    hb = [S.tile('hb0', D, BF16), S.tile('hb1', D, BF16)]
    hT = S.tile('hT', 8 * 512, BF16)
    aT = S.tile('aT', NFC * 512, BF16)
    sg = [S.tile('sg0', 512, BF16), S.tile('sg1', 512, BF16)]
    ssq = S.tile('ssq', 4, F32)
    sd = S.tile('sd', 4, F32)
    rstd = S.tile('rstd', 4, F32)
    if final:
        gfin = S.tile('gfin', D, F32)
        S.dma(gfin[:, :], C.din['norm_final_b'], r=[C.dinT], w=[gfin])
    for ti in range(C.NTOK // 512):
        t0 = ti * 512
        S.dma(xt[:, :].rearrange("p (a d) -> p a d", a=4), src[t0:t0 + 512, :].rearrange("(a p) d -> p a d", p=128),
              r=[src], w=[xt])
        if DBG['ffn_steps'] >= 1:
            norm_front(S, C, xt, hb, hT, junk, ssq, sd, rstd)
        for f in range(NFC if DBG['ffn_steps'] >= 2 else 0):
            bg = 2 + f % 2
            bu = 4 + f % 2
            for c in range(8):
                S.mm(S.pb(bg), wg[:, c * DFF + f * 128:c * DFF + (f + 1) * 128], hT[:, c * 512:(c + 1) * 512],
                     c == 0, c == 7, [wg, hT], [S.banks[bg]])
            for c in range(8):
                S.mm(S.pb(bu), wu[:, c * DFF + f * 128:c * DFF + (f + 1) * 128], hT[:, c * 512:(c + 1) * 512],
                     c == 0, c == 7, [wu, hT], [S.banks[bu]])
            s_ = sg[f % 2]
            S.act(s_[:, :], S.pb(bg), AF.Silu, [S.banks[bg]], [s_])
            S.tt(aT[:, f * 512:(f + 1) * 512], s_[:, :], S.pb(bu), ALU.mult, [s_, S.banks[bu]], [aT])
        for a in range(4 if DBG['ffn_steps'] >= 3 else 0):
            for hf in range(2):
                by = 6 + (a * 2 + hf) % 2
                for f in range(NFC):
                    S.mm(S.pb(by), aT[:, f * 512 + a * 128:f * 512 + (a + 1) * 128],
                         wd[:, f * D + hf * 512:f * D + (hf + 1) * 512], f == 0, f == NFC - 1, [aT, wd], [S.banks[by]])
                xs = xt[:, a * D + hf * 512:a * D + (hf + 1) * 512]
                S.stt(xs, S.pb(by), 0.5, xs, ALU.mult, ALU.add, [S.banks[by], xt], [xt])
        if final:
            for a in range(4):
                S.act(junk[:, :], xt[:, a * D:(a + 1) * D], AF.Square, [xt], [junk, ssq], accum_out=ssq[:, a:a + 1])
            S.act(sd[:, 0:4], ssq[:, 0:4], AF.Sqrt, [ssq, C.eps], [sd], scale=1.0 / D, bias=C.eps[:, 0:1])
            S.recip(rstd[:, 0:4], sd[:, 0:4], [sd], [rstd])
            for a in range(4):
                xs = xt[:, a * D:(a + 1) * D]
                S.stt(xs, xs, rstd[:, a:a + 1], gfin[:, :], ALU.mult, ALU.mult, [xt, rstd, gfin], [xt])
        S.dma(dst[t0:t0 + 512, :].rearrange("(a p) d -> p a d", p=128), xt[:, :].rearrange("p (a d) -> p a d", a=4),
              r=[xt], w=[dst])
    S.phase_end()


def proj_phase(S, C, l):
    S.phase_begin()
    NTOK = C.NTOK
    wi = S.tile('wi', 8 * NWX, BF16)
    wkv = S.tile('wkv', 512, BF16)
    C.stage = [S.tile('st0', 512, F32), S.tile('st1', 512, F32)]
    C.stage_i = 0
    gcol = S.tile('gcol', 8, F32)
    C.gcolT = gcol
    S.dma(gcol[:, :], C.din['g_mix'][l], r=[C.dinT], w=[gcol])
    for c in range(8):
        load_cast(S, C, wi, c * NWX, C.din['w_in_x'][l, c * 128:(c + 1) * 128, :], 128, NWX, C.dinT, gcol[:, c:c + 1])
    load_cast(S, C, wkv, 0, C.din['w_kv_up'][l], 128, 512, C.dinT)
    gck = S.tile('gck', 1, F32)
    S.dma(gck[:, :], C.din['g_ckv'][l], r=[C.dinT], w=[gck])
    lbl = S.tile('lbl', C.DEPTH * 4, F32, parts=64)
    S.dma(lbl[:, :], C.din['lb_logits'], r=[C.dinT], w=[lbl])
    lbe = S.tile('lbe', C.DEPTH * 4, F32, parts=64)
    for i in range(C.DEPTH):
        S.tt(lbe[:, i * 4:(i + 1) * 4], lbl[:, i * 4:(i + 1) * 4], lbl[:, 0:4], ALU.subtract, [lbl], [lbe])
    S.act(lbe[:, :], lbe[:, :], AF.Exp, [lbe], [lbe])
    tot = S.tile('lbtot', 4, F32, parts=64)
    num = S.tile('lbnum', 4, F32, parts=64)
    S.cp(tot[:, :], lbe[:, 0:4], [lbe], [tot])
    S.memset(num[:, :], 0.0, [num])
    for i in range(1, C.DEPTH):
        S.tt(tot[:, :], tot[:, :], lbe[:, i * 4:(i + 1) * 4], ALU.add, [tot, lbe], [tot])
        if i <= l:
            S.tt(num[:, :], num[:, :], lbe[:, i * 4:(i + 1) * 4], ALU.add, [num, lbe], [num])
    lb = S.tile('lb', 4, F32, parts=64)
    oml = S.tile('oml', 4, F32, parts=64)
    S.recip(tot[:, :], tot[:, :], [tot], [tot])
    S.tt(lb[:, :], num[:, :], tot[:, :], ALU.mult, [num, tot], [lb])
    S.ts(oml[:, :], lb[:, :], -1.0, 1.0, ALU.mult, ALU.add, [lb], [oml])

    xt = S.tile('xt', 4 * D, F32)
    junk = S.tile('junk', D, BF16)
    hb = [S.tile('hb0', D, BF16), S.tile('hb1', D, BF16)]
    hT = S.tile('hT', 8 * 512, BF16)
    ssq = S.tile('ssq', 4, F32)
    sd = S.tile('sd', 4, F32)
    rstd = S.tile('rstd', 4, F32)
    qk = S.tile('qk', 10 * 512, BF16)
    cf = S.tile('cf', 512, F32)
    csq = S.tile('csq', 512, BF16)
    crs = S.tile('crs', 512, F32)
    cTb = S.tile('cTb', 512, BF16)
    kast = S.tile('kast', 2 * 512, BF16)
    iqs = S.tile('iqs', 7 * 512, BF16)
    wP = S.tile('wP', 512, F32)
    wN = S.tile('wN', 512, F32)
    vst = S.tile('vst', 4 * 3 * 260, BF16)
    hvst = S.tile('hvst', 8 * 256, BF16, parts=64)
    hst = S.tile('hst', 5 * 4 * 512, BF16, parts=64)
    dst_ = S.tile('dst', 32, F32, parts=64)
    hf_ = [S.tile(f'hf{i}', 512, F32, parts=64) for i in range(8)]
    S.memset(vst[:, :], 1.0, [vst])
    vst4 = vst[:, :].rearrange("p (a m h c) -> p a m h c", a=4, m=3, h=4)
    fm = C.fm
    for ti in range(NTOK // 512):
        t0 = ti * 512
        S.dma(xt[:, :].rearrange("p (a d) -> p a d", a=4), C.xres[t0:t0 + 512, :].rearrange("(a p) d -> p a d", p=128),
              r=[C.xres], w=[xt])
        norm_front(S, C, xt, hb, hT, junk, ssq, sd, rstd)

        def fmproj(bank, col0, M):
            for c in range(8):
                S.mm(S.pb(bank, 0, 512, F32, 0, M), wi[:, c * NWX + col0:c * NWX + col0 + M], hT[:, c * 512:(c + 1) * 512],
                     c == 0, c == 7, [wi, hT], [S.banks[bank]])
        for g in range(10):
            b = 2 + g % 2
            fmproj(b, g * 128, 128)
            if g < 6:
                S.act(qk[:, g * 512:(g + 1) * 512], S.pb(b), AF.Copy, [S.banks[b]], [qk], scale=0.125)
            else:
                S.cp(qk[:, g * 512:(g + 1) * 512], S.pb(b), [S.banks[b]], [qk])
        S.dma(fm[0:1280, t0:t0 + 512].rearrange("(g p) t -> p g t", p=128), qk[:, :].rearrange("p (g t) -> p g t", g=10),
              r=[qk], w=[fm])
        fmproj(4, 10 * 128, 128)
        S.cp(cf[:, :], S.pb(4), [S.banks[4]], [cf], eng='act')
        S.act(csq[:, :], S.pb(4), AF.Square, [S.banks[4]], [csq])
        S.mm(S.pb(5), C.ones[:, :], csq[:, :], True, True, [C.ones, csq], [S.banks[5]])
        S.act(crs[:, :], S.pb(5), AF.Sqrt, [S.banks[5], C.eps], [crs], scale=1.0 / 128, bias=C.eps[:, 0:1])
        S.recip(crs[:, :], crs[:, :], [crs], [crs])
        S.stt(cTb[:, :], cf[:, :], gck[:, 0:1], crs[:, :], ALU.mult, ALU.mult, [cf, gck, crs], [cTb])
        for pr in range(2):
            S.mm(S.pb(6 + pr), wkv[:, pr * 128:(pr + 1) * 128], cTb[:, :], True, True, [wkv, cTb], [S.banks[6 + pr]])
            S.act(kast[:, pr * 512:(pr + 1) * 512], S.pb(6 + pr), AF.Copy, [S.banks[6 + pr]], [kast])
        S.dma(fm[R_K[0]:R_K[0] + 256, t0:t0 + 512].rearrange("(g p) t -> p g t", p=128),
              kast[:, :].rearrange("p (g t) -> p g t", g=2), r=[kast], w=[fm])
        for a in range(4):
            b = 2 + a % 2
            S.mm(S.pb(b, 0, 256), cTb[:, a * 128:(a + 1) * 128], wkv[:, 256:512], True, True, [cTb, wkv], [S.banks[b]])
            S.cp(vst4[:, a, 0, :, 0:64], S.pb(b, 0, 256).rearrange("p (h c) -> p h c", h=4), [S.banks[b]], [vst])
        for g in range(3):
            fmproj(4, (11 + g) * 128, 128)
            fmproj(5, (14 + g) * 128, 128)
            S.act(wP[:, :], S.pb(5), AF.Relu, [S.banks[5]], [wP])
            S.act(wN[:, :], S.pb(5), AF.Relu, [S.banks[5]], [wN], scale=-1.0)
            S.tt(iqs[:, g * 512:(g + 1) * 512], S.pb(4), wP[:, :], ALU.mult, [S.banks[4], wP], [iqs])
            S.stt(iqs[:, (3 + g) * 512:(4 + g) * 512], S.pb(4), -1.0, wN[:, :], ALU.mult, ALU.mult, [S.banks[4], wN], [iqs])
        fmproj(6, 17 * 128, 128)
        S.cp(iqs[:, 6 * 512:7 * 512], S.pb(6), [S.banks[6]], [iqs], eng='act')
        S.dma(fm[R_IQP:R_IQP + 896, t0:t0 + 512].rearrange("(g p) t -> p g t", p=128),
              iqs[:, :].rearrange("p (g t) -> p g t", g=7), r=[iqs], w=[fm])
        for a in range(4):
            b = 2 + a % 2
            for c in range(8):
                S.mm(S.pb(b), hT[:, c * 512 + a * 128:c * 512 + (a + 1) * 128], wi[:, c * NWX + TMV:c * NWX + TMV + 512],
                     c == 0, c == 7, [hT, wi], [S.banks[b]])
            S.cp(vst4[:, a, 1:3, :, 0:64], S.pb(b).rearrange("p (m h c) -> p m h c", m=2, h=4), [S.banks[b]], [vst],
                 eng=('act' if a % 2 else 'dve'))
        for m_ in range(3):
            S.dma(C.v1s[m_, t0:t0 + 512, :].rearrange("(a p) c -> p a c", p=128),
                  vst[:, :].rearrange("p (a m c) -> p a m c", a=4, m=3)[:, :, m_, :], r=[vst], w=[C.v1s])
        for ch in range(8):
            b = 4 + (ch // 2) % 2
            o = (ch % 2) * 256
            for c in range(8):
                S.mm(S.pb(b, o, o + 256, F32, 0, 64), hT[:, c * 512 + ch * 64:c * 512 + (ch + 1) * 64],
                     wi[:, c * NWX + TMI:c * NWX + TMI + 256], c == 0, c == 7, [hT, wi], [S.banks[b]])
            S.cp(hvst[:, ch * 256:(ch + 1) * 256], S.pb(b, o, o + 256, F32, 0, 64), [S.banks[b]], [hvst],
                 eng=('act' if ch % 2 else 'dve'))
        S.dma(C.hv[t0:t0 + 512, :].rearrange("(c p) v -> p c v", p=64), hvst[:, :].rearrange("p (c v) -> p c v", c=8),
              r=[hvst], w=[C.hv])
        hst4 = hst[:, :].rearrange("p (k h t) -> p k h t", k=5, h=4)
        for hd in range(4):
            fmproj(6, HG0 + hd * 64, 64)
            fmproj(7, HG0 + 256 + hd * 64, 64)
            fmproj(2, HG0 + 512 + hd * 64, 64)
            q_ps = S.pb(6, 0, 512, F32, 0, 64)
            sig, f_, lf, kin, b_, d1, d4, e1 = hf_
            S.act(sig[:, :], S.pb(7, 0, 512, F32, 0, 64), AF.Sigmoid, [S.banks[7]], [sig])
            S.ts(f_[:, :], sig[:, :], oml[:, hd:hd + 1], lb[:, hd:hd + 1], ALU.mult, ALU.add, [sig, oml, lb], [f_])
            S.act(lf[:, :], f_[:, :], AF.Ln, [f_], [lf])
            S.ts(kin[:, :], f_[:, :], -1.0, 1.0, ALU.mult, ALU.add, [f_], [kin])
            S.op('dve', lambda e, b_=b_, lf=lf: e.tensor_tensor_scan(b_[:, :], C.cmask[:, :], lf[:, :], 0.0,
                                                                        op0=ALU.mult, op1=ALU.add), [C.cmask, lf], [b_])
            b3 = b_[:, :].rearrange("p (c j) -> p c j", j=64)
            S.tt(d1[:, :].rearrange("p (c j) -> p c j", j=64), b3, b3[:, :, 31:32].broadcast_to([64, 8, 64]),
                 ALU.subtract, [b_], [d1])
            S.tt(d4[:, :].rearrange("p (c j) -> p c j", j=64), b3[:, :, 63:64].broadcast_to([64, 8, 64]), b3,
                 ALU.subtract, [b_], [d4])
            S.act(e1[:, :], d1[:, :], AF.Exp, [d1], [e1])
            S.tt(hst4[:, 0, hd, :], q_ps, e1[:, :], ALU.mult, [S.banks[6], e1], [hst])
            S.act(e1[:, :], d1[:, :], AF.Exp, [d1], [e1], scale=-1.0)
            S.tt(hst4[:, 1, hd, :], kin[:, :], e1[:, :], ALU.mult, [kin, e1], [hst])
            S.act(e1[:, :], b_[:, :], AF.Exp, [b_], [e1])
            S.tt(hst4[:, 2, hd, :], q_ps, e1[:, :], ALU.mult, [S.banks[6], e1], [hst])
            S.cp(dst_[:, hd * 8:(hd + 1) * 8], e1[:, :].rearrange("p (c j) -> p c j", j=64)[:, :, 63], [e1], [dst_])
            S.act(e1[:, :], d4[:, :], AF.Exp, [d4], [e1])
            S.tt(hst4[:, 3, hd, :], kin[:, :], e1[:, :], ALU.mult, [kin, e1], [hst])
            S.act(hst4[:, 4, hd, :], S.pb(2, 0, 512, F32, 0, 64), AF.Silu, [S.banks[2]], [hst])
        for k_ in range(5):
            S.dma(fm[R_H + k_ * 256:R_H + (k_ + 1) * 256, t0:t0 + 512].rearrange("(h p) t -> p h t", p=64), hst4[:, k_],
                  r=[hst], w=[fm])
        S.dma(C.hdec[:, t0 // 64:t0 // 64 + 8].rearrange("(h p) c -> p h c", p=64),
              dst_[:, :].rearrange("p (h c) -> p h c", h=4), r=[dst_], w=[C.hdec])
    S.phase_end()


def attn_group(S, C, m, G, s0, qT, kT, v1, HK, extra, pts, ost, st):
    T_ = C.T
    nkt = 4 * G + 4
    jmin = max(0, 4 * G - 16) if m == 2 else 0
    for hl in range(4):
        pr, hf = divmod(hl, 2)
        p0 = 64 * hf
        head = m * 4 + hl
        bo = 3 + st['oi'] % 2
        st['oi'] += 1
        for j in range(jmin, nkt):
            c0 = max(0, j - 4 * G) * 128
            N = 512 - c0
            dist0 = (4 * G * 128 + c0) - j * 128
            near = (m == 2) or dist0 < NEAR
            bs = st['si'] % 3
            st['si'] += 1
            nmm = 1 + (1 if near else 0)
            if m == 0:
                nmm += sum(1 for qt in range(4) if 4 * G + qt >= j)
            if m == 1 and DBG['bmask']:
                nmm += 1
            k = 0
            S.mm(S.pb(bs, c0, 512), kT[p0:p0 + 64, pr * T_ + j * 128:pr * T_ + (j + 1) * 128],
                 qT[p0:p0 + 64, pr * T_ + G * 512 + c0:pr * T_ + (G + 1) * 512], True, nmm == 1, [kT, qT], [S.banks[bs]])
            k += 1
            if near:
                S.mm(S.pb(bs, c0, 512), C.antiI[:, :], HK[hl][:, dist0:dist0 + N], False, k == nmm - 1,
                     [C.antiI, HK[hl]], [S.banks[bs]])
                k += 1
            if m == 0:
                for qt in range(4):
                    if 4 * G + qt >= j:
                        mk = extra[qt]
                        S.mm(S.pb(bs, qt * 128, (qt + 1) * 128), mk[:, j * 128:(j + 1) * 128], C.ident[:, :], False,
                             k == nmm - 1, [mk, C.ident], [S.banks[bs]])
                        k += 1
            if m == 1 and DBG['bmask']:
                ex = extra[hl // 2]
                hq = 32 * (hl % 2)
                S.mm(S.pb(bs, c0, 512), C.esel[hq:hq + 32, (j // 2) * 128:(j // 2 + 1) * 128],
                     ex[hq:hq + 32, c0:512], False, True, [C.esel, ex], [S.banks[bs]])
            pt = pts[st['pi'] % 3]
            st['pi'] += 1
            bcol = C.negM[:, 0:1] if near else C.far[:, head:head + 1]
            S.act(pt[:, c0:512], S.pb(bs, c0, 512), AF.Exp, [S.banks[bs], C.far, C.negM], [pt], bias=bcol)
            S.mm(S.pb(bo, c0, 512, F32, 0, 65), v1[:, j * 260 + hl * 65:j * 260 + hl * 65 + 65], pt[:, c0:512],
                 j == jmin, j == nkt - 1, [v1, pt], [S.banks[bo]])
        den, dhi, dlo, rd = st['den'], st['dhi'], st['dlo'], st['rd']
        S.cp(den[64:65, :], S.pb(bo, 0, 512, F32, 64, 65), [S.banks[bo]], [den], eng='act')
        S.cp(dhi[64:65, :], den[64:65, :], [den], [dhi])
        S.tt(dlo[64:65, :], den[64:65, :], dhi[64:65, :], ALU.subtract, [den, dhi], [dlo])
        S.mm(S.pb(6, 0, 512, F32, 0, 64), C.ones[64:65, 0:64], dhi[64:65, :], True, False, [C.ones, dhi], [S.banks[6]])
        S.mm(S.pb(6, 0, 512, F32, 0, 64), C.ones[64:65, 0:64], dlo[64:65, :], False, True, [C.ones, dlo], [S.banks[6]])
        S.recip(rd[0:64, :], S.pb(6, 0, 512, F32, 0, 64), [S.banks[6]], [rd])
        S.tt(ost[0:64, hl * 512:(hl + 1) * 512], S.pb(bo, 0, 512, F32, 0, 64), rd[0:64, :], ALU.mult,
             [S.banks[bo], rd], [ost])
    S.dma(C.oT[m_rows(m):m_rows(m) + 256, s0 + G * 512:s0 + (G + 1) * 512].rearrange("(h p) t -> p h t", p=64),
          ost[0:64, :].rearrange("p (h t) -> p h t", h=4), r=[ost], w=[C.oT])


def m_rows(m):
    return {0: 0, 1: 256, 2: 768}[m]


def load_hankel(S, C, m, HK):
    for hl in range(4):
        head = m * 4 + hl
        U = UD if m == 2 else UAB
        L = LD if m == 2 else LAB
        src = bass.AP(tensor=C.vecs.h.tensor, offset=head * LD, ap=[[1, 128], [1, U]])
        S.dma(HK[hl][:, 0:U], src, r=[C.vecs], w=[HK[hl]])


def mixer_attn_phase(S, C, l, si, m):
    T_ = C.T
    s0 = si * T_
    S.phase_begin()
    qT = S.tile('qT', 2 * T_, BF16)
    kT = S.tile('kT', 2 * T_, BF16)
    v1 = S.tile('v1', (T_ // 128) * 260, BF16)
    HK = [S.tile(f'HK{i}', UD if m == 2 else UAB, BF16) for i in range(4)]
    pts = [S.tile(f'pt{i}', 512, BF16) for i in range(3)]
    ost = S.tile('ost', 4 * 512, BF16, parts=64)
    st = dict(oi=0, si=0, pi=0, den=S.tile('den', 512, F32), dhi=S.tile('dhi', 512, BF16),
              dlo=S.tile('dlo', 512, BF16), rd=S.tile('rd', 512, F32, parts=64))
    fm = C.fm
    S.dma(qT[:, :].rearrange("p (g t) -> p g t", g=2), fm[R_Q[m]:R_Q[m] + 256, s0:s0 + T_].rearrange("(g p) t -> p g t", p=128),
          r=[fm], w=[qT])
    S.dma(kT[:, :].rearrange("p (g t) -> p g t", g=2), fm[R_K[m]:R_K[m] + 256, s0:s0 + T_].rearrange("(g p) t -> p g t", p=128),
          r=[fm], w=[kT])
    for j0 in range(0, T_ // 128, 4):
        S.dma(v1[:, j0 * 260:(j0 + 4) * 260].rearrange("p (j c) -> p j c", c=260),
              C.v1s[m, s0 + j0 * 128:s0 + (j0 + 4) * 128, :].rearrange("(j p) c -> p j c", p=128), r=[C.v1s], w=[v1])
    load_hankel(S, C, m, HK)
    NG = T_ // 512
    if m == 2:
        for G in range(NG):
            attn_group(S, C, m, G, s0, qT, kT, v1, HK, None, pts, ost, st)
    elif m == 1:
        nb = T_ // 256
        C.esel = S.tile('esel', 16 * 128, BF16, parts=64)
        S.dma(C.esel[:, :], C.din['c_esel'], r=[C.dinT], w=[C.esel])
        kmf = S.tile('kmf', 2 * nb, F32)
        kmT = S.tile('kmT', 2 * 16, BF16)
        S.memset(kmT[:, :], 0.0, [kmT])
        S.op('dve', lambda e: e.tensor_reduce(kmf[:, :].rearrange("p (g n) -> p g n", g=2), kT[:, :].rearrange("p (g n k) -> p g n k", g=2, n=nb),
                                              axis=AX.X, op=ALU.add), [kT], [kmf])
        S.ts(kmT[:, :].rearrange("p (g n) -> p g n", g=2)[:, :, 0:nb], kmf[:, :].rearrange("p (g n) -> p g n", g=2),
             1.0 / 256, None, ALU.mult, None, [kmf], [kmT])
        gm = S.tile('gm', 64, F32)
        m8 = S.tile('m8', 32, F32)
        mb = S.tile('mb', 128, BF16)
        mbT = [S.tile('mbT0', 512, BF16, parts=64), S.tile('mbT1', 512, BF16, parts=64)]
        S.memset(gm[:, :], -1e30, [gm])
        S.memset(mb[:, :], 0.0, [mb])
        for G in range(NG):
            for qt in range(4):
                n = 4 * G + qt
                own = n // 2
                if own > 0 and DBG['gate']:
                    for hl in range(4):
                        pr, hf = divmod(hl, 2)
                        p0 = 64 * hf
                        gb = 7 if hf == 0 else 5
                        S.mm(S.pb(gb, pr * 16, pr * 16 + own), qT[p0:p0 + 64, pr * T_ + n * 128:pr * T_ + (n + 1) * 128],
                             kmT[p0:p0 + 64, pr * 16:pr * 16 + own], True, True, [qT, kmT], [S.banks[gb]])
                    gm4 = gm[:, :].rearrange("p (r f n) -> p r f n", r=2, f=2)
                    for hf_ in range(2):
                        gb = 7 if hf_ == 0 else 5
                        S.cp(gm4[:, :, hf_, 0:own], S.pb(gb, 0, 32).rearrange("p (r n) -> p r n", r=2)[:, :, 0:own],
                             [S.banks[gb]], [gm])
                for hl in range(4):
                    S.op('dve', lambda e, hl=hl: e.max(m8[:, hl * 8:(hl + 1) * 8], gm[:, hl * 16:(hl + 1) * 16]), [gm], [m8])
                    S.ts(mb[:, hl * 32:hl * 32 + 16], gm[:, hl * 16:(hl + 1) * 16], m8[:, hl * 8 + 2:hl * 8 + 3], NEG,
                         ALU.is_lt, ALU.mult, [gm, m8], [mb])
                S.memset(mb[:, :].rearrange("p (h n) -> p h n", h=4)[:, :, own:own + 1], 0.0, [mb])
                for pr in range(2):
                    S.tr(S.pb(7, 256 + pr * 128, 384 + pr * 128, BF16, 0, 64), mb[:, pr * 64:(pr + 1) * 64], C.ident[:, :],
                         [mb, C.ident], [S.banks[7]])
                    S.cp(mbT[pr][:, qt * 128:(qt + 1) * 128], S.pb(7, 256 + pr * 128, 384 + pr * 128, BF16, 0, 64),
                         [S.banks[7]], [mbT[pr]], eng='act')
            attn_group(S, C, m, G, s0, qT, kT, v1, HK, mbT, pts, ost, st)
    else:
        iqP = S.tile('iqP', 3 * T_, BF16)
        iqN = S.tile('iqN', 3 * T_, BF16)
        ikT = S.tile('ikT', T_, BF16)
        S.dma(iqP[:, :].rearrange("p (g t) -> p g t", g=3), fm[R_IQP:R_IQP + 384, s0:s0 + T_].rearrange("(g p) t -> p g t", p=128),
              r=[fm], w=[iqP])
        S.dma(iqN[:, :].rearrange("p (g t) -> p g t", g=3), fm[R_IQN:R_IQN + 384, s0:s0 + T_].rearrange("(g p) t -> p g t", p=128),
              r=[fm], w=[iqN])
        S.dma(ikT[:, :], fm[R_IK:R_IK + 128, s0:s0 + T_], r=[fm], w=[ikT])
        idx = S.tile('idx', T_, F32)
        jk = S.tile('jk', T_, BF16)
        jk2 = S.tile('jk2', T_, BF16)
        sB = S.tile('sB', 1, F32)
        mks = [S.tile(f'mk{i}', T_, BF16) for i in range(4)]
        cand = S.tile('cand', 1, F32)
        cnt = S.tile('cnt', 1, F32)
        dd = S.tile('dd', 1, F32)
        topk = min(256, T_ // 4)
        R = 64.0
        it = 0
        for G in range(NG):
            for qt in range(4):
                n = 4 * G + qt
                Sn = (n + 1) * 128
                first = True
                for sgn in range(2):
                    src = iqP if sgn == 0 else iqN
                    aop = ALU.max if sgn == 0 else ALU.min
                    for h in range(8):
                        g, hp = divmod(h, 3)
                        for c_lo in range(0, Sn, 2048):
                            c_hi = min(Sn, c_lo + 2048)
                            b0 = (it % 2) * 4
                            it += 1
                            nb_ = (c_hi - c_lo + 511) // 512
                            for k in range(nb_):
                                a0 = c_lo + k * 512
                                a1 = min(c_hi, a0 + 512)
                                S.mm(S.pb(b0 + k, 0, a1 - a0), src[32 * hp:32 * hp + 32, g * T_ + n * 128:g * T_ + (n + 1) * 128],
                                     ikT[32 * hp:32 * hp + 32, a0:a1], True, True, [src, ikT], [S.banks[b0 + k]])
                            bl = [S.banks[b0 + k] for k in range(nb_)]
                            if first:
                                S.ts(idx[:, c_lo:c_hi], S.pspan(b0, c_hi - c_lo), 0.0, None, aop, None, bl, [idx])
                            else:
                                S.stt(idx[:, c_lo:c_hi], S.pspan(b0, c_hi - c_lo), 0.0, idx[:, c_lo:c_hi], aop, ALU.add,
                                      bl + [idx], [idx])
                        first = False
                S.tt(idx[:, n * 128:(n + 1) * 128], idx[:, n * 128:(n + 1) * 128], C.tri[:, :], ALU.add, [idx, C.tri], [idx])
                mk = mks[qt]
                if Sn <= topk:
                    S.ts(mk[:, 0:Sn], idx[:, 0:Sn], -1e29, NEG, ALU.is_lt, ALU.mult, [idx], [mk])
                    continue
                S.memset(cand[:, :], 0.0, [cand])
                step = R
                hA = max(128, (int(Sn * 0.42) // 128) * 128)
                nB = Sn - hA
                for i in range(C.NBIS):
                    S.ts(jk[:, 0:hA], idx[:, 0:hA], cand[:, 0:1], 0.0, ALU.is_ge, ALU.add, [idx, cand], [jk, cnt],
                         accum_out=cnt[:, 0:1])
                    S.act(jk2[:, hA:Sn], idx[:, hA:Sn], AF.Sign, [idx, cand], [jk2, sB], bias=cand[:, 0:1], scale=-1.0,
                          accum_out=sB[:, 0:1])
                    S.stt(cnt[:, :], sB[:, :], -0.5, cnt[:, :], ALU.mult, ALU.add, [sB, cnt], [cnt])
                    S.ts(dd[:, :], cnt[:, :], topk - 0.5 - nB / 2.0, step, ALU.is_ge, ALU.mult, [cnt], [dd])
                    S.stt(cand[:, :], dd[:, :], -step / 2, cand[:, :], ALU.add, ALU.add, [dd, cand], [cand])
                    step /= 2
                S.ts(cand[:, :], cand[:, :], -step, None, ALU.add, None, [cand], [cand])
                S.ts(mk[:, 0:Sn], idx[:, 0:Sn], cand[:, 0:1], NEG, ALU.is_lt, ALU.mult, [idx, cand], [mk])
            attn_group(S, C, m, G, s0, qT, kT, v1, HK, mks, pts, ost, st)
    S.phase_end()


def hgrn_phase(S, C, l, si):
    T_ = C.T
    s0 = si * T_
    S.phase_begin()
    fm = C.fm
    ins = [S.tile(f'hin{i}', 5 * 4 * 512, BF16, parts=64) for i in range(2)]
    vin = [S.tile(f'hvin{i}', 8 * 256, BF16, parts=64) for i in range(2)]
    dec = [S.tile(f'hdec{i}', 32, F32, parts=64) for i in range(2)]
    ATs = S.tile('ATs', 256, BF16, parts=64)
    ksT = S.tile('ksT', 256, BF16, parts=64)
    S32 = S.tile('S32', 256, F32, parts=64)
    Sbf = S.tile('Sbf', 256, BF16, parts=64)
    sq = S.tile('hsq', 512, BF16, parts=64)
    sdt = S.tile('hsd', 512, F32, parts=64)
    t1 = S.tile('ht1', 512, F32, parts=64)
    ost = S.tile('host', 4 * 512, BF16, parts=64)
    hg = S.tile('hgain', 4, F32, parts=64)
    S.dma(hg[:, :], C.din['g_hgrn'][l], r=[C.dinT], w=[hg])
    for ti in range(T_ // 512):
        t0 = s0 + ti * 512
        i4 = ins[ti % 2][:, :].rearrange("p (k h t) -> p k h t", k=5, h=4)
        for k_ in range(5):
            S.dma(i4[:, k_], fm[R_H + k_ * 256:R_H + (k_ + 1) * 256, t0:t0 + 512].rearrange("(h p) t -> p h t", p=64),
                  r=[fm], w=[ins[ti % 2]])
        vv = vin[ti % 2]
        S.dma(vv[:, :].rearrange("p (c v) -> p c v", c=8), C.hv[t0:t0 + 512, :].rearrange("(c p) v -> p c v", p=64),
              r=[C.hv], w=[vv])
        dc = dec[ti % 2]
        S.dma(dc[:, :].rearrange("p (h c) -> p h c", h=4), C.hdec[:, t0 // 64:t0 // 64 + 8].rearrange("(h p) c -> p h c", p=64),
              r=[C.hdec], w=[dc])
        inT = ins[ti % 2]
        for ch in range(8):
            cs = slice(ch * 64, (ch + 1) * 64)
            first = (ti == 0 and ch == 0)
            for hd in range(4):
                S.mm(S.pb(0, hd * 64, (hd + 1) * 64, F32, 0, 64), i4[:, 1, hd, cs], i4[:, 0, hd, cs], True, True,
                     [inT], [S.banks[0]])
            S.tt(ATs[:, :], S.pb(0, 0, 256, F32, 0, 64), C.hmask[:, :], ALU.mult, [S.banks[0], C.hmask], [ATs])
            for hd in range(4):
                S.tr(S.pb(1, hd * 64, (hd + 1) * 64, BF16, 0, 64), i4[:, 3, hd, cs], C.ident[0:64, 0:64],
                     [inT, C.ident], [S.banks[1]])
            S.cp(ksT[:, :], S.pb(1, 0, 256, BF16, 0, 64), [S.banks[1]], [ksT], eng='act')
            for hd in range(4):
                vs = vv[:, ch * 256 + hd * 64:ch * 256 + (hd + 1) * 64]
                S.mm(S.pb(3 + hd, ch * 64, (ch + 1) * 64, F32, 0, 64), vs, ATs[:, hd * 64:(hd + 1) * 64], True, first,
                     [vv, ATs], [S.banks[3 + hd]])
                if not first:
                    S.mm(S.pb(3 + hd, ch * 64, (ch + 1) * 64, F32, 0, 64), Sbf[:, hd * 64:(hd + 1) * 64], i4[:, 2, hd, cs],
                         False, True, [Sbf, inT], [S.banks[3 + hd]])
            for hd in range(4):
                vs = vv[:, ch * 256 + hd * 64:ch * 256 + (hd + 1) * 64]
                S.mm(S.pb(2, hd * 64, (hd + 1) * 64, F32, 0, 64), ksT[:, hd * 64:(hd + 1) * 64], vs, True, True,
                     [ksT, vv], [S.banks[2]])
            if first:
                S.cp(S32[:, :], S.pb(2, 0, 256, F32, 0, 64), [S.banks[2]], [S32])
            else:
                for hd in range(4):
                    s_ = S32[:, hd * 64:(hd + 1) * 64]
                    S.stt(s_, s_, dc[:, hd * 8 + ch:hd * 8 + ch + 1], S.pb(2, hd * 64, (hd + 1) * 64, F32, 0, 64),
                          ALU.mult, ALU.add, [S32, dc, S.banks[2]], [S32])
            S.cp(Sbf[:, :], S32[:, :], [S32], [Sbf], eng='act')
        for hd in range(4):
            ops = S.pb(3 + hd, 0, 512, F32, 0, 64)
            S.act(sq[:, :], ops, AF.Square, [S.banks[3 + hd]], [sq])
            S.mm(S.pb(7, 0, 512, F32, 0, 64), C.ones[0:64, 0:64], sq[:, :], True, True, [C.ones, sq], [S.banks[7]])
            S.act(sdt[:, :], S.pb(7, 0, 512, F32, 0, 64), AF.Sqrt, [S.banks[7], C.eps], [sdt], scale=1.0 / 64,
                  bias=C.eps[0:64, 0:1])
            S.recip(sdt[:, :], sdt[:, :], [sdt], [sdt])
            S.stt(t1[:, :], ops, hg[:, hd:hd + 1], sdt[:, :], ALU.mult, ALU.mult, [S.banks[3 + hd], hg, sdt], [t1])
            S.tt(ost[:, hd * 512:(hd + 1) * 512], t1[:, :], i4[:, 4, hd, :], ALU.mult, [t1, inT], [ost])
        S.dma(C.oT[512:768, t0:t0 + 512].rearrange("(h p) t -> p h t", p=64), ost[:, :].rearrange("p (h t) -> p h t", h=4),
              r=[ost], w=[C.oT])
    S.phase_end()


def wout_phase(S, C, l):
    S.phase_begin()
    wo = S.tile('wo', 8 * D, BF16)
    C.stage = [S.tile('st0', 512, F32), S.tile('st1', 512, F32)]
    C.stage_i = 0
    for c in range(8):
        load_cast(S, C, wo, c * D, C.din['w_out'][l, c * 128:(c + 1) * 128, :], 128, D, C.dinT)
    xts = [S.tile(f'xt{i}', 4 * D, F32) for i in range(2)]
    ots = [S.tile(f'ot{i}', 8 * 512, BF16) for i in range(2)]
    for ti in range(C.NTOK // 512):
        t0 = ti * 512
        xt = xts[ti % 2]
        ot = ots[ti % 2]
        S.dma(xt[:, :].rearrange("p (a d) -> p a d", a=4), C.xres[t0:t0 + 512, :].rearrange("(a p) d -> p a d", p=128),
              r=[C.xres], w=[xt])
        S.dma(ot[:, :].rearrange("p (c t) -> p c t", c=8), C.oT[:, t0:t0 + 512].rearrange("(c p) t -> p c t", p=128),
              r=[C.oT], w=[ot])
        for a in range(4):
            for hf in range(2):
                b = (a * 2 + hf) % 4
                for c in range(8):
                    S.mm(S.pb(b), ot[:, c * 512 + a * 128:c * 512 + (a + 1) * 128], wo[:, c * D + hf * 512:c * D + (hf + 1) * 512],
                         c == 0, c == 7, [ot, wo], [S.banks[b]])
                xs = xt[:, a * D + hf * 512:a * D + (hf + 1) * 512]
                S.tt(xs, S.pb(b), xs, ALU.add, [S.banks[b], xt], [xt])
        S.dma(C.xres[t0:t0 + 512, :].rearrange("(a p) d -> p a d", p=128), xt[:, :].rearrange("p (a d) -> p a d", a=4),
              r=[xt], w=[C.xres])
    S.phase_end()


def build(T_=4096, NSEQ=2, DEPTH=2, NBIS=23, stop_after=None):
    nc = bass.Bass("TRN2", target_bir_lowering=False)
    S = Sched(nc)
    C = Ctx()
    C.T, C.NSEQ, C.DEPTH, C.NBIS = T_, NSEQ, DEPTH, NBIS
    NTOK = C.NTOK = T_ * NSEQ
    C.dinT = T(None, 'din')
    din = {}

    def inp(name, shape, dt=F32):
        din[name] = nc.dram_tensor(name, list(shape), dt, kind="ExternalInput").ap()
    inp('x', [NTOK, D])
    for nm in ('ffn1', 'ffn2'):
        inp(nm + '_gate', [DEPTH, D, DFF])
        inp(nm + '_up', [DEPTH, D, DFF])
        inp(nm + '_down', [DEPTH, DFF, D])
        inp('g_' + nm, [DEPTH, 128, 8])
    inp('g_mix', [DEPTH, 128, 8])
    inp('w_in_x', [DEPTH, D, NWX])
    inp('g_ckv', [DEPTH, 128, 1])
    inp('w_kv_up', [DEPTH, 128, 512])
    inp('lb_logits', [64, DEPTH * 4])
    inp('g_hgrn', [DEPTH, 64, 4])
    inp('w_out', [DEPTH, D, D])
    inp('rel_bias', [32, 12])
    inp('norm_final_b', [128, D])
    inp('c_ident', [128, 128], BF16)
    inp('c_anti', [128, 128], BF16)
    inp('c_ones', [128, 128], BF16)
    inp('c_tri', [128, 128])
    inp('c_hmask', [64, 256])
    inp('c_cmask', [64, 512])
    inp('c_esel', [64, 16 * 128], BF16)
    inp('c_ohab', [33, LAB], BF16)
    inp('c_ohd', [33, LD], BF16)
    C.din = din
    y = S.dram('y', [NTOK, D], F32, kind="ExternalOutput")
    C.xres = S.dram('xres', [NTOK, D], F32)
    C.fm = S.dram('fm', [FMROWS, NTOK], BF16)
    C.v1s = S.dram('v1s', [3, NTOK, 260], BF16)
    C.hv = S.dram('hv', [NTOK, 256], BF16)
    C.hdec = S.dram('hdec', [256, NTOK // 64], F32)
    C.oT = S.dram('oT', [1024, NTOK], BF16, kind=('ExternalOutput' if stop_after is not None else 'Internal'))
    C.vecs = S.dram('vecs', [12, LD], BF16)
    xin = T(din['x'], 'xin', dram=True)

    def cload(name, key, cols, dt, parts=128):
        t = S.tile(name, cols, dt, parts=parts)
        S.dma(t[:, :], din[key], r=[C.dinT], w=[t])
        return t
    C.ident = cload('ident', 'c_ident', 128, BF16)
    C.antiI = cload('antiI', 'c_anti', 128, BF16)
    C.ones = cload('ones', 'c_ones', 128, BF16)
    C.tri = cload('tri', 'c_tri', 128, F32)
    C.hmask = cload('hmask', 'c_hmask', 256, F32, parts=64)
    C.cmask = cload('cmask', 'c_cmask', 512, F32, parts=64)
    C.eps = S.tile('eps', 1, F32)
    S.memset(C.eps[:, :], EPS, [C.eps])
    C.negM = S.tile('negM', 1, F32)
    S.memset(C.negM[:, :], 0.0, [C.negM])
    C.far = S.tile('far', 12, F32)
    S.dma(C.far[:, :], bass.AP(tensor=din['rel_bias'].tensor, offset=31 * 12, ap=[[0, 128], [1, 12]]), r=[C.dinT], w=[C.far])
    S.phase_begin()
    tabf = S.tile('tabf', 12, F32, parts=33)
    tabb = S.tile('tabb', 12, BF16, parts=33)
    S.memset(tabf[:, :], 1.0, [tabf])
    S.dma(tabf[0:32, :], din['rel_bias'], r=[C.dinT], w=[tabf])
    S.cp(tabb[:, :], tabf[:, :], [tabf], [tabb])
    ohab = cload('ohab', 'c_ohab', LAB, BF16, parts=33)
    ohd = cload('ohd', 'c_ohd', LD, BF16, parts=33)
    vst_ = S.tile('vecst', LD, BF16, parts=12)
    S.memset(vst_[:, :], 0.0, [vst_])
    for (oh, L, h0, h1) in ((ohab, LAB, 0, 8), (ohd, LD, 8, 12)):
        for c0 in range(0, L, 512):
            c1 = min(L, c0 + 512)
            S.mm(S.pb(0, 0, c1 - c0, F32, 0, 12), tabb[:, :], oh[:, c0:c1], True, True, [tabb, oh], [S.banks[0]])
            tmp = S.tile(f'vtmp{h0}_{c0}', 512, BF16, parts=12)
            S.cp(tmp[:, 0:c1 - c0], S.pb(0, 0, c1 - c0, F32, 0, 12), [S.banks[0]], [tmp])
            S.dma(C.vecs[h0:h1, c0:c1], tmp[h0:h1, 0:c1 - c0], r=[tmp], w=[C.vecs])
    S.phase_end()

    for l in range(DEPTH):
        if stop_after == ('const', l):
            break
        ffn_phase(S, C, l, 0, xin if l == 0 else C.xres, C.xres, False)
        if stop_after == ('ffn1', l):
            break
        proj_phase(S, C, l)
        if stop_after == ('proj', l):
            break
        for si in range(NSEQ):
            for m in (0, 1, 2):
                if m in DBG['mixers']:
                    mixer_attn_phase(S, C, l, si, m)
            if 3 in DBG['mixers']:
                hgrn_phase(S, C, l, si)
        wout_phase(S, C, l)
        if stop_after == ('wout', l):
            break
        last = (l == DEPTH - 1)
        ffn_phase(S, C, l, 1, C.xres, y if last else C.xres, last)
    if stop_after is not None:
        S.phase_begin()
        xt = S.tile('dbgx', 4 * D, F32)
        for ti in range(NTOK // 512):
            t0 = ti * 512
            S.dma(xt[:, :].rearrange("p (a d) -> p a d", a=4), C.xres[t0:t0 + 512, :].rearrange("(a p) d -> p a d", p=128),
                  r=[C.xres], w=[xt])
            S.dma(y[t0:t0 + 512, :].rearrange("(a p) d -> p a d", p=128), xt[:, :].rearrange("p (a d) -> p a d", a=4),
                  r=[xt], w=[y])
        S.phase_end()
    counts = S.finish()
    return nc, counts


def host_consts(DEPTH, inputs):
    bf = ml_dtypes.bfloat16
    f32 = np.float32
    c = {}
    c['c_ident'] = np.eye(128, dtype=f32).astype(bf)
    c['c_anti'] = np.eye(128, dtype=f32)[::-1].copy().astype(bf)
    c['c_ones'] = np.ones((128, 128), f32).astype(bf)
    t = np.arange(128)
    c['c_tri'] = np.where(t[None, :] <= t[:, None], 0.0, -1e30).astype(f32)
    s = np.arange(64)
    hm = (s[:, None] <= s[None, :]).astype(f32)
    c['c_hmask'] = np.ascontiguousarray(np.tile(hm, (1, 4)))
    cm = np.ones((64, 512), f32)
    cm[:, ::64] = 0.0
    c['c_cmask'] = cm
    es = np.zeros((64, 16, 128), f32)
    for hl in range(2):
        for n in range(16):
            es[hl * 32 + n, n, :] = 1.0
    c['c_esel'] = es.reshape(64, 16 * 128).astype(bf)
    a, d = onehots()
    c['c_ohab'] = a.astype(bf)
    c['c_ohd'] = d.astype(bf)
    cols = win_cols()
    c['w_in_x'] = np.ascontiguousarray(inputs['w_in'][:, :, cols])

    def gc(v):
        return np.ascontiguousarray(v.reshape(DEPTH, 8, 128).transpose(0, 2, 1))
    c['g_ffn1'] = gc(inputs['norm_ffn1'])
    c['g_ffn2'] = gc(inputs['norm_ffn2'])
    c['g_mix'] = gc(inputs['norm_mix'])
    c['g_ckv'] = np.ascontiguousarray(inputs['ckv_norm'].reshape(DEPTH, 128, 1))
    c['g_hgrn'] = np.ascontiguousarray(inputs['hgrn_norm'].reshape(DEPTH, 4, 64).transpose(0, 2, 1))
    c['lb_logits'] = np.ascontiguousarray(inputs['hgrn_lb_logits'].reshape(DEPTH, 4, 64).transpose(2, 0, 1).reshape(64, DEPTH * 4))
    c['norm_final_b'] = np.ascontiguousarray(np.broadcast_to(inputs['norm_final'][None, :], (128, D)))
    for k in ('ffn1_gate', 'ffn1_up', 'ffn1_down', 'ffn2_gate', 'ffn2_up', 'ffn2_down', 'w_kv_up', 'w_out', 'rel_bias'):
        c[k] = np.ascontiguousarray(inputs[k])
    return c


_CACHE = {}


def run(inputs, T_, NSEQ, DEPTH, ncores, NBIS=23, stop_after=None):
    inputs = {k: np.asarray(v) for k, v in inputs.items()}
    key = (T_, NSEQ, DEPTH, NBIS, stop_after)
    if key not in _CACHE:
        _CACHE[key] = build(T_, NSEQ, DEPTH, NBIS, stop_after)
    nc, counts = _CACHE[key]
    consts = host_consts(DEPTH, inputs)
    x = inputs['x'].astype(np.float32, copy=False)
    in_maps = []
    for ci in range(ncores):
        mp = dict(consts)
        mp['x'] = np.ascontiguousarray(x[ci * NSEQ:(ci + 1) * NSEQ].reshape(NSEQ * T_, D))
        in_maps.append(mp)
    import os, time as _t
    _t0 = _t.time()
    res = run_bass_kernel_spmd(nc, in_maps, core_ids=list(range(ncores)), trace=bool(os.environ.get('KTRACE')))
    if os.environ.get('KTRACE'):
        print('EXEC_NS', getattr(res, 'exec_time_ns', None), 'wall', _t.time() - _t0)
    out = np.concatenate([np.asarray(r['y']).reshape(NSEQ, T_, D) for r in res.results], axis=0)
    if stop_after is not None:
        DBG['oT'] = [np.asarray(r['oT']) for r in res.results]
    return out.astype(np.float32)


def kernel(**inputs):
    return run(inputs, 4096, 2, 2, 8)
```

```python
import math
import contextlib
import numpy as np
import ml_dtypes
import concourse.bass as bass
import concourse.mybir as mybir
from concourse.bass_utils import run_bass_kernel_spmd

F32 = mybir.dt.float32
BF16 = mybir.dt.bfloat16
AF = mybir.ActivationFunctionType
ALU = mybir.AluOpType
AX = mybir.AxisListType

ENG = ('pe', 'act', 'dve', 'pool', 'sp')
SEM_PERIOD = 20000
NSLOT = 8
NEG = -30000.0
D = 1024
DFF = 2816
NFC = DFF // 128
EPS = 1e-6
SBUF_BYTES = 206 * 1024

OFF = dict(a_q=0, a_ckv=256, a_iq=384, a_ik=640, a_iw=672, b_q=680, b_k=936, b_v=1192,
           c_q=1448, c_f=1704, c_i=1960, c_g=2216, d_q=2472, d_k=2728, d_v=2984)


def win_cols():
    cols = []
    for nm in ('a_q', 'b_q', 'd_q', 'b_k', 'd_k'):
        cols += list(range(OFF[nm], OFF[nm] + 256))
    cols += list(range(OFF['a_ckv'], OFF['a_ckv'] + 128))
    for g in range(3):
        for hp in range(4):
            h = min(3 * g + hp, 7) if hp < 3 else min(3 * g, 7)
            cols += list(range(OFF['a_iq'] + 32 * h, OFF['a_iq'] + 32 * h + 32))
    for g in range(3):
        for hp in range(4):
            h = min(3 * g + hp, 7) if hp < 3 else min(3 * g, 7)
            cols += [OFF['a_iw'] + h] * 32
    for _ in range(4):
        cols += list(range(OFF['a_ik'], OFF['a_ik'] + 32))
    for nm in ('c_q', 'c_f', 'c_g'):
        cols += list(range(OFF[nm], OFF[nm] + 256))
    cols += list(range(OFF['b_v'], OFF['b_v'] + 256))
    cols += list(range(OFF['d_v'], OFF['d_v'] + 256))
    cols += list(range(OFF['c_i'], OFF['c_i'] + 256))
    return np.array(cols, dtype=np.int64)


NWX = 18 * 128 + 768 + 512 + 256
HG0 = 2304
TMV = 2304 + 768
TMI = TMV + 512

R_Q = {0: 0, 1: 256, 2: 512}
R_K = {1: 768, 2: 1024, 0: 1280}
R_IQP, R_IQN, R_IK = 1536, 1920, 2304
R_H = 2432
FMROWS = R_H + 5 * 256

UAB = 2048
UD = 2560
LAB = UAB + 128
LD = UD + 128
NEAR = 1664


def t5_bucket_np(d):
    n = np.maximum(d, 0)
    nf = np.maximum(n, 1).astype(np.float32)
    large = 16 + (np.log(nf / np.float32(16)) / np.float32(math.log(2048 / 16)) * np.float32(16)).astype(np.int32)
    large = np.minimum(large, 31)
    return np.where(n < 16, n, large)


def onehots():
    def mk(L, dil):
        x = np.arange(L)
        d = x - 127
        oh = np.zeros((33, L), np.float32)
        b = t5_bucket_np(d)
        if not dil:
            valid = d >= 0
            const = np.where(valid, 0.0, NEG)
        else:
            mult = ((d >= 0) & (d <= 128)).astype(np.int32) + ((d >= 0) & (d <= 512) & (d % 4 == 0)) \
                + ((d >= 0) & (d <= 2048) & (d % 16 == 0))
            valid = mult > 0
            const = np.where(valid, np.log(np.maximum(mult, 1)), NEG)
        oh[b[valid], x[valid]] = 1.0
        oh[32] = const
        return oh
    return mk(LAB, False), mk(LD, True)


class T:
    __slots__ = ('h', 'name', 'w', 'r', 'dram')

    def __init__(self, h, name, dram=False):
        self.h = h
        self.name = name
        self.w = None
        self.r = {}
        self.dram = dram

    def __getitem__(self, idx):
        return self.h[idx]


class Sched:
    def __init__(self, nc):
        self.nc = nc
        self.stack = contextlib.ExitStack()
        self.streams = {e: [] for e in ENG}
        self.known = {e: {} for e in ENG}
        self.marked = {e: set() for e in ENG}
        self.ndma = {e: 0 for e in ENG}
        self.selfsync = True
        self.arena = self.stack.enter_context(nc.sbuf_tensor("arena", [128, SBUF_BYTES // 4], F32))
        self.psum = self.stack.enter_context(nc.psum_tensor("psum", [128, 4096], F32))
        self.banks = [T(None, f"bank{i}") for i in range(8)]
        self.top = 0
        self.marks = []
        self.live = []
        self.dmaq = 0

    def tile(self, name, cols, dt, parts=128):
        nbytes = cols * (2 if dt == BF16 else 4)
        nbytes = (nbytes + 63) // 64 * 64
        assert self.top + nbytes <= SBUF_BYTES, (name, self.top, nbytes)
        a = self.arena[0:parts, self.top // 4:(self.top + nbytes) // 4]
        if dt == BF16:
            a = a.bitcast(BF16)
        a = a[:, 0:cols]
        self.top += nbytes
        t = T(a, name)
        self.live.append(t)
        return t

    def phase_begin(self):
        self.marks.append((self.top, len(self.live)))

    def phase_end(self):
        self.barrier()
        self.top, n = self.marks.pop()
        del self.live[n:]

    def pb(self, b, c0=0, c1=512, dt=F32, p0=0, p1=128):
        a = self.psum[p0:p1, b * 512:(b + 1) * 512]
        if dt == BF16:
            a = a.bitcast(BF16)
        return a[:, c0:c1]

    def pspan(self, b0, ncols, p0=0, p1=128):
        return self.psum[p0:p1, b0 * 512:b0 * 512 + ncols]

    def dram(self, name, shape, dt, kind="Internal"):
        h = self.nc.dram_tensor(name, list(shape), dt, kind=kind)
        return T(h.ap(), name, dram=True)

    def _need(self, eng, ev, waits, kind):
        if ev is None:
            return
        if ev[0] == 'c':
            if ev[1] == eng:
                if eng == 'pe' or kind != 'raw' or not self.selfsync:
                    return
            key = ('c', ev[1])
            val = ev[2]
        else:
            key = ('d', ev[1], ev[2])
            val = ev[3]
        if self.known[eng].get(key, -1) >= val:
            return
        self.known[eng][key] = val
        waits.append(ev)
        if ev[0] == 'c':
            self.marked[ev[1]].add(ev[2])

    def _deps(self, eng, r, w):
        waits = []
        for t in r:
            self._need(eng, t.w, waits, 'raw')
        for t in w:
            self._need(eng, t.w, waits, 'waw')
            for ev in t.r.values():
                self._need(eng, ev, waits, 'war')
        return waits

    def op(self, eng, fn, r=(), w=()):
        waits = self._deps(eng, r, w)
        idx = len(self.streams[eng])
        ev = ('c', eng, idx)
        self.streams[eng].append((waits, fn, None))
        for t in r:
            t.r[eng] = ev
        for t in w:
            t.w = ev
            t.r = {}
        return ev

    def dma(self, out_ap, in_ap, r=(), w=(), q=None, **kw):
        if q is None:
            q = 'sp'
        waits = self._deps(q, r, w)
        k = self.ndma[q]
        self.ndma[q] += 1
        slot = k % NSLOT
        cnt = k // NSLOT + 1
        if cnt > 1:
            self._need(q, ('d', q, slot, cnt - 1), waits, 'raw')
        ev = ('d', q, slot, cnt)
        self.streams[q].append(
            (waits, lambda e: e.dma_start(out=out_ap, in_=in_ap, **kw), (q, slot)))
        for t in r:
            t.r[('d', q, slot)] = ev
        for t in w:
            t.w = ev
            t.r = {}
        return ev

    def _all_events(self):
        evs = []
        for q in ENG:
            n = self.ndma[q]
            for slot in range(min(n, NSLOT)):
                evs.append(('d', q, slot, (n - 1 - slot) // NSLOT + 1))
        for e in ENG:
            n = len(self.streams[e])
            for i in range(n - 1, -1, -1):
                if self.streams[e][i][2] is None and self.streams[e][i][1] is not None:
                    evs.append(('c', e, i))
                    break
        return evs

    def barrier(self):
        evs = self._all_events()
        for e in ENG:
            waits = []
            for ev in evs:
                if ev[0] == 'c' and ev[1] == e:
                    continue
                self._need(e, ev, waits, 'raw')
            if waits:
                self.streams[e].append((waits, None, None))

    def finish(self):
        nc = self.nc
        self.barrier()
        self.streams['sp'].append(([], lambda e: e.nop(), None))
        rank = {}
        for e in ENG:
            ms = sorted(self.marked[e])
            rank[e] = {idx: i + 1 for i, idx in enumerate(ms)}
        csem = {e: [self.stack.enter_context(nc.semaphore(f"c_{e}_{i}"))
                    for i in range((len(rank[e]) + SEM_PERIOD - 1) // SEM_PERIOD)] for e in ENG}
        dsem = {}
        for q in ENG:
            for slot in range(min(self.ndma[q], NSLOT)):
                dsem[(q, slot)] = self.stack.enter_context(nc.semaphore(f"d_{q}_{slot}"))

        def ev2sem(ev):
            if ev[0] == 'c':
                c = rank[ev[1]][ev[2]]
                return csem[ev[1]][(c - 1) // SEM_PERIOD], (c - 1) % SEM_PERIOD + 1
            return dsem[(ev[1], ev[2])], 16 * ev[3]

        def replay(ename, eng):
            for idx, (waits, fn, dinfo) in enumerate(self.streams[ename]):
                for ev in waits:
                    s, v = ev2sem(ev)
                    eng.wait_ge(s, v)
                if fn is None:
                    continue
                inst = fn(eng)
                if dinfo is not None:
                    inst.then_inc(dsem[dinfo], 16)
                elif idx in rank[ename]:
                    c = rank[ename][idx]
                    inst.then_inc(csem[ename][(c - 1) // SEM_PERIOD], 1)

        allsems = [h for e in ENG for h in csem[e]] + list(dsem.values())
        for h in allsems:
            nc.gpsimd.sem_clear(h)
        nc.all_engine_barrier()
        with nc.Block() as block:
            @block.tensor
            def _(e):
                replay('pe', e)

            @block.scalar
            def _(e):
                replay('act', e)

            @block.vector
            def _(e):
                replay('dve', e)

            @block.gpsimd
            def _(e):
                replay('pool', e)

            @block.sync
            def _(e):
                replay('sp', e)
        nc.all_engine_barrier()
        for h in allsems:
            nc.gpsimd.sem_clear(h)
        nc.all_engine_barrier()
        self.stack.close()
        return {e: len(self.streams[e]) for e in ENG}

    def mm(self, out, lhsT, rhs, start, stop, r, w):
        return self.op('pe', lambda e: e.matmul(out, lhsT=lhsT, rhs=rhs, start=start, stop=stop,
                                                skip_group_check=True), r, w)

    def tr(self, out, in_, ident, r, w):
        return self.op('pe', lambda e: e.transpose(out, in_, ident), r, w)

    def act(self, out, in_, func, r, w, **kw):
        return self.op('act', lambda e: e.activation(out, in_, func, **kw), r, w)

    def ts(self, out, in0, s1, s2, op0, op1, r, w, eng='dve', accum_out=None):
        if op1 is None:
            return self.op(eng, lambda e: e.tensor_scalar(out, in0, s1, None, op0=op0), r, w)
        if accum_out is not None:
            return self.op(eng, lambda e: e.tensor_scalar(out, in0, s1, s2, op0=op0, op1=op1,
                                                          accum_out=accum_out), r, w)
        return self.op(eng, lambda e: e.tensor_scalar(out, in0, s1, s2, op0=op0, op1=op1), r, w)

    def stt(self, out, in0, sc, in1, op0, op1, r, w):
        return self.op('dve', lambda e: e.scalar_tensor_tensor(out, in0, sc, in1, op0=op0, op1=op1), r, w)

    def tt(self, out, in0, in1, op, r, w, eng='dve'):
        return self.op(eng, lambda e: e.tensor_tensor(out, in0, in1, op=op), r, w)

    def cp(self, out, in_, r, w, eng='dve'):
        if eng == 'act':
            return self.op('act', lambda e: e.copy(out, in_), r, w)
        return self.op(eng, lambda e: e.tensor_copy(out, in_), r, w)

    def recip(self, out, in_, r, w):
        return self.op('dve', lambda e: e.reciprocal(out, in_), r, w)

    def memset(self, ap, val, w, eng='dve'):
        return self.op(eng, lambda e: e.memset(ap, val), (), w)


class Ctx:
    pass


DBG = {'ffn_steps': 9, 'const': True, 'mixers': (0, 1, 2, 3), 'gate': True, 'bmask': True}


def load_cast(S, C, dst, dst_c0, src_ap, nrows, ncols, srcT, gcol=None):
    c = 0
    while c < ncols:
        n = min(512, ncols - c)
        st = C.stage[C.stage_i % 2]
        C.stage_i += 1
        S.dma(st[0:nrows, 0:n], src_ap[:, c:c + n], r=[srcT], w=[st])
        if gcol is None:
            S.ts(dst[0:nrows, dst_c0 + c:dst_c0 + c + n], st[0:nrows, 0:n], 1.0, 1.0, ALU.mult, ALU.mult,
                 [st], [dst], eng='pool')
        else:
            S.ts(dst[0:nrows, dst_c0 + c:dst_c0 + c + n], st[0:nrows, 0:n], gcol, 1.0, ALU.mult, ALU.mult,
                 [st, C.gcolT], [dst], eng='pool')
        c += n


def norm_front(S, C, xt, hbs, hT, junk, ssq, sd, rstd, ntr_bank=(0, 1)):
    for a in range(4):
        S.act(junk[:, :], xt[:, a * D:(a + 1) * D], AF.Square, [xt], [junk, ssq], accum_out=ssq[:, a:a + 1])
    S.act(sd[:, 0:4], ssq[:, 0:4], AF.Sqrt, [ssq, C.eps], [sd], scale=1.0 / D, bias=C.eps[:, 0:1])
    S.recip(rstd[:, 0:4], sd[:, 0:4], [sd], [rstd])
    hT3 = hT[:, :].rearrange("p (c t) -> p c t", c=8)
    for a in range(4):
        hb = hbs[a % 2]
        b = ntr_bank[a % 2]
        S.act(hb[:, :], xt[:, a * D:(a + 1) * D], AF.Copy, [xt, rstd], [hb], scale=rstd[:, a:a + 1])
        for c in range(8):
            S.tr(S.pb(b, c * 128, (c + 1) * 128, BF16), hb[:, c * 128:(c + 1) * 128], C.ident[:, :],
                 [hb, C.ident], [S.banks[b]])
        S.cp(hT3[:, :, a * 128:(a + 1) * 128], S.pb(b, 0, 1024, BF16).rearrange("p (c t) -> p c t", c=8),
             [S.banks[b]], [hT], eng=('act' if a % 2 else 'dve'))


def ffn_phase(S, C, l, which, src, dst, final):
    nm = 'ffn1' if which == 0 else 'ffn2'
    S.phase_begin()
    wg = S.tile('wg', 8 * DFF, BF16)
    wu = S.tile('wu', 8 * DFF, BF16)
    wd = S.tile('wd', NFC * D, BF16)
    C.stage = [S.tile('st0', 512, F32), S.tile('st1', 512, F32)]
    C.stage_i = 0
    gcol = S.tile('gcol', 8, F32)
    C.gcolT = gcol
    S.dma(gcol[:, :], C.din['g_' + nm][l], r=[C.dinT], w=[gcol])
    for c in range(8):
        load_cast(S, C, wg, c * DFF, C.din[nm + '_gate'][l, c * 128:(c + 1) * 128, :], 128, DFF, C.dinT, gcol[:, c:c + 1])
        load_cast(S, C, wu, c * DFF, C.din[nm + '_up'][l, c * 128:(c + 1) * 128, :], 128, DFF, C.dinT, gcol[:, c:c + 1])
    for f in range(NFC):
        load_cast(S, C, wd, f * D, C.din[nm + '_down'][l, f * 128:(f + 1) * 128, :], 128, D, C.dinT)
    xt = S.tile('xt', 4 * D, F32)
    junk = S.tile('junk', D, BF16)
    hb = [S.tile('hb0', D, BF16), S.tile('hb1', D, BF16)]
    hT = S.tile('hT', 8 * 512, BF16)
    aT = S.tile('aT', NFC * 512, BF16)
    sg = [S.tile('sg0', 512, BF16), S.tile('sg1', 512, BF16)]
    ssq = S.tile('ssq', 4, F32)
    sd = S.tile('sd', 4, F32)
    rstd = S.tile('rstd', 4, F32)
    if final:
        gfin = S.tile('gfin', D, F32)
        S.dma(gfin[:, :], C.din['norm_final_b'], r=[C.dinT], w=[gfin])
    for ti in range(C.NTOK // 512):
        t0 = ti * 512
        S.dma(xt[:, :].rearrange("p (a d) -> p a d", a=4), src[t0:t0 + 512, :].rearrange("(a p) d -> p a d", p=128),
              r=[src], w=[xt])
        if DBG['ffn_steps'] >= 1:
            norm_front(S, C, xt, hb, hT, junk, ssq, sd, rstd)
        for f in range(NFC if DBG['ffn_steps'] >= 2 else 0):
            bg = 2 + f % 2
            bu = 4 + f % 2
            for c in range(8):
                S.mm(S.pb(bg), wg[:, c * DFF + f * 128:c * DFF + (f + 1) * 128], hT[:, c * 512:(c + 1) * 512],
                     c == 0, c == 7, [wg, hT], [S.banks[bg]])
            for c in range(8):
                S.mm(S.pb(bu), wu[:, c * DFF + f * 128:c * DFF + (f + 1) * 128], hT[:, c * 512:(c + 1) * 512],
                     c == 0, c == 7, [wu, hT], [S.banks[bu]])
            s_ = sg[f % 2]
            S.act(s_[:, :], S.pb(bg), AF.Silu, [S.banks[bg]], [s_])
            S.tt(aT[:, f * 512:(f + 1) * 512], s_[:, :], S.pb(bu), ALU.mult, [s_, S.banks[bu]], [aT])
        for a in range(4 if DBG['ffn_steps'] >= 3 else 0):
            for hf in range(2):
                by = 6 + (a * 2 + hf) % 2
                for f in range(NFC):
                    S.mm(S.pb(by), aT[:, f * 512 + a * 128:f * 512 + (a + 1) * 128],
                         wd[:, f * D + hf * 512:f * D + (hf + 1) * 512], f == 0, f == NFC - 1, [aT, wd], [S.banks[by]])
                xs = xt[:, a * D + hf * 512:a * D + (hf + 1) * 512]
                S.stt(xs, S.pb(by), 0.5, xs, ALU.mult, ALU.add, [S.banks[by], xt], [xt])
        if final:
            for a in range(4):
                S.act(junk[:, :], xt[:, a * D:(a + 1) * D], AF.Square, [xt], [junk, ssq], accum_out=ssq[:, a:a + 1])
            S.act(sd[:, 0:4], ssq[:, 0:4], AF.Sqrt, [ssq, C.eps], [sd], scale=1.0 / D, bias=C.eps[:, 0:1])
            S.recip(rstd[:, 0:4], sd[:, 0:4], [sd], [rstd])
            for a in range(4):
                xs = xt[:, a * D:(a + 1) * D]
                S.stt(xs, xs, rstd[:, a:a + 1], gfin[:, :], ALU.mult, ALU.mult, [xt, rstd, gfin], [xt])
        S.dma(dst[t0:t0 + 512, :].rearrange("(a p) d -> p a d", p=128), xt[:, :].rearrange("p (a d) -> p a d", a=4),
              r=[xt], w=[dst])
    S.phase_end()


def proj_phase(S, C, l):
    S.phase_begin()
    NTOK = C.NTOK
    wi = S.tile('wi', 8 * NWX, BF16)
    wkv = S.tile('wkv', 512, BF16)
    C.stage = [S.tile('st0', 512, F32), S.tile('st1', 512, F32)]
    C.stage_i = 0
    gcol = S.tile('gcol', 8, F32)
    C.gcolT = gcol
    S.dma(gcol[:, :], C.din['g_mix'][l], r=[C.dinT], w=[gcol])
    for c in range(8):
        load_cast(S, C, wi, c * NWX, C.din['w_in_x'][l, c * 128:(c + 1) * 128, :], 128, NWX, C.dinT, gcol[:, c:c + 1])
    load_cast(S, C, wkv, 0, C.din['w_kv_up'][l], 128, 512, C.dinT)
    gck = S.tile('gck', 1, F32)
    S.dma(gck[:, :], C.din['g_ckv'][l], r=[C.dinT], w=[gck])
    lbl = S.tile('lbl', C.DEPTH * 4, F32, parts=64)
    S.dma(lbl[:, :], C.din['lb_logits'], r=[C.dinT], w=[lbl])
    lbe = S.tile('lbe', C.DEPTH * 4, F32, parts=64)
    for i in range(C.DEPTH):
        S.tt(lbe[:, i * 4:(i + 1) * 4], lbl[:, i * 4:(i + 1) * 4], lbl[:, 0:4], ALU.subtract, [lbl], [lbe])
    S.act(lbe[:, :], lbe[:, :], AF.Exp, [lbe], [lbe])
    tot = S.tile('lbtot', 4, F32, parts=64)
    num = S.tile('lbnum', 4, F32, parts=64)
    S.cp(tot[:, :], lbe[:, 0:4], [lbe], [tot])
    S.memset(num[:, :], 0.0, [num])
    for i in range(1, C.DEPTH):
        S.tt(tot[:, :], tot[:, :], lbe[:, i * 4:(i + 1) * 4], ALU.add, [tot, lbe], [tot])
        if i <= l:
            S.tt(num[:, :], num[:, :], lbe[:, i * 4:(i + 1) * 4], ALU.add, [num, lbe], [num])
    lb = S.tile('lb', 4, F32, parts=64)
    oml = S.tile('oml', 4, F32, parts=64)
    S.recip(tot[:, :], tot[:, :], [tot], [tot])
    S.tt(lb[:, :], num[:, :], tot[:, :], ALU.mult, [num, tot], [lb])
    S.ts(oml[:, :], lb[:, :], -1.0, 1.0, ALU.mult, ALU.add, [lb], [oml])

    xt = S.tile('xt', 4 * D, F32)
    junk = S.tile('junk', D, BF16)
    hb = [S.tile('hb0', D, BF16), S.tile('hb1', D, BF16)]
    hT = S.tile('hT', 8 * 512, BF16)
    ssq = S.tile('ssq', 4, F32)
    sd = S.tile('sd', 4, F32)
    rstd = S.tile('rstd', 4, F32)
    qk = S.tile('qk', 10 * 512, BF16)
    cf = S.tile('cf', 512, F32)
    csq = S.tile('csq', 512, BF16)
    crs = S.tile('crs', 512, F32)
    cTb = S.tile('cTb', 512, BF16)
    kast = S.tile('kast', 2 * 512, BF16)
    iqs = S.tile('iqs', 7 * 512, BF16)
    wP = S.tile('wP', 512, F32)
    wN = S.tile('wN', 512, F32)
    vst = S.tile('vst', 4 * 3 * 260, BF16)
    hvst = S.tile('hvst', 8 * 256, BF16, parts=64)
    hst = S.tile('hst', 5 * 4 * 512, BF16, parts=64)
    dst_ = S.tile('dst', 32, F32, parts=64)
    hf_ = [S.tile(f'hf{i}', 512, F32, parts=64) for i in range(8)]
    S.memset(vst[:, :], 1.0, [vst])
    vst4 = vst[:, :].rearrange("p (a m h c) -> p a m h c", a=4, m=3, h=4)
    fm = C.fm
    for ti in range(NTOK // 512):
        t0 = ti * 512
        S.dma(xt[:, :].rearrange("p (a d) -> p a d", a=4), C.xres[t0:t0 + 512, :].rearrange("(a p) d -> p a d", p=128),
              r=[C.xres], w=[xt])
        norm_front(S, C, xt, hb, hT, junk, ssq, sd, rstd)

        def fmproj(bank, col0, M):
            for c in range(8):
                S.mm(S.pb(bank, 0, 512, F32, 0, M), wi[:, c * NWX + col0:c * NWX + col0 + M], hT[:, c * 512:(c + 1) * 512],
                     c == 0, c == 7, [wi, hT], [S.banks[bank]])
        for g in range(10):
            b = 2 + g % 2
            fmproj(b, g * 128, 128)
            if g < 6:
                S.act(qk[:, g * 512:(g + 1) * 512], S.pb(b), AF.Copy, [S.banks[b]], [qk], scale=0.125)
            else:
                S.cp(qk[:, g * 512:(g + 1) * 512], S.pb(b), [S.banks[b]], [qk])
        S.dma(fm[0:1280, t0:t0 + 512].rearrange("(g p) t -> p g t", p=128), qk[:, :].rearrange("p (g t) -> p g t", g=10),
              r=[qk], w=[fm])
        fmproj(4, 10 * 128, 128)
        S.cp(cf[:, :], S.pb(4), [S.banks[4]], [cf], eng='act')
        S.act(csq[:, :], S.pb(4), AF.Square, [S.banks[4]], [csq])
        S.mm(S.pb(5), C.ones[:, :], csq[:, :], True, True, [C.ones, csq], [S.banks[5]])
        S.act(crs[:, :], S.pb(5), AF.Sqrt, [S.banks[5], C.eps], [crs], scale=1.0 / 128, bias=C.eps[:, 0:1])
        S.recip(crs[:, :], crs[:, :], [crs], [crs])
        S.stt(cTb[:, :], cf[:, :], gck[:, 0:1], crs[:, :], ALU.mult, ALU.mult, [cf, gck, crs], [cTb])
        for pr in range(2):
            S.mm(S.pb(6 + pr), wkv[:, pr * 128:(pr + 1) * 128], cTb[:, :], True, True, [wkv, cTb], [S.banks[6 + pr]])
            S.act(kast[:, pr * 512:(pr + 1) * 512], S.pb(6 + pr), AF.Copy, [S.banks[6 + pr]], [kast])
        S.dma(fm[R_K[0]:R_K[0] + 256, t0:t0 + 512].rearrange("(g p) t -> p g t", p=128),
              kast[:, :].rearrange("p (g t) -> p g t", g=2), r=[kast], w=[fm])
        for a in range(4):
            b = 2 + a % 2
            S.mm(S.pb(b, 0, 256), cTb[:, a * 128:(a + 1) * 128], wkv[:, 256:512], True, True, [cTb, wkv], [S.banks[b]])
            S.cp(vst4[:, a, 0, :, 0:64], S.pb(b, 0, 256).rearrange("p (h c) -> p h c", h=4), [S.banks[b]], [vst])
        for g in range(3):
            fmproj(4, (11 + g) * 128, 128)
            fmproj(5, (14 + g) * 128, 128)
            S.act(wP[:, :], S.pb(5), AF.Relu, [S.banks[5]], [wP])
            S.act(wN[:, :], S.pb(5), AF.Relu, [S.banks[5]], [wN], scale=-1.0)
            S.tt(iqs[:, g * 512:(g + 1) * 512], S.pb(4), wP[:, :], ALU.mult, [S.banks[4], wP], [iqs])
            S.stt(iqs[:, (3 + g) * 512:(4 + g) * 512], S.pb(4), -1.0, wN[:, :], ALU.mult, ALU.mult, [S.banks[4], wN], [iqs])
        fmproj(6, 17 * 128, 128)
        S.cp(iqs[:, 6 * 512:7 * 512], S.pb(6), [S.banks[6]], [iqs], eng='act')
        S.dma(fm[R_IQP:R_IQP + 896, t0:t0 + 512].rearrange("(g p) t -> p g t", p=128),
              iqs[:, :].rearrange("p (g t) -> p g t", g=7), r=[iqs], w=[fm])
        for a in range(4):
            b = 2 + a % 2
            for c in range(8):
                S.mm(S.pb(b), hT[:, c * 512 + a * 128:c * 512 + (a + 1) * 128], wi[:, c * NWX + TMV:c * NWX + TMV + 512],
                     c == 0, c == 7, [hT, wi], [S.banks[b]])
            S.cp(vst4[:, a, 1:3, :, 0:64], S.pb(b).rearrange("p (m h c) -> p m h c", m=2, h=4), [S.banks[b]], [vst],
                 eng=('act' if a % 2 else 'dve'))
        for m_ in range(3):
            S.dma(C.v1s[m_, t0:t0 + 512, :].rearrange("(a p) c -> p a c", p=128),
                  vst[:, :].rearrange("p (a m c) -> p a m c", a=4, m=3)[:, :, m_, :], r=[vst], w=[C.v1s])
        for ch in range(8):
            b = 4 + (ch // 2) % 2
            o = (ch % 2) * 256
            for c in range(8):
                S.mm(S.pb(b, o, o + 256, F32, 0, 64), hT[:, c * 512 + ch * 64:c * 512 + (ch + 1) * 64],
                     wi[:, c * NWX + TMI:c * NWX + TMI + 256], c == 0, c == 7, [hT, wi], [S.banks[b]])
            S.cp(hvst[:, ch * 256:(ch + 1) * 256], S.pb(b, o, o + 256, F32, 0, 64), [S.banks[b]], [hvst],
                 eng=('act' if ch % 2 else 'dve'))
        S.dma(C.hv[t0:t0 + 512, :].rearrange("(c p) v -> p c v", p=64), hvst[:, :].rearrange("p (c v) -> p c v", c=8),
              r=[hvst], w=[C.hv])
        hst4 = hst[:, :].rearrange("p (k h t) -> p k h t", k=5, h=4)
        for hd in range(4):
            fmproj(6, HG0 + hd * 64, 64)
            fmproj(7, HG0 + 256 + hd * 64, 64)
            fmproj(2, HG0 + 512 + hd * 64, 64)
            q_ps = S.pb(6, 0, 512, F32, 0, 64)
            sig, f_, lf, kin, b_, d1, d4, e1 = hf_
            S.act(sig[:, :], S.pb(7, 0, 512, F32, 0, 64), AF.Sigmoid, [S.banks[7]], [sig])
            S.ts(f_[:, :], sig[:, :], oml[:, hd:hd + 1], lb[:, hd:hd + 1], ALU.mult, ALU.add, [sig, oml, lb], [f_])
            S.act(lf[:, :], f_[:, :], AF.Ln, [f_], [lf])
            S.ts(kin[:, :], f_[:, :], -1.0, 1.0, ALU.mult, ALU.add, [f_], [kin])
            S.op('dve', lambda e, b_=b_, lf=lf: e.tensor_tensor_scan(b_[:, :], C.cmask[:, :], lf[:, :], 0.0,
                                                                        op0=ALU.mult, op1=ALU.add), [C.cmask, lf], [b_])
            b3 = b_[:, :].rearrange("p (c j) -> p c j", j=64)
            S.tt(d1[:, :].rearrange("p (c j) -> p c j", j=64), b3, b3[:, :, 31:32].broadcast_to([64, 8, 64]),
                 ALU.subtract, [b_], [d1])
            S.tt(d4[:, :].rearrange("p (c j) -> p c j", j=64), b3[:, :, 63:64].broadcast_to([64, 8, 64]), b3,
                 ALU.subtract, [b_], [d4])
            S.act(e1[:, :], d1[:, :], AF.Exp, [d1], [e1])
            S.tt(hst4[:, 0, hd, :], q_ps, e1[:, :], ALU.mult, [S.banks[6], e1], [hst])
            S.act(e1[:, :], d1[:, :], AF.Exp, [d1], [e1], scale=-1.0)
            S.tt(hst4[:, 1, hd, :], kin[:, :], e1[:, :], ALU.mult, [kin, e1], [hst])
            S.act(e1[:, :], b_[:, :], AF.Exp, [b_], [e1])
            S.tt(hst4[:, 2, hd, :], q_ps, e1[:, :], ALU.mult, [S.banks[6], e1], [hst])
            S.cp(dst_[:, hd * 8:(hd + 1) * 8], e1[:, :].rearrange("p (c j) -> p c j", j=64)[:, :, 63], [e1], [dst_])
            S.act(e1[:, :], d4[:, :], AF.Exp, [d4], [e1])
            S.tt(hst4[:, 3, hd, :], kin[:, :], e1[:, :], ALU.mult, [kin, e1], [hst])
            S.act(hst4[:, 4, hd, :], S.pb(2, 0, 512, F32, 0, 64), AF.Silu, [S.banks[2]], [hst])
        for k_ in range(5):
            S.dma(fm[R_H + k_ * 256:R_H + (k_ + 1) * 256, t0:t0 + 512].rearrange("(h p) t -> p h t", p=64), hst4[:, k_],
                  r=[hst], w=[fm])
        S.dma(C.hdec[:, t0 // 64:t0 // 64 + 8].rearrange("(h p) c -> p h c", p=64),
              dst_[:, :].rearrange("p (h c) -> p h c", h=4), r=[dst_], w=[C.hdec])
    S.phase_end()


def attn_group(S, C, m, G, s0, qT, kT, v1, HK, extra, pts, ost, st):
    T_ = C.T
    nkt = 4 * G + 4
    jmin = max(0, 4 * G - 16) if m == 2 else 0
    for hl in range(4):
        pr, hf = divmod(hl, 2)
        p0 = 64 * hf
        head = m * 4 + hl
        bo = 3 + st['oi'] % 2
        st['oi'] += 1
        for j in range(jmin, nkt):
            c0 = max(0, j - 4 * G) * 128
            N = 512 - c0
            dist0 = (4 * G * 128 + c0) - j * 128
            near = (m == 2) or dist0 < NEAR
            bs = st['si'] % 3
            st['si'] += 1
            nmm = 1 + (1 if near else 0)
            if m == 0:
                nmm += sum(1 for qt in range(4) if 4 * G + qt >= j)
            if m == 1 and DBG['bmask']:
                nmm += 1
            k = 0
            S.mm(S.pb(bs, c0, 512), kT[p0:p0 + 64, pr * T_ + j * 128:pr * T_ + (j + 1) * 128],
                 qT[p0:p0 + 64, pr * T_ + G * 512 + c0:pr * T_ + (G + 1) * 512], True, nmm == 1, [kT, qT], [S.banks[bs]])
            k += 1
            if near:
                S.mm(S.pb(bs, c0, 512), C.antiI[:, :], HK[hl][:, dist0:dist0 + N], False, k == nmm - 1,
                     [C.antiI, HK[hl]], [S.banks[bs]])
                k += 1
            if m == 0:
                for qt in range(4):
                    if 4 * G + qt >= j:
                        mk = extra[qt]
                        S.mm(S.pb(bs, qt * 128, (qt + 1) * 128), mk[:, j * 128:(j + 1) * 128], C.ident[:, :], False,
                             k == nmm - 1, [mk, C.ident], [S.banks[bs]])
                        k += 1
            if m == 1 and DBG['bmask']:
                ex = extra[hl // 2]
                hq = 32 * (hl % 2)
                S.mm(S.pb(bs, c0, 512), C.esel[hq:hq + 32, (j // 2) * 128:(j // 2 + 1) * 128],
                     ex[hq:hq + 32, c0:512], False, True, [C.esel, ex], [S.banks[bs]])
            pt = pts[st['pi'] % 3]
            st['pi'] += 1
            bcol = C.negM[:, 0:1] if near else C.far[:, head:head + 1]
            S.act(pt[:, c0:512], S.pb(bs, c0, 512), AF.Exp, [S.banks[bs], C.far, C.negM], [pt], bias=bcol)
            S.mm(S.pb(bo, c0, 512, F32, 0, 65), v1[:, j * 260 + hl * 65:j * 260 + hl * 65 + 65], pt[:, c0:512],
                 j == jmin, j == nkt - 1, [v1, pt], [S.banks[bo]])
        den, dhi, dlo, rd = st['den'], st['dhi'], st['dlo'], st['rd']
        S.cp(den[64:65, :], S.pb(bo, 0, 512, F32, 64, 65), [S.banks[bo]], [den], eng='act')
        S.cp(dhi[64:65, :], den[64:65, :], [den], [dhi])
        S.tt(dlo[64:65, :], den[64:65, :], dhi[64:65, :], ALU.subtract, [den, dhi], [dlo])
        S.mm(S.pb(6, 0, 512, F32, 0, 64), C.ones[64:65, 0:64], dhi[64:65, :], True, False, [C.ones, dhi], [S.banks[6]])
        S.mm(S.pb(6, 0, 512, F32, 0, 64), C.ones[64:65, 0:64], dlo[64:65, :], False, True, [C.ones, dlo], [S.banks[6]])
        S.recip(rd[0:64, :], S.pb(6, 0, 512, F32, 0, 64), [S.banks[6]], [rd])
        S.tt(ost[0:64, hl * 512:(hl + 1) * 512], S.pb(bo, 0, 512, F32, 0, 64), rd[0:64, :], ALU.mult,
             [S.banks[bo], rd], [ost])
    S.dma(C.oT[m_rows(m):m_rows(m) + 256, s0 + G * 512:s0 + (G + 1) * 512].rearrange("(h p) t -> p h t", p=64),
          ost[0:64, :].rearrange("p (h t) -> p h t", h=4), r=[ost], w=[C.oT])


def m_rows(m):
    return {0: 0, 1: 256, 2: 768}[m]


def load_hankel(S, C, m, HK):
    for hl in range(4):
        head = m * 4 + hl
        U = UD if m == 2 else UAB
        L = LD if m == 2 else LAB
        src = bass.AP(tensor=C.vecs.h.tensor, offset=head * LD, ap=[[1, 128], [1, U]])
        S.dma(HK[hl][:, 0:U], src, r=[C.vecs], w=[HK[hl]])


def mixer_attn_phase(S, C, l, si, m):
    T_ = C.T
    s0 = si * T_
    S.phase_begin()
    qT = S.tile('qT', 2 * T_, BF16)
    kT = S.tile('kT', 2 * T_, BF16)
    v1 = S.tile('v1', (T_ // 128) * 260, BF16)
    HK = [S.tile(f'HK{i}', UD if m == 2 else UAB, BF16) for i in range(4)]
    pts = [S.tile(f'pt{i}', 512, BF16) for i in range(3)]
    ost = S.tile('ost', 4 * 512, BF16, parts=64)
    st = dict(oi=0, si=0, pi=0, den=S.tile('den', 512, F32), dhi=S.tile('dhi', 512, BF16),
              dlo=S.tile('dlo', 512, BF16), rd=S.tile('rd', 512, F32, parts=64))
    fm = C.fm
    S.dma(qT[:, :].rearrange("p (g t) -> p g t", g=2), fm[R_Q[m]:R_Q[m] + 256, s0:s0 + T_].rearrange("(g p) t -> p g t", p=128),
          r=[fm], w=[qT])
    S.dma(kT[:, :].rearrange("p (g t) -> p g t", g=2), fm[R_K[m]:R_K[m] + 256, s0:s0 + T_].rearrange("(g p) t -> p g t", p=128),
          r=[fm], w=[kT])
    for j0 in range(0, T_ // 128, 4):
        S.dma(v1[:, j0 * 260:(j0 + 4) * 260].rearrange("p (j c) -> p j c", c=260),
              C.v1s[m, s0 + j0 * 128:s0 + (j0 + 4) * 128, :].rearrange("(j p) c -> p j c", p=128), r=[C.v1s], w=[v1])
    load_hankel(S, C, m, HK)
    NG = T_ // 512
    if m == 2:
        for G in range(NG):
            attn_group(S, C, m, G, s0, qT, kT, v1, HK, None, pts, ost, st)
    elif m == 1:
        nb = T_ // 256
        C.esel = S.tile('esel', 16 * 128, BF16, parts=64)
        S.dma(C.esel[:, :], C.din['c_esel'], r=[C.dinT], w=[C.esel])
        kmf = S.tile('kmf', 2 * nb, F32)
        kmT = S.tile('kmT', 2 * 16, BF16)
        S.memset(kmT[:, :], 0.0, [kmT])
        S.op('dve', lambda e: e.tensor_reduce(kmf[:, :].rearrange("p (g n) -> p g n", g=2), kT[:, :].rearrange("p (g n k) -> p g n k", g=2, n=nb),
                                              axis=AX.X, op=ALU.add), [kT], [kmf])
        S.ts(kmT[:, :].rearrange("p (g n) -> p g n", g=2)[:, :, 0:nb], kmf[:, :].rearrange("p (g n) -> p g n", g=2),
             1.0 / 256, None, ALU.mult, None, [kmf], [kmT])
        gm = S.tile('gm', 64, F32)
        m8 = S.tile('m8', 32, F32)
        mb = S.tile('mb', 128, BF16)
        mbT = [S.tile('mbT0', 512, BF16, parts=64), S.tile('mbT1', 512, BF16, parts=64)]
        S.memset(gm[:, :], -1e30, [gm])
        S.memset(mb[:, :], 0.0, [mb])
        for G in range(NG):
            for qt in range(4):
                n = 4 * G + qt
                own = n // 2
                if own > 0 and DBG['gate']:
                    for hl in range(4):
                        pr, hf = divmod(hl, 2)
                        p0 = 64 * hf
                        gb = 7 if hf == 0 else 5
                        S.mm(S.pb(gb, pr * 16, pr * 16 + own), qT[p0:p0 + 64, pr * T_ + n * 128:pr * T_ + (n + 1) * 128],
                             kmT[p0:p0 + 64, pr * 16:pr * 16 + own], True, True, [qT, kmT], [S.banks[gb]])
                    gm4 = gm[:, :].rearrange("p (r f n) -> p r f n", r=2, f=2)
                    for hf_ in range(2):
                        gb = 7 if hf_ == 0 else 5
                        S.cp(gm4[:, :, hf_, 0:own], S.pb(gb, 0, 32).rearrange("p (r n) -> p r n", r=2)[:, :, 0:own],
                             [S.banks[gb]], [gm])
                for hl in range(4):
                    S.op('dve', lambda e, hl=hl: e.max(m8[:, hl * 8:(hl + 1) * 8], gm[:, hl * 16:(hl + 1) * 16]), [gm], [m8])
                    S.ts(mb[:, hl * 32:hl * 32 + 16], gm[:, hl * 16:(hl + 1) * 16], m8[:, hl * 8 + 2:hl * 8 + 3], NEG,
                         ALU.is_lt, ALU.mult, [gm, m8], [mb])
                S.memset(mb[:, :].rearrange("p (h n) -> p h n", h=4)[:, :, own:own + 1], 0.0, [mb])
                for pr in range(2):
                    S.tr(S.pb(7, 256 + pr * 128, 384 + pr * 128, BF16, 0, 64), mb[:, pr * 64:(pr + 1) * 64], C.ident[:, :],
                         [mb, C.ident], [S.banks[7]])
                    S.cp(mbT[pr][:, qt * 128:(qt + 1) * 128], S.pb(7, 256 + pr * 128, 384 + pr * 128, BF16, 0, 64),
                         [S.banks[7]], [mbT[pr]], eng='act')
            attn_group(S, C, m, G, s0, qT, kT, v1, HK, mbT, pts, ost, st)
    else:
        iqP = S.tile('iqP', 3 * T_, BF16)
        iqN = S.tile('iqN', 3 * T_, BF16)
        ikT = S.tile('ikT', T_, BF16)
        S.dma(iqP[:, :].rearrange("p (g t) -> p g t", g=3), fm[R_IQP:R_IQP + 384, s0:s0 + T_].rearrange("(g p) t -> p g t", p=128),
              r=[fm], w=[iqP])
        S.dma(iqN[:, :].rearrange("p (g t) -> p g t", g=3), fm[R_IQN:R_IQN + 384, s0:s0 + T_].rearrange("(g p) t -> p g t", p=128),
              r=[fm], w=[iqN])
        S.dma(ikT[:, :], fm[R_IK:R_IK + 128, s0:s0 + T_], r=[fm], w=[ikT])
        idxs = [S.tile('idx0', T_, F32), S.tile('idx1', T_, F32)]
        mks = [S.tile(f'mk{i}', T_, BF16) for i in range(4)]
        cand = S.tile('cand', 1, F32)
        cnt = S.tile('cnt', 1, F32)
        dd = S.tile('dd', 1, F32)
        sB = S.tile('sB', 1, F32)
        topk = min(256, T_ // 4)
        R = 64.0
        itc = [0]

        def gen_accum(n):
            idx = idxs[n % 2]
            Sn = (n + 1) * 128
            first = True
            for sgn in range(2):
                src = iqP if sgn == 0 else iqN
                aop = ALU.max if sgn == 0 else ALU.min
                for h in range(8):
                    g, hp = divmod(h, 3)
                    for c_lo in range(0, Sn, 2048):
                        c_hi = min(Sn, c_lo + 2048)
                        b0 = (itc[0] % 2) * 4
                        itc[0] += 1
                        nb_ = (c_hi - c_lo + 511) // 512
                        for k in range(nb_):
                            a0 = c_lo + k * 512
                            a1 = min(c_hi, a0 + 512)
                            S.mm(S.pb(b0 + k, 0, a1 - a0), src[32 * hp:32 * hp + 32, g * T_ + n * 128:g * T_ + (n + 1) * 128],
                                 ikT[32 * hp:32 * hp + 32, a0:a1], True, True, [src, ikT], [S.banks[b0 + k]])
                        bl = [S.banks[b0 + k] for k in range(nb_)]
                        if first:
                            S.ts(idx[:, c_lo:c_hi], S.pspan(b0, c_hi - c_lo), 0.0, None, aop, None, bl, [idx])
                        else:
                            S.stt(idx[:, c_lo:c_hi], S.pspan(b0, c_hi - c_lo), 0.0, idx[:, c_lo:c_hi], aop, ALU.add,
                                  bl + [idx], [idx])
                        yield
                    first = False
            S.tt(idx[:, n * 128:(n + 1) * 128], idx[:, n * 128:(n + 1) * 128], C.tri[:, :], ALU.add, [idx, C.tri], [idx])
            yield

        def gen_bisect(n, qt):
            idx = idxs[n % 2]
            Sn = (n + 1) * 128
            mk = mks[qt]
            if Sn <= topk:
                S.ts(mk[:, 0:Sn], idx[:, 0:Sn], -1e29, NEG, ALU.is_lt, ALU.mult, [idx], [mk])
                return
            jA = T(mk.h, 'jA')
            jB = T(mk.h, 'jB')
            S.memset(cand[:, :], 0.0, [cand, mk])
            step = R
            hA = 0 if qt < 3 else max(128, (int(Sn * 0.42) // 128) * 128)
            nB = Sn - hA
            for i in range(C.NBIS):
                if hA > 0:
                    S.ts(jA[:, 0:hA], idx[:, 0:hA], cand[:, 0:1], 0.0, ALU.is_ge, ALU.add, [idx, cand], [jA, cnt],
                         accum_out=cnt[:, 0:1])
                S.act(jB[:, hA:Sn], idx[:, hA:Sn], AF.Sign, [idx, cand], [jB, sB], bias=cand[:, 0:1], scale=-1.0,
                      accum_out=sB[:, 0:1])
                yield
                if hA > 0:
                    S.stt(cnt[:, :], sB[:, :], -0.5, cnt[:, :], ALU.mult, ALU.add, [sB, cnt], [cnt])
                else:
                    S.ts(cnt[:, :], sB[:, :], -0.5, None, ALU.mult, None, [sB], [cnt])
                S.ts(dd[:, :], cnt[:, :], topk - 0.5 - nB / 2.0, step, ALU.is_ge, ALU.mult, [cnt], [dd])
                S.stt(cand[:, :], dd[:, :], -step / 2, cand[:, :], ALU.add, ALU.add, [dd, cand], [cand])
                step /= 2
            S.ts(cand[:, :], cand[:, :], -step, None, ALU.add, None, [cand], [cand])
            S.ts(mk[:, 0:Sn], idx[:, 0:Sn], cand[:, 0:1], NEG, ALU.is_lt, ALU.mult, [idx, cand, jA, jB], [mk])

        def drain(gen):
            for _ in gen:
                pass

        for G in range(NG):
            drain(gen_accum(4 * G))
            for qt in range(4):
                n = 4 * G + qt
                gb = gen_bisect(n, qt)
                ga = gen_accum(n + 1) if qt < 3 else iter(())
                nacc = 0 if qt == 3 else 16 * ((n + 2) * 128 + 2047) // 2048 + 1
                per = max(1, -(-nacc // C.NBIS))
                for _ in gb:
                    for _k in range(per):
                        next(ga, None)
                drain(ga)
            attn_group(S, C, m, G, s0, qT, kT, v1, HK, mks, pts, ost, st)
    S.phase_end()


def hgrn_phase(S, C, l, si):
    T_ = C.T
    s0 = si * T_
    S.phase_begin()
    fm = C.fm
    ins = [S.tile(f'hin{i}', 5 * 4 * 512, BF16, parts=64) for i in range(2)]
    vin = [S.tile(f'hvin{i}', 8 * 256, BF16, parts=64) for i in range(2)]
    dec = [S.tile(f'hdec{i}', 32, F32, parts=64) for i in range(2)]
    ATs = S.tile('ATs', 256, BF16, parts=64)
    ksT = S.tile('ksT', 256, BF16, parts=64)
    S32 = S.tile('S32', 256, F32, parts=64)
    Sbf = S.tile('Sbf', 256, BF16, parts=64)
    sq = S.tile('hsq', 512, BF16, parts=64)
    sdt = S.tile('hsd', 512, F32, parts=64)
    t1 = S.tile('ht1', 512, F32, parts=64)
    ost = S.tile('host', 4 * 512, BF16, parts=64)
    hg = S.tile('hgain', 4, F32, parts=64)
    S.dma(hg[:, :], C.din['g_hgrn'][l], r=[C.dinT], w=[hg])
    for ti in range(T_ // 512):
        t0 = s0 + ti * 512
        i4 = ins[ti % 2][:, :].rearrange("p (k h t) -> p k h t", k=5, h=4)
        for k_ in range(5):
            S.dma(i4[:, k_], fm[R_H + k_ * 256:R_H + (k_ + 1) * 256, t0:t0 + 512].rearrange("(h p) t -> p h t", p=64),
                  r=[fm], w=[ins[ti % 2]])
        vv = vin[ti % 2]
        S.dma(vv[:, :].rearrange("p (c v) -> p c v", c=8), C.hv[t0:t0 + 512, :].rearrange("(c p) v -> p c v", p=64),
              r=[C.hv], w=[vv])
        dc = dec[ti % 2]
        S.dma(dc[:, :].rearrange("p (h c) -> p h c", h=4), C.hdec[:, t0 // 64:t0 // 64 + 8].rearrange("(h p) c -> p h c", p=64),
              r=[C.hdec], w=[dc])
        inT = ins[ti % 2]
        for ch in range(8):
            cs = slice(ch * 64, (ch + 1) * 64)
            first = (ti == 0 and ch == 0)
            for hd in range(4):
                S.mm(S.pb(0, hd * 64, (hd + 1) * 64, F32, 0, 64), i4[:, 1, hd, cs], i4[:, 0, hd, cs], True, True,
                     [inT], [S.banks[0]])
            S.tt(ATs[:, :], S.pb(0, 0, 256, F32, 0, 64), C.hmask[:, :], ALU.mult, [S.banks[0], C.hmask], [ATs])
            for hd in range(4):
                S.tr(S.pb(1, hd * 64, (hd + 1) * 64, BF16, 0, 64), i4[:, 3, hd, cs], C.ident[0:64, 0:64],
                     [inT, C.ident], [S.banks[1]])
            S.cp(ksT[:, :], S.pb(1, 0, 256, BF16, 0, 64), [S.banks[1]], [ksT], eng='act')
            for hd in range(4):
                vs = vv[:, ch * 256 + hd * 64:ch * 256 + (hd + 1) * 64]
                S.mm(S.pb(3 + hd, ch * 64, (ch + 1) * 64, F32, 0, 64), vs, ATs[:, hd * 64:(hd + 1) * 64], True, first,
                     [vv, ATs], [S.banks[3 + hd]])
                if not first:
                    S.mm(S.pb(3 + hd, ch * 64, (ch + 1) * 64, F32, 0, 64), Sbf[:, hd * 64:(hd + 1) * 64], i4[:, 2, hd, cs],
                         False, True, [Sbf, inT], [S.banks[3 + hd]])
            for hd in range(4):
                vs = vv[:, ch * 256 + hd * 64:ch * 256 + (hd + 1) * 64]
                S.mm(S.pb(2, hd * 64, (hd + 1) * 64, F32, 0, 64), ksT[:, hd * 64:(hd + 1) * 64], vs, True, True,
                     [ksT, vv], [S.banks[2]])
            if first:
                S.cp(S32[:, :], S.pb(2, 0, 256, F32, 0, 64), [S.banks[2]], [S32])
            else:
                for hd in range(4):
                    s_ = S32[:, hd * 64:(hd + 1) * 64]
                    S.stt(s_, s_, dc[:, hd * 8 + ch:hd * 8 + ch + 1], S.pb(2, hd * 64, (hd + 1) * 64, F32, 0, 64),
                          ALU.mult, ALU.add, [S32, dc, S.banks[2]], [S32])
            S.cp(Sbf[:, :], S32[:, :], [S32], [Sbf], eng='act')
        for hd in range(4):
            ops = S.pb(3 + hd, 0, 512, F32, 0, 64)
            S.act(sq[:, :], ops, AF.Square, [S.banks[3 + hd]], [sq])
            S.mm(S.pb(7, 0, 512, F32, 0, 64), C.ones[0:64, 0:64], sq[:, :], True, True, [C.ones, sq], [S.banks[7]])
            S.act(sdt[:, :], S.pb(7, 0, 512, F32, 0, 64), AF.Sqrt, [S.banks[7], C.eps], [sdt], scale=1.0 / 64,
                  bias=C.eps[0:64, 0:1])
            S.recip(sdt[:, :], sdt[:, :], [sdt], [sdt])
            S.stt(t1[:, :], ops, hg[:, hd:hd + 1], sdt[:, :], ALU.mult, ALU.mult, [S.banks[3 + hd], hg, sdt], [t1])
            S.tt(ost[:, hd * 512:(hd + 1) * 512], t1[:, :], i4[:, 4, hd, :], ALU.mult, [t1, inT], [ost])
        S.dma(C.oT[512:768, t0:t0 + 512].rearrange("(h p) t -> p h t", p=64), ost[:, :].rearrange("p (h t) -> p h t", h=4),
              r=[ost], w=[C.oT])
    S.phase_end()


def wout_phase(S, C, l):
    S.phase_begin()
    wo = S.tile('wo', 8 * D, BF16)
    C.stage = [S.tile('st0', 512, F32), S.tile('st1', 512, F32)]
    C.stage_i = 0
    for c in range(8):
        load_cast(S, C, wo, c * D, C.din['w_out'][l, c * 128:(c + 1) * 128, :], 128, D, C.dinT)
    xts = [S.tile(f'xt{i}', 4 * D, F32) for i in range(2)]
    ots = [S.tile(f'ot{i}', 8 * 512, BF16) for i in range(2)]
    for ti in range(C.NTOK // 512):
        t0 = ti * 512
        xt = xts[ti % 2]
        ot = ots[ti % 2]
        S.dma(xt[:, :].rearrange("p (a d) -> p a d", a=4), C.xres[t0:t0 + 512, :].rearrange("(a p) d -> p a d", p=128),
              r=[C.xres], w=[xt])
        S.dma(ot[:, :].rearrange("p (c t) -> p c t", c=8), C.oT[:, t0:t0 + 512].rearrange("(c p) t -> p c t", p=128),
              r=[C.oT], w=[ot])
        for a in range(4):
            for hf in range(2):
                b = (a * 2 + hf) % 4
                for c in range(8):
                    S.mm(S.pb(b), ot[:, c * 512 + a * 128:c * 512 + (a + 1) * 128], wo[:, c * D + hf * 512:c * D + (hf + 1) * 512],
                         c == 0, c == 7, [ot, wo], [S.banks[b]])
                xs = xt[:, a * D + hf * 512:a * D + (hf + 1) * 512]
                S.tt(xs, S.pb(b), xs, ALU.add, [S.banks[b], xt], [xt])
        S.dma(C.xres[t0:t0 + 512, :].rearrange("(a p) d -> p a d", p=128), xt[:, :].rearrange("p (a d) -> p a d", a=4),
              r=[xt], w=[C.xres])
    S.phase_end()


def build(T_=4096, NSEQ=2, DEPTH=2, NBIS=23, stop_after=None):
    nc = bass.Bass("TRN2", target_bir_lowering=False)
    S = Sched(nc)
    C = Ctx()
    C.T, C.NSEQ, C.DEPTH, C.NBIS = T_, NSEQ, DEPTH, NBIS
    NTOK = C.NTOK = T_ * NSEQ
    C.dinT = T(None, 'din')
    din = {}

    def inp(name, shape, dt=F32):
        din[name] = nc.dram_tensor(name, list(shape), dt, kind="ExternalInput").ap()
    inp('x', [NTOK, D])
    for nm in ('ffn1', 'ffn2'):
        inp(nm + '_gate', [DEPTH, D, DFF])
        inp(nm + '_up', [DEPTH, D, DFF])
        inp(nm + '_down', [DEPTH, DFF, D])
        inp('g_' + nm, [DEPTH, 128, 8])
    inp('g_mix', [DEPTH, 128, 8])
    inp('w_in_x', [DEPTH, D, NWX])
    inp('g_ckv', [DEPTH, 128, 1])
    inp('w_kv_up', [DEPTH, 128, 512])
    inp('lb_logits', [64, DEPTH * 4])
    inp('g_hgrn', [DEPTH, 64, 4])
    inp('w_out', [DEPTH, D, D])
    inp('rel_bias', [32, 12])
    inp('norm_final_b', [128, D])
    inp('c_ident', [128, 128], BF16)
    inp('c_anti', [128, 128], BF16)
    inp('c_ones', [128, 128], BF16)
    inp('c_tri', [128, 128])
    inp('c_hmask', [64, 256])
    inp('c_cmask', [64, 512])
    inp('c_esel', [64, 16 * 128], BF16)
    inp('c_ohab', [33, LAB], BF16)
    inp('c_ohd', [33, LD], BF16)
    C.din = din
    y = S.dram('y', [NTOK, D], F32, kind="ExternalOutput")
    C.xres = S.dram('xres', [NTOK, D], F32)
    C.fm = S.dram('fm', [FMROWS, NTOK], BF16)
    C.v1s = S.dram('v1s', [3, NTOK, 260], BF16)
    C.hv = S.dram('hv', [NTOK, 256], BF16)
    C.hdec = S.dram('hdec', [256, NTOK // 64], F32)
    C.oT = S.dram('oT', [1024, NTOK], BF16, kind=('ExternalOutput' if stop_after is not None else 'Internal'))
    C.vecs = S.dram('vecs', [12, LD], BF16)
    xin = T(din['x'], 'xin', dram=True)

    def cload(name, key, cols, dt, parts=128):
        t = S.tile(name, cols, dt, parts=parts)
        S.dma(t[:, :], din[key], r=[C.dinT], w=[t])
        return t
    C.ident = cload('ident', 'c_ident', 128, BF16)
    C.antiI = cload('antiI', 'c_anti', 128, BF16)
    C.ones = cload('ones', 'c_ones', 128, BF16)
    C.tri = cload('tri', 'c_tri', 128, F32)
    C.hmask = cload('hmask', 'c_hmask', 256, F32, parts=64)
    C.cmask = cload('cmask', 'c_cmask', 512, F32, parts=64)
    C.eps = S.tile('eps', 1, F32)
    S.memset(C.eps[:, :], EPS, [C.eps])
    C.negM = S.tile('negM', 1, F32)
    S.memset(C.negM[:, :], 0.0, [C.negM])
    C.far = S.tile('far', 12, F32)
    S.dma(C.far[:, :], bass.AP(tensor=din['rel_bias'].tensor, offset=31 * 12, ap=[[0, 128], [1, 12]]), r=[C.dinT], w=[C.far])
    S.phase_begin()
    tabf = S.tile('tabf', 12, F32, parts=33)
    tabb = S.tile('tabb', 12, BF16, parts=33)
    S.memset(tabf[:, :], 1.0, [tabf])
    S.dma(tabf[0:32, :], din['rel_bias'], r=[C.dinT], w=[tabf])
    S.cp(tabb[:, :], tabf[:, :], [tabf], [tabb])
    ohab = cload('ohab', 'c_ohab', LAB, BF16, parts=33)
    ohd = cload('ohd', 'c_ohd', LD, BF16, parts=33)
    vst_ = S.tile('vecst', LD, BF16, parts=12)
    S.memset(vst_[:, :], 0.0, [vst_])
    for (oh, L, h0, h1) in ((ohab, LAB, 0, 8), (ohd, LD, 8, 12)):
        for c0 in range(0, L, 512):
            c1 = min(L, c0 + 512)
            S.mm(S.pb(0, 0, c1 - c0, F32, 0, 12), tabb[:, :], oh[:, c0:c1], True, True, [tabb, oh], [S.banks[0]])
            tmp = S.tile(f'vtmp{h0}_{c0}', 512, BF16, parts=12)
            S.cp(tmp[:, 0:c1 - c0], S.pb(0, 0, c1 - c0, F32, 0, 12), [S.banks[0]], [tmp])
            S.dma(C.vecs[h0:h1, c0:c1], tmp[h0:h1, 0:c1 - c0], r=[tmp], w=[C.vecs])
    S.phase_end()

    for l in range(DEPTH):
        if stop_after == ('const', l):
            break
        ffn_phase(S, C, l, 0, xin if l == 0 else C.xres, C.xres, False)
        if stop_after == ('ffn1', l):
            break
        proj_phase(S, C, l)
        if stop_after == ('proj', l):
            break
        for si in range(NSEQ):
            for m in (0, 1, 2):
                if m in DBG['mixers']:
                    mixer_attn_phase(S, C, l, si, m)
            if 3 in DBG['mixers']:
                hgrn_phase(S, C, l, si)
        wout_phase(S, C, l)
        if stop_after == ('wout', l):
            break
        last = (l == DEPTH - 1)
        ffn_phase(S, C, l, 1, C.xres, y if last else C.xres, last)
    if stop_after is not None:
        S.phase_begin()
        xt = S.tile('dbgx', 4 * D, F32)
        for ti in range(NTOK // 512):
            t0 = ti * 512
            S.dma(xt[:, :].rearrange("p (a d) -> p a d", a=4), C.xres[t0:t0 + 512, :].rearrange("(a p) d -> p a d", p=128),
                  r=[C.xres], w=[xt])
            S.dma(y[t0:t0 + 512, :].rearrange("(a p) d -> p a d", p=128), xt[:, :].rearrange("p (a d) -> p a d", a=4),
                  r=[xt], w=[y])
        S.phase_end()
    counts = S.finish()
    return nc, counts


def host_consts(DEPTH, inputs):
    bf = ml_dtypes.bfloat16
    f32 = np.float32
    c = {}
    c['c_ident'] = np.eye(128, dtype=f32).astype(bf)
    c['c_anti'] = np.eye(128, dtype=f32)[::-1].copy().astype(bf)
    c['c_ones'] = np.ones((128, 128), f32).astype(bf)
    t = np.arange(128)
    c['c_tri'] = np.where(t[None, :] <= t[:, None], 0.0, -1e30).astype(f32)
    s = np.arange(64)
    hm = (s[:, None] <= s[None, :]).astype(f32)
    c['c_hmask'] = np.ascontiguousarray(np.tile(hm, (1, 4)))
    cm = np.ones((64, 512), f32)
    cm[:, ::64] = 0.0
    c['c_cmask'] = cm
    es = np.zeros((64, 16, 128), f32)
    for hl in range(2):
        for n in range(16):
            es[hl * 32 + n, n, :] = 1.0
    c['c_esel'] = es.reshape(64, 16 * 128).astype(bf)
    a, d = onehots()
    c['c_ohab'] = a.astype(bf)
    c['c_ohd'] = d.astype(bf)
    cols = win_cols()
    c['w_in_x'] = np.ascontiguousarray(inputs['w_in'][:, :, cols])

    def gc(v):
        return np.ascontiguousarray(v.reshape(DEPTH, 8, 128).transpose(0, 2, 1))
    c['g_ffn1'] = gc(inputs['norm_ffn1'])
    c['g_ffn2'] = gc(inputs['norm_ffn2'])
    c['g_mix'] = gc(inputs['norm_mix'])
    c['g_ckv'] = np.ascontiguousarray(inputs['ckv_norm'].reshape(DEPTH, 128, 1))
    c['g_hgrn'] = np.ascontiguousarray(inputs['hgrn_norm'].reshape(DEPTH, 4, 64).transpose(0, 2, 1))
    c['lb_logits'] = np.ascontiguousarray(inputs['hgrn_lb_logits'].reshape(DEPTH, 4, 64).transpose(2, 0, 1).reshape(64, DEPTH * 4))
    c['norm_final_b'] = np.ascontiguousarray(np.broadcast_to(inputs['norm_final'][None, :], (128, D)))
    for k in ('ffn1_gate', 'ffn1_up', 'ffn1_down', 'ffn2_gate', 'ffn2_up', 'ffn2_down', 'w_kv_up', 'w_out', 'rel_bias'):
        c[k] = np.ascontiguousarray(inputs[k])
    return c


_CACHE = {}


def run(inputs, T_, NSEQ, DEPTH, ncores, NBIS=23, stop_after=None):
    inputs = {k: np.asarray(v) for k, v in inputs.items()}
    key = (T_, NSEQ, DEPTH, NBIS, stop_after)
    if key not in _CACHE:
        _CACHE[key] = build(T_, NSEQ, DEPTH, NBIS, stop_after)
    nc, counts = _CACHE[key]
    consts = host_consts(DEPTH, inputs)
    x = inputs['x'].astype(np.float32, copy=False)
    in_maps = []
    for ci in range(ncores):
        mp = dict(consts)
        mp['x'] = np.ascontiguousarray(x[ci * NSEQ:(ci + 1) * NSEQ].reshape(NSEQ * T_, D))
        in_maps.append(mp)
    import os, time as _t
    _t0 = _t.time()
    res = run_bass_kernel_spmd(nc, in_maps, core_ids=list(range(ncores)), trace=bool(os.environ.get('KTRACE')))
    if os.environ.get('KTRACE'):
        print('EXEC_NS', getattr(res, 'exec_time_ns', None), 'wall', _t.time() - _t0)
    out = np.concatenate([np.asarray(r['y']).reshape(NSEQ, T_, D) for r in res.results], axis=0)
    if stop_after is not None:
        DBG['oT'] = [np.asarray(r['oT']) for r in res.results]
    return out.astype(np.float32)


def kernel(**inputs):
    return run(inputs, 4096, 2, 2, 8)
```
